# Optimizing a Trainium2 kernel written in Bass

```python
import math
import jax
import jax.numpy as jnp
from jax import lax
import numpy as np

D_MODEL = 1024
BATCH = 8
SEQ = 8192
DEPTH = 4

CTX_LEN = 256
GRID_W = 64
N_EVEN = (DEPTH + 1) // 2
N_ODD = DEPTH // 2
EPS = 1e-6
N_MOD = 6

POOL_WIDTH = D_MODEL // 2
POOL_WINDOWS = (2, 4, 8, 16)
POOL_GROUP = POOL_WIDTH // len(POOL_WINDOWS)

MLA_HEADS = 8
Q_LORA = 256
KV_LORA = 128
QK_NOPE = 64
QK_ROPE = 32
V_HEAD = 64
QK_HEAD = QK_NOPE + QK_ROPE
ROPE_PAIRS_AXIS = QK_ROPE // 4
ROPE_BASE = 10000.0
Q_BLOCK = 128
EVEN_IN = POOL_WIDTH + Q_LORA + KV_LORA + QK_ROPE
EVEN_MIX = POOL_WIDTH + MLA_HEADS * V_HEAD

S5_WIDTH = D_MODEL
S5_GROUP = 16
S5_GROUPS = S5_WIDTH // S5_GROUP
S5_STATE = 64
DT_MIN = 1e-3
DT_MAX = 1e-1

N_EXPERTS = 16
CAPACITY_FACTOR = 2
D_FF_EXPERT = 2752

kernel_name = 'hybrid_pool_mla_s5_ecmoe_dit'


def rmsnorm(x, g):
    xf = x.astype(jnp.float32)
    y = xf * lax.rsqrt(jnp.mean(xf * xf, axis=-1, keepdims=True) + EPS)
    return (y * g.astype(jnp.float32)).astype(x.dtype)


def modulate(h, shift, scale):
    return h * (1 + scale) + shift


def centred_mean_minus_self(u, window):
    n = u.shape[1]
    lo = window // 2
    hi = window - lo - 1
    uf = u.astype(jnp.float32)
    cs = jnp.concatenate([jnp.zeros_like(uf[:, :1]), jnp.cumsum(uf, axis=1)], axis=1)
    cs = jnp.pad(cs, ((0, 0), (lo, hi), (0, 0)), mode='edge')
    total = cs[:, window:window + n] - cs[:, :n]
    t = jnp.arange(n)
    count = (jnp.minimum(t + hi + 1, n) - jnp.maximum(t - lo, 0)).astype(jnp.float32)
    return total / count[None, :, None] - uf


def pool_mixer(u, pool_w, pool_scale):
    b, n, _ = u.shape
    groups = jnp.stack(
        [centred_mean_minus_self(u[..., gi * POOL_GROUP:(gi + 1) * POOL_GROUP], w)
         for gi, w in enumerate(POOL_WINDOWS)], axis=2)
    y = jnp.einsum('bngc,gcd->bngd', groups.astype(u.dtype), pool_w)
    return y.reshape(b, n, POOL_WIDTH) * pool_scale


def axial_rope_table(rows):
    n = rows * GRID_W
    pos = jnp.arange(n)
    row = (pos // GRID_W).astype(jnp.float32)
    col = (pos % GRID_W).astype(jnp.float32)
    inv = ROPE_BASE ** (-jnp.arange(ROPE_PAIRS_AXIS, dtype=jnp.float32) / ROPE_PAIRS_AXIS)
    ang = jnp.concatenate([row[:, None] * inv, col[:, None] * inv], axis=-1)
    return jnp.cos(ang)[:, None, :], jnp.sin(ang)[:, None, :]


def apply_rope_tail(t, rope):
    if rope is None:
        return t
    cos, sin = rope
    half = QK_ROPE // 2
    tr = t[..., QK_NOPE:].astype(jnp.float32)
    t1, t2 = tr[..., :half], tr[..., half:]
    rot = jnp.concatenate([t1 * cos - t2 * sin, t1 * sin + t2 * cos], axis=-1)
    return jnp.concatenate([t[..., :QK_NOPE], rot.astype(t.dtype)], axis=-1)


def mla_queries(z, qa_g, w_uq, q_norm_g, rope):
    b, n, _ = z.shape
    cq = rmsnorm(z[..., :Q_LORA], qa_g)
    q = (cq @ w_uq).reshape(b, n, MLA_HEADS, QK_HEAD)
    return apply_rope_tail(rmsnorm(q, q_norm_g), rope)


def mla_keys_values(z, kva_g, w_ukv, k_norm_g, rope):
    b, n, _ = z.shape
    ckv = rmsnorm(z[..., Q_LORA:Q_LORA + KV_LORA], kva_g)
    k_pe = z[..., Q_LORA + KV_LORA:]
    kv = (ckv @ w_ukv).reshape(b, n, MLA_HEADS, QK_NOPE + V_HEAD)
    k_nope, v = kv[..., :QK_NOPE], kv[..., QK_NOPE:]
    k = jnp.concatenate([k_nope, jnp.broadcast_to(k_pe[:, :, None, :], (b, n, MLA_HEADS, QK_ROPE))], axis=-1)
    return apply_rope_tail(rmsnorm(k, k_norm_g), rope), v


def attend(q, k, v):
    s = jnp.einsum('bqhd,bkhd->bhqk', q, k, preferred_element_type=jnp.float32) * (QK_HEAD ** -0.5)
    p = jax.nn.softmax(s, axis=-1).astype(v.dtype)
    return jnp.einsum('bhqk,bkhd->bqhd', p, v, preferred_element_type=jnp.float32).astype(v.dtype)


def latent_attention(q, k_lat, v_lat, k_ctx, v_ctx):
    b, n, h, dq = q.shape
    k_all = jnp.concatenate([k_ctx, k_lat], axis=1)
    v_all = jnp.concatenate([v_ctx, v_lat], axis=1)
    q_blocks = q.reshape(b, n // Q_BLOCK, Q_BLOCK, h, dq).transpose(1, 0, 2, 3, 4)
    out = lax.map(lambda qb: attend(qb, k_all, v_all), q_blocks)
    return out.transpose(1, 0, 2, 3, 4).reshape(b, n, h * V_HEAD)


def even_mixer(a_lat, a_ctx, need_ctx, rope, w_in, pool_w, pool_scale,
               qa_g, w_uq, kva_g, w_ukv, q_norm_g, k_norm_g, w_out):
    p_lat = a_lat @ w_in
    p_ctx = a_ctx @ w_in
    z_lat, z_ctx = p_lat[..., POOL_WIDTH:], p_ctx[..., POOL_WIDTH:]
    k_lat, v_lat = mla_keys_values(z_lat, kva_g, w_ukv, k_norm_g, rope)
    k_ctx, v_ctx = mla_keys_values(z_ctx, kva_g, w_ukv, k_norm_g, None)
    q_lat = mla_queries(z_lat, qa_g, w_uq, q_norm_g, rope)
    att_lat = latent_attention(q_lat, k_lat, v_lat, k_ctx, v_ctx)
    pool_lat = pool_mixer(p_lat[..., :POOL_WIDTH], pool_w, pool_scale)
    mix_lat = jnp.concatenate([pool_lat, att_lat], axis=-1) @ w_out
    if not need_ctx:
        return mix_lat, None
    b, m, _ = a_ctx.shape
    q_ctx = mla_queries(z_ctx, qa_g, w_uq, q_norm_g, None)
    att_ctx = attend(q_ctx, k_ctx, v_ctx).reshape(b, m, MLA_HEADS * V_HEAD)
    pool_ctx = pool_mixer(p_ctx[..., :POOL_WIDTH], pool_w, pool_scale)
    mix_ctx = jnp.concatenate([pool_ctx, att_ctx], axis=-1) @ w_out
    return mix_lat, mix_ctx


def zoh_discretise(lam_re, lam_im, log_dt, b_re, b_im):
    lam = lax.complex(lam_re.astype(jnp.float32), lam_im.astype(jnp.float32))
    dt = jnp.exp(log_dt.astype(jnp.float32))[:, None]
    a_bar = jnp.exp(lam * dt)
    b_mat = lax.complex(b_re.astype(jnp.float32), b_im.astype(jnp.float32))
    b_bar = ((a_bar - 1) / lam)[..., None] * b_mat
    return a_bar, b_bar


def _linear_recurrence(e1, e2):
    a1, b1 = e1
    a2, b2 = e2
    return a1 * a2, a2 * b1 + b2


def diag_scan(u_g, a_bar, b_bar, h0, reverse):
    n = u_g.shape[1]
    bu = jnp.einsum('bngp,gsp->bngs', u_g.astype(jnp.complex64), b_bar)
    if h0 is not None:
        bu = bu.at[:, n - 1 if reverse else 0].add(a_bar * h0)
    a = jnp.broadcast_to(a_bar, (1, n) + a_bar.shape)
    _, h = lax.associative_scan(_linear_recurrence, (a, bu), reverse=reverse, axis=1)
    return h


def ssm_readout(h, c_mat):
    return jnp.real(jnp.einsum('bngs,gps->bngp', h, c_mat))


def s5_bidirectional(u_lat, u_ctx, need_ctx, lam_re, lam_im, log_dt, b_re, b_im, c_re, c_im, d):
    b, n, w = u_lat.shape
    m = u_ctx.shape[1]
    ug_lat = u_lat.astype(jnp.float32).reshape(b, n, S5_GROUPS, S5_GROUP)
    ug_ctx = u_ctx.astype(jnp.float32).reshape(b, m, S5_GROUPS, S5_GROUP)
    df = d.astype(jnp.float32)
    y_lat = df * u_lat.astype(jnp.float32)
    y_ctx = df * u_ctx.astype(jnp.float32)
    for direction in range(2):
        reverse = direction == 1
        a_bar, b_bar = zoh_discretise(lam_re[direction], lam_im[direction], log_dt[direction],
                                      b_re[direction], b_im[direction])
        c_mat = lax.complex(c_re[direction].astype(jnp.float32), c_im[direction].astype(jnp.float32))
        h_ctx = diag_scan(ug_ctx, a_bar, b_bar, None, reverse)
        h0 = h_ctx[:, 0] if reverse else h_ctx[:, -1]
        h_lat = diag_scan(ug_lat, a_bar, b_bar, h0, reverse)
        y_lat = y_lat + ssm_readout(h_lat, c_mat).reshape(b, n, w)
        if need_ctx:
            y_ctx = y_ctx + ssm_readout(h_ctx, c_mat).reshape(b, m, w)
    return y_lat.astype(u_lat.dtype), y_ctx.astype(u_ctx.dtype)


def glu_out(y, w_out):
    o = jax.nn.gelu(y) @ w_out
    return o[..., :D_MODEL] * jax.nn.sigmoid(o[..., D_MODEL:])


def odd_mixer(a_lat, a_ctx, need_ctx, w_in, lam_re, lam_im, log_dt, b_re, b_im, c_re, c_im, d, w_out):
    u_lat = a_lat @ w_in
    u_ctx = a_ctx @ w_in
    y_lat, y_ctx = s5_bidirectional(u_lat, u_ctx, need_ctx, lam_re, lam_im, log_dt, b_re, b_im, c_re, c_im, d)
    mix_lat = glu_out(y_lat, w_out)
    mix_ctx = glu_out(y_ctx, w_out) if need_ctx else None
    return mix_lat, mix_ctx


def expert_choice_ffn(h, router_w, w_gate, w_up, w_down):
    b, n, _ = h.shape
    cap = CAPACITY_FACTOR * n // N_EXPERTS
    aff = jax.nn.softmax(jnp.einsum('bnd,de->bne', h, router_w, preferred_element_type=jnp.float32), axis=-1)
    gate, idx = lax.top_k(jnp.swapaxes(aff, 1, 2), cap)
    bidx = jnp.arange(b)[:, None, None]
    xe = h[bidx, idx]
    g = jnp.einsum('becd,edf->becf', xe, w_gate)
    u = jnp.einsum('becd,edf->becf', xe, w_up)
    ye = jnp.einsum('becf,efd->becd', jax.nn.silu(g) * u, w_down)
    return jnp.zeros_like(h).at[bidx, idx].add(gate[..., None].astype(h.dtype) * ye)


def setup_inputs(seed: int = 0) -> dict:
    key = jax.random.key(seed)
    ks = iter(jax.random.split(key, 40))

    def nrm(shape, scale):
        return jax.random.normal(next(ks), shape, jnp.float32) * scale

    def gain(shape):
        return 1.0 + nrm(shape, 0.02)

    D = D_MODEL
    inputs = {}
    inputs['x'] = nrm((BATCH, SEQ, D), 1.0)
    inputs['c'] = nrm((BATCH, D), 1.0)
    inputs['ctx'] = nrm((BATCH, CTX_LEN, D), 1.0)
    inputs['c_ctx'] = nrm((D,), 1.0)
    inputs['ada_w'] = nrm((DEPTH, D, N_MOD * D), 0.5 * D ** -0.5)
    inputs['ada_b'] = nrm((DEPTH, N_MOD * D), 0.01)
    inputs['norm1_g'] = gain((DEPTH, D))
    inputs['norm2_g'] = gain((DEPTH, D))
    inputs['router_w'] = nrm((DEPTH, D, N_EXPERTS), D ** -0.5)
    inputs['exp_w_gate'] = nrm((DEPTH, N_EXPERTS, D, D_FF_EXPERT), D ** -0.5)
    inputs['exp_w_up'] = nrm((DEPTH, N_EXPERTS, D, D_FF_EXPERT), D ** -0.5)
    inputs['exp_w_down'] = nrm((DEPTH, N_EXPERTS, D_FF_EXPERT, D), D_FF_EXPERT ** -0.5)
    inputs['ev_w_in'] = nrm((N_EVEN, D, EVEN_IN), D ** -0.5)
    inputs['pool_w'] = nrm((N_EVEN, len(POOL_WINDOWS), POOL_GROUP, POOL_GROUP), POOL_GROUP ** -0.5)
    inputs['pool_scale'] = gain((N_EVEN, POOL_WIDTH))
    inputs['q_a_norm_g'] = gain((N_EVEN, Q_LORA))
    inputs['w_uq'] = nrm((N_EVEN, Q_LORA, MLA_HEADS * QK_HEAD), Q_LORA ** -0.5)
    inputs['kv_a_norm_g'] = gain((N_EVEN, KV_LORA))
    inputs['w_ukv'] = nrm((N_EVEN, KV_LORA, MLA_HEADS * (QK_NOPE + V_HEAD)), KV_LORA ** -0.5)
    inputs['q_norm_g'] = gain((N_EVEN, QK_HEAD))
    inputs['k_norm_g'] = gain((N_EVEN, QK_HEAD))
    inputs['ev_w_out'] = nrm((N_EVEN, EVEN_MIX, D), EVEN_MIX ** -0.5)
    inputs['od_w_in'] = nrm((N_ODD, D, S5_WIDTH), D ** -0.5)
    inputs['s5_lam_re'] = -0.5 + nrm((N_ODD, 2, S5_GROUPS, S5_STATE), 0.01)
    inputs['s5_lam_im'] = jnp.pi * jnp.arange(S5_STATE, dtype=jnp.float32) + nrm((N_ODD, 2, S5_GROUPS, S5_STATE), 0.01)
    inputs['s5_log_dt'] = jax.random.uniform(next(ks), (N_ODD, 2, S5_GROUPS), jnp.float32,
                                             minval=math.log(DT_MIN), maxval=math.log(DT_MAX))
    inputs['s5_b_re'] = nrm((N_ODD, 2, S5_GROUPS, S5_STATE, S5_GROUP), (2 * S5_GROUP) ** -0.5)
    inputs['s5_b_im'] = nrm((N_ODD, 2, S5_GROUPS, S5_STATE, S5_GROUP), (2 * S5_GROUP) ** -0.5)
    inputs['s5_c_re'] = nrm((N_ODD, 2, S5_GROUPS, S5_GROUP, S5_STATE), 0.25)
    inputs['s5_c_im'] = nrm((N_ODD, 2, S5_GROUPS, S5_GROUP, S5_STATE), 0.25)
    inputs['s5_d'] = nrm((N_ODD, S5_WIDTH), 1.0)
    inputs['od_w_out'] = nrm((N_ODD, S5_WIDTH, 2 * D), S5_WIDTH ** -0.5)
    return inputs


def reference(x, c, ctx, c_ctx, ada_w, ada_b, norm1_g, norm2_g, router_w, exp_w_gate, exp_w_up, exp_w_down,
              ev_w_in, pool_w, pool_scale, q_a_norm_g, w_uq, kv_a_norm_g, w_ukv, q_norm_g, k_norm_g, ev_w_out,
              od_w_in, s5_lam_re, s5_lam_im, s5_log_dt, s5_b_re, s5_b_im, s5_c_re, s5_c_im, s5_d, od_w_out):
    rows = x.shape[1] // GRID_W
    rope = axial_rope_table(rows)
    h_lat, h_ctx = x, ctx
    cond_lat = jax.nn.silu(c)[:, None, :]
    cond_ctx = jax.nn.silu(c_ctx)[None, None, :]
    for layer in range(DEPTH):
        need_ctx = layer < DEPTH - 1
        sh1, sc1, g1, sh2, sc2, g2 = jnp.split(cond_lat @ ada_w[layer] + ada_b[layer], N_MOD, axis=-1)
        csh1, csc1, cg1, csh2, csc2, cg2 = jnp.split(cond_ctx @ ada_w[layer] + ada_b[layer], N_MOD, axis=-1)
        a_lat = modulate(rmsnorm(h_lat, norm1_g[layer]), sh1, sc1)
        a_ctx = modulate(rmsnorm(h_ctx, norm1_g[layer]), csh1, csc1)
        j = layer // 2
        if layer % 2 == 0:
            mix_lat, mix_ctx = even_mixer(a_lat, a_ctx, need_ctx, rope, ev_w_in[j], pool_w[j], pool_scale[j],
                                          q_a_norm_g[j], w_uq[j], kv_a_norm_g[j], w_ukv[j],
                                          q_norm_g[j], k_norm_g[j], ev_w_out[j])
        else:
            mix_lat, mix_ctx = odd_mixer(a_lat, a_ctx, need_ctx, od_w_in[j], s5_lam_re[j], s5_lam_im[j],
                                         s5_log_dt[j], s5_b_re[j], s5_b_im[j], s5_c_re[j], s5_c_im[j],
                                         s5_d[j], od_w_out[j])
        h_lat = h_lat + g1 * mix_lat
        f_lat = expert_choice_ffn(modulate(rmsnorm(h_lat, norm2_g[layer]), sh2, sc2),
                                  router_w[layer], exp_w_gate[layer], exp_w_up[layer], exp_w_down[layer])
        h_lat = h_lat + g2 * f_lat
        if need_ctx:
            h_ctx = h_ctx + cg1 * mix_ctx
            f_ctx = expert_choice_ffn(modulate(rmsnorm(h_ctx, norm2_g[layer]), csh2, csc2),
                                      router_w[layer], exp_w_gate[layer], exp_w_up[layer], exp_w_down[layer])
            h_ctx = h_ctx + cg2 * f_ctx
    return h_lat
```

```python
import math
import numpy as np
from contextlib import ExitStack
import concourse.bass as bass
import concourse.mybir as mybir
from concourse.bass_utils import run_bass_kernel_spmd

F32 = mybir.dt.float32
BF16 = mybir.dt.bfloat16
I32 = mybir.dt.int32
U32 = mybir.dt.uint32
AF = mybir.ActivationFunctionType
ALU = mybir.AluOpType

D = 1024
KC = 8
NE = 16
DFF = 2752
NFC = 22
EPS = 1e-6
TWO_PI = 2.0 * math.pi


class K:
    def __init__(self, nc, es, n_dma_sems=8):
        self.nc = nc
        self.es = es
        self.eng = {"pe": nc.tensor, "act": nc.scalar, "dve": nc.vector, "pool": nc.gpsimd, "sp": nc.sync}
        self.csem, self.ccnt, self.cgen = {}, {}, {}
        for e in ("pe", "act", "dve", "pool"):
            self.cgen[e] = 0
            self.csem[e] = es.enter_context(nc.semaphore("c_%s_0" % e))
            self.ccnt[e] = 0
        self.dsem, self.dcnt, self.dnext, self.dgen = {}, {}, {}, {}
        for q in ("sp", "act", "pool"):
            self.dsem[q] = [es.enter_context(nc.semaphore("d_%s%d_0" % (q, i))) for i in range(n_dma_sems)]
            self.dcnt[q] = [0] * n_dma_sems
            self.dgen[q] = [0] * n_dma_sems
            self.dnext[q] = 0
        self.waited = {e: {} for e in self.eng}
        self.lastw = {}
        self.readers = {}
        self.ninstr = 0

    def _wait(self, e, tok):
        if tok is None:
            return
        sem, val, key = tok
        if self.waited[e].get(key, 0) >= val:
            return
        self.eng[e].wait_ge(sem, val)
        self.waited[e][key] = val

    def _deps(self, e, reads, writes):
        for r in reads:
            self._wait(e, self.lastw.get(r))
        for w in writes:
            self._wait(e, self.lastw.get(w))
            for t in self.readers.get(w, ()):
                self._wait(e, t)

    def _record(self, tok, reads, writes):
        for r in reads:
            lst = self.readers.setdefault(r, [])
            lst.append(tok)
            if len(lst) > 24:
                d = {}
                for t in lst:
                    if t[2] not in d or d[t[2]][1] < t[1]:
                        d[t[2]] = t
                self.readers[r] = list(d.values())
        for w in writes:
            self.lastw[w] = tok
            self.readers[w] = []

    def op(self, e, fn, reads=(), writes=()):
        self._deps(e, reads, writes)
        if self.ccnt[e] >= 30000:
            self.cgen[e] += 1
            self.csem[e] = self.es.enter_context(self.nc.semaphore("c_%s_%d" % (e, self.cgen[e])))
            self.ccnt[e] = 0
        ins = fn(self.eng[e])
        self.ccnt[e] += 1
        ins.then_inc(self.csem[e], 1)
        tok = (self.csem[e], self.ccnt[e], "c_%s_%d" % (e, self.cgen[e]))
        self._record(tok, reads, writes)
        self.ninstr += 1
        return tok

    def dma(self, q, out=None, in_=None, reads=(), writes=(), fn=None, **kw):
        self._deps(q, reads, writes)
        i = self.dnext[q]
        self.dnext[q] = (i + 1) % len(self.dsem[q])
        if self.dcnt[q][i] >= 30000:
            self._wait(q, (self.dsem[q][i], self.dcnt[q][i], "d_%s%d_%d" % (q, i, self.dgen[q][i])))
            self.dgen[q][i] += 1
            self.dsem[q][i] = self.es.enter_context(self.nc.semaphore("d_%s%d_%d" % (q, i, self.dgen[q][i])))
            self.dcnt[q][i] = 0
        sem = self.dsem[q][i]
        key = "d_%s%d_%d" % (q, i, self.dgen[q][i])
        if self.dcnt[q][i] > 0:
            self._wait(q, (sem, self.dcnt[q][i], key))
        if fn is not None:
            ins = fn(self.eng[q])
        else:
            ins = self.eng[q].dma_start(out=out, in_=in_, **kw)
        self.dcnt[q][i] += 16
        ins.then_inc(sem, 16)
        tok = (sem, self.dcnt[q][i], key)
        self._record(tok, reads, writes)
        self.ninstr += 1
        return tok

    def barrier(self):
        toks = []
        for e in ("pe", "act", "dve", "pool"):
            if self.ccnt[e] > 0:
                toks.append((self.csem[e], self.ccnt[e], "c_%s_%d" % (e, self.cgen[e])))
        for q in ("sp", "act", "pool"):
            for i in range(len(self.dsem[q])):
                if self.dcnt[q][i] > 0:
                    toks.append((self.dsem[q][i], self.dcnt[q][i], "d_%s%d_%d" % (q, i, self.dgen[q][i])))
        for e in self.eng:
            for t in toks:
                if not (t[2].startswith("c_" + e + "_")):
                    self._wait(e, t)

    def wait_all(self, e):
        for r, t in list(self.lastw.items()):
            self._wait(e, t)


class Prog:
    def __init__(self, T, M, depth, dbg=()):
        self.T, self.M, self.depth = T, M, depth
        self.TT = T + M
        self.NB = self.TT // 128
        self.dbg = dbg
        self.capL = 2 * T // NE
        self.capC = 2 * M // NE
        nc = self.nc = bass.Bass("TRN2", target_bir_lowering=False)
        self.es = ExitStack()
        self.k = K(nc, self.es)
        self._uid = 0

        def din(name, shape, dt=F32):
            return nc.dram_tensor(name, list(shape), dt, kind="ExternalInput").ap()

        self.din = din
        TT = self.TT
        self.x = din("x", [T, D])
        self.ctx = din("ctx", [M, D])
        self.cfm = din("cfm", [128, 2 * KC])
        self.ident = din("ident", [128, 128])
        self.pos96 = din("pos96", [96, TT])
        self.inv96 = din("inv96", [96, 1])
        self.permT = din("permT", [96, 96])
        self.selpe = din("selpe", [32, 96])
        self.band = din("band", [2, 128, 32])
        nl = depth
        ne_ = (depth + 1) // 2
        no_ = max(1, depth // 2)
        self.ada_w = din("ada_w", [nl, D, 6 * D])
        self.ada_b = din("ada_b", [nl, 6 * D])
        self.norm1_g = din("norm1_g", [nl, D])
        self.norm2_g = din("norm2_g", [nl, D])
        self.router_w = din("router_w", [nl, D, NE])
        self.w_gate = din("exp_w_gate", [nl, NE, D, DFF])
        self.w_up = din("exp_w_up", [nl, NE, D, DFF])
        self.w_down = din("exp_w_down", [nl, NE, DFF, D])
        self.ev_w_in = din("ev_w_in", [ne_, D, 928])
        self.pool_w = din("pool_w", [ne_, 4, 128, 128])
        self.pool_scale = din("pool_scale", [ne_, 512])
        self.qa_g = din("q_a_norm_g", [ne_, 256])
        self.w_uq = din("w_uq", [ne_, 256, 768])
        self.kva_g = din("kv_a_norm_g", [ne_, 128])
        self.w_ukv = din("w_ukv", [ne_, 128, 1024])
        self.qn_g = din("q_norm_g", [ne_, 96])
        self.kn_g = din("k_norm_g", [ne_, 96])
        self.ev_w_out = din("ev_w_out", [ne_, D, D])
        self.od_w_in = din("od_w_in", [no_, D, D])
        self.s5_lam_re = din("s5_lam_re", [no_, 2, 64, 64])
        self.s5_lam_im = din("s5_lam_im", [no_, 2, 64, 64])
        self.s5_log_dt = din("s5_log_dt", [no_, 2, 64])
        self.s5_b_re = din("s5_b_re", [no_, 2, 64, 64, 16])
        self.s5_b_im = din("s5_b_im", [no_, 2, 64, 64, 16])
        self.s5_c_re = din("s5_c_re", [no_, 2, 64, 16, 64])
        self.s5_c_im = din("s5_c_im", [no_, 2, 64, 16, 64])
        self.s5_d = din("s5_d", [no_, D])
        self.od_w_out = din("od_w_out", [no_, D, 2 * D])
        self.y = nc.dram_tensor("y", [T, D], F32, kind="ExternalOutput").ap()

        def scr(name, shape, dt=F32):
            return nc.dram_tensor(name, list(shape), dt, kind=("ExternalOutput" if name in dbg else "Internal")).ap()

        self.scr = scr
        self.h_all = scr("h_all", [TT, D])
        self.h2rows = scr("h2rows", [TT, D])
        self.poolT = scr("poolT", [512, TT])
        self.catT = scr("catT", [D, TT], BF16)
        self.qT = scr("qT", [8, 96, TT], BF16)
        self.kT = scr("kT", [8, 96, TT], BF16)
        self.vR = scr("vR", [8, TT, 64], BF16)
        self.attU = scr("attU", [8, 64, TT])
        self.asum = scr("asum", [8, TT])
        self.cosT = scr("cosT", [96, TT])
        self.sinT = scr("sinT", [96, TT])
        self.dbg_out = {}

        self.P = self.es
        self.idt = self.sb("idt", [128, 128])
        self.ones = self.sb("ones", [128, 128])
        self.cf = self.sb("cf", [128, 2 * KC])
        self.modrows = scr("modrows", [2, 6 * D])
        self.epsT = self.sb("epsT", [128, 1])
        self.ps = [self.es.enter_context(nc.psum_tensor("ps%d" % i, [128, 512], F32)) for i in range(8)]
        self.psi = 0

    def sb(self, name, shape, dt=F32, es=None):
        self._uid += 1
        return (es or self.es).enter_context(self.nc.sbuf_tensor("%s_u%d" % (name, self._uid), list(shape), dt))

    def dbg_tensor(self, name, shape, dt=F32):
        t = self.nc.dram_tensor("dbg_" + name, list(shape), dt, kind="ExternalOutput").ap()
        self.dbg_out[name] = t
        return t

    def nps(self, lo=0, hi=8):
        i = lo + (self.psi % (hi - lo))
        self.psi += 1
        return i

    def hreg(self, t0, n):
        return [("h", b) for b in range(t0 // 128, (t0 + n + 127) // 128)]

    def col_load(self, dst, src1d, p, writes):
        ncol = src1d.shape[0] // p
        for c in range(ncol):
            self.k.dma("sp", dst[0:p, c:c + 1], src1d[c * p:(c + 1) * p].rearrange("(p o) -> p o", o=1), writes=writes)

    def bcast_row(self, q, dst, row_ap, n, writes):
        self.k.dma(q, dst, row_ap.partition_broadcast(dst.shape[0]), writes=writes)

    def build(self):
        k, nc = self.k, self.nc
        T, M, TT = self.T, self.M, self.TT
        k.dma("sp", self.idt[:], self.ident[:, :], writes=["idt"])
        k.op("pool", lambda e: e.memset(self.ones[:], 1.0), writes=["ones"])
        k.op("pool", lambda e: e.memset(self.epsT[:], EPS), writes=["epsT"])
        k.dma("sp", self.h_all[0:M, :], self.ctx[:, :], writes=self.hreg(0, M))
        nch = max(1, T // 1024)
        for i in range(nch):
            n = T // nch
            k.dma("sp", self.h_all[M + i * n:M + (i + 1) * n, :], self.x[i * n:(i + 1) * n, :], writes=self.hreg(M + i * n, n))
        with ExitStack() as es:
            cf = self.cf
            sg = self.sb("cf_sg", [128, 2 * KC], es=es)
            k.dma("sp", cf[:], self.cfm[:, :], writes=["cf"])
            k.op("act", lambda e: e.activation(out=sg[:], in_=cf[:], func=AF.Sigmoid), reads=["cf"], writes=["cf_sg"])
            k.op("dve", lambda e: e.tensor_tensor(out=cf[:], in0=cf[:], in1=sg[:], op=ALU.mult), reads=["cf", "cf_sg"], writes=["cf"])
            self.rope_tables(es)
            k.barrier()
        for layer in range(self.depth):
            need_ctx = layer < self.depth - 1
            if True:
                self.adaln(layer)
                if layer % 2 == 0:
                    self.even_layer(layer, need_ctx)
                else:
                    self.odd_layer(layer, need_ctx)
                self.moe_layer(layer, need_ctx)
        nch = max(1, T // 1024)
        for i in range(nch):
            n = T // nch
            k.dma("sp", self.y[i * n:(i + 1) * n, :], self.h_all[M + i * n:M + (i + 1) * n, :], reads=self.hreg(M + i * n, n), writes=[("y", i)])
        for name, (src, reg) in getattr(self, "dbg_copies", {}).items():
            pass
        k.wait_all("sp")
        self.es.close()
        return nc

    def rope_tables(self, es0):
        k = self.k
        TT = self.TT
        with ExitStack() as es:
            W = 1024
            inv = self.sb("rp_inv", [96, 1], es=es)
            k.dma("sp", inv[:], self.inv96[:, :], writes=["rp_inv"])
            tl = [(self.sb("rp_a%d" % i, [96, W], es=es), self.sb("rp_ai%d" % i, [96, W], I32, es=es), self.sb("rp_b%d" % i, [96, W], es=es), self.sb("rp_c%d" % i, [96, W], es=es)) for i in range(2)]
            for ti_, t0 in enumerate(range(0, TT, W)):
                n = min(W, TT - t0)
                a, ai, b, c = tl[ti_ % 2]
                ra, rai, rb, rc = ("rpa", ti_ % 2), ("rpai", ti_ % 2), ("rpb", ti_ % 2), ("rpc", ti_ % 2)
                k.dma("sp", a[:, 0:n], self.pos96[:, t0:t0 + n], writes=[ra])
                k.op("dve", lambda e: e.tensor_scalar(out=a[:, 0:n], in0=a[:, 0:n], scalar1=inv[:, 0:1], scalar2=None, op0=ALU.mult), reads=[ra, "rp_inv"], writes=[ra])
                k.op("dve", lambda e: e.tensor_scalar(out=b[:, 0:n], in0=a[:, 0:n], scalar1=1.0 / TWO_PI, scalar2=None, op0=ALU.mult), reads=[ra], writes=[rb])
                k.op("dve", lambda e: e.tensor_copy(out=ai[:, 0:n], in_=b[:, 0:n]), reads=[rb], writes=[rai])
                k.op("dve", lambda e: e.tensor_copy(out=b[:, 0:n], in_=ai[:, 0:n]), reads=[rai], writes=[rb])
                k.op("dve", lambda e: e.scalar_tensor_tensor(out=a[:, 0:n], in0=b[:, 0:n], scalar=-TWO_PI, in1=a[:, 0:n], op0=ALU.mult, op1=ALU.add), reads=[ra, rb], writes=[ra])
                k.op("dve", lambda e: e.tensor_scalar(out=a[:, 0:n], in0=a[:, 0:n], scalar1=math.pi, scalar2=-math.pi, op0=ALU.min, op1=ALU.max), reads=[ra], writes=[ra])
                k.op("act", lambda e: e.activation(out=b[:, 0:n], in_=a[:, 0:n], func=AF.Sin), reads=[ra, rb], writes=[rb])
                k.dma("sp", self.sinT[:, t0:t0 + n], b[:, 0:n], reads=[rb], writes=[("sinT", t0)])
                k.op("dve", lambda e: e.scalar_tensor_tensor(out=c[:, 0:n], in0=a[:, 0:n], scalar=-1.0, in1=a[:, 0:n], op0=ALU.mult, op1=ALU.max), reads=[ra], writes=[rc])
                k.op("dve", lambda e: e.tensor_scalar(out=c[:, 0:n], in0=c[:, 0:n], scalar1=-1.0, scalar2=math.pi / 2, op0=ALU.mult, op1=ALU.add), reads=[rc], writes=[rc])
                k.op("act", lambda e: e.activation(out=c[:, 0:n], in_=c[:, 0:n], func=AF.Sin), reads=[rc], writes=[rc])
                k.dma("sp", self.cosT[:, t0:t0 + n], c[:, 0:n], reads=[rc], writes=[("cosT", t0)])
            k.barrier()
            self.rope_regs = [("sinT", t0) for t0 in range(0, TT, W)] + [("cosT", t0) for t0 in range(0, TT, W)]

    def adaln(self, layer):
        k = self.k
        with ExitStack() as es:
            wt = [self.sb("adaw_%d" % i, [128, KC, 512], es=es) for i in range(2)]
            bt = self.sb("adab", [2, 6 * D], es=es)
            mod = self.sb("adamod", [2, 6 * D], es=es)
            g1 = self.sb("ada_g1", [2, D], es=es)
            g2 = self.sb("ada_g2", [2, D], es=es)
            k.dma("sp", bt[:], self.ada_b[layer, :].partition_broadcast(2), writes=["adab"])
            k.dma("sp", g1[:], self.norm1_g[layer, :].partition_broadcast(2), writes=["ada_g"])
            k.dma("sp", g2[:], self.norm2_g[layer, :].partition_broadcast(2), writes=["ada_g"])
            for nb in range(12):
                i = nb % 2
                c0 = nb * 512
                k.dma("pool" if nb % 2 else "sp", wt[i][:], self.ada_w[layer, :, c0:c0 + 512].rearrange("(kc p) n -> p kc n", p=128), writes=[("adaw", i)])
                b = self.nps()
                for kc in range(KC):
                    k.op("pe", lambda e: e.matmul(self.ps[b][0:2, :], lhsT=self.cf[:, kc:2 * KC:KC], rhs=wt[i][:, kc, :], start=(kc == 0), stop=(kc == KC - 1)),
                         reads=["cf", ("adaw", i)], writes=[("ps", b)])
                k.op("dve", lambda e: e.tensor_tensor(out=mod[:, c0:c0 + 512], in0=self.ps[b][0:2, :], in1=bt[:, c0:c0 + 512], op=ALU.add),
                     reads=[("ps", b), "adab"], writes=["adamod"])
            for (gt, c0) in ((g1, D), (g2, 4 * D)):
                k.op("dve", lambda e: e.scalar_tensor_tensor(out=mod[:, c0:c0 + D], in0=mod[:, c0:c0 + D], scalar=1.0, in1=gt[:], op0=ALU.add, op1=ALU.mult),
                     reads=["adamod", "ada_g"], writes=["adamod"])
            k.dma("sp", self.modrows[:, :], mod[:], reads=["adamod"], writes=["modrows"])
            k.barrier()

    def load_mod(self, es, chunk, name):
        out = []
        for s in range(2):
            t = self.sb("%s_%d" % (name, s), [128, D], es=es)
            self.k.dma("sp", t[:], self.modrows[s, chunk * D:(chunk + 1) * D].partition_broadcast(128), reads=["modrows"], writes=[(name, s)])
            out.append(t)
        return out

    def tm_norm(self, ht, hreg, at, areg, s, Wt, SHt, n, tag):
        k = self.k
        i = self.nsc_i = (getattr(self, "nsc_i", 0) + 1) % len(self.nsc)
        ss, junk = self.nsc[i]
        rs, rj = ("nss", i), ("nj", i)
        k.op("act", lambda e: e.activation(out=junk[0:n, :], in_=ht[0:n, :], func=AF.Square, accum_out=ss[0:n, 0:1]), reads=[hreg], writes=[rs, rj])
        k.op("dve", lambda e: e.tensor_scalar(out=ss[0:n, 1:2], in0=ss[0:n, 0:1], scalar1=1.0 / D, scalar2=EPS, op0=ALU.mult, op1=ALU.add), reads=[rs], writes=[rs])
        k.op("act", lambda e: e.activation(out=ss[0:n, 2:3], in_=ss[0:n, 1:2], func=AF.Sqrt), reads=[rs], writes=[rs])
        k.op("dve", lambda e: e.reciprocal(out=ss[0:n, 3:4], in_=ss[0:n, 2:3]), reads=[rs], writes=[rs])
        k.op("dve", lambda e: e.scalar_tensor_tensor(out=at[0:n, :], in0=ht[0:n, :], scalar=ss[0:n, 3:4], in1=Wt[s][0:n, :], op0=ALU.mult, op1=ALU.mult),
             reads=[hreg, rs, (tag + "_W", s)], writes=[areg])
        k.op("pool", lambda e: e.tensor_tensor(out=at[0:n, :], in0=at[0:n, :], in1=SHt[s][0:n, :], op=ALU.add), reads=[areg, (tag + "_SH", s)], writes=[areg])

    def transpose_block(self, at, areg, n, dst, dreg, col0, evac="act"):
        k = self.k
        for half in range(2):
            b = self.nps()
            for j in range(4):
                kc = half * 4 + j
                k.op("pe", lambda e: e.transpose(out=self.ps[b][:, j * 128:j * 128 + n], in_=at[0:n, kc * 128:(kc + 1) * 128], identity=self.idt[0:n, 0:n]),
                     reads=[areg, "idt"], writes=[("ps", b)])
            src = self.ps[b][:, :].rearrange("p (j c) -> p j c", j=4)[:, :, 0:n]
            dsta = dst[:, half * 4:half * 4 + 4, col0:col0 + n]
            if evac == "act":
                k.op("act", lambda e: e.activation(out=dsta, in_=src, func=AF.Identity), reads=[("ps", b)], writes=[dreg])
            else:
                k.op("dve", lambda e: e.tensor_copy(out=dsta, in_=src), reads=[("ps", b)], writes=[dreg])

    def load_w_bf16(self, dst, src_rows_ap, writes, q="pool"):
        nk = dst.shape[1]
        for kc in range(nk):
            self.k.dma(q, dst[:, kc, :], src_rows_ap[kc * 128:(kc + 1) * 128, :], writes=writes, max_dma_last_dim=4096)

    def stiles(self):
        out = []
        for t0 in range(0, self.M, 512):
            out.append((t0, min(512, self.M - t0), 1))
        for t0 in range(self.M, self.TT, 512):
            out.append((t0, min(512, self.TT - t0), 0))
        return out

    def even_layer(self, layer, need_ctx):
        j = layer // 2
        self.even_proj(layer, j)
        self.pool_stage(j)
        self.attention(j, need_ctx)
        self.mix_out(layer, self.ev_w_out[j], 8, need_ctx, self.even_cat_loader)

    def even_proj(self, layer, j):
        k = self.k
        M, TT = self.M, self.TT
        with ExitStack() as es:
            wIn = self.sb("ev_wIn", [128, KC, 928], BF16, es=es)
            self.load_w_bf16(wIn, self.ev_w_in[j], ["ev_wIn"])
            wuq = self.sb("ev_wuq", [128, 2, 768], BF16, es=es)
            self.load_w_bf16(wuq, self.w_uq[j], ["ev_wuq"])
            wkpad = self.sb("ev_wk", [128, 8, 96], BF16, es=es)
            wv = self.sb("ev_wv", [128, 8, 64], BF16, es=es)
            k.op("pool", lambda e: e.memset(wkpad[:], 0.0), writes=["ev_wk"])
            ukv = self.w_ukv[j].rearrange("k (h two d) -> k h two d", h=8, two=2)
            k.dma("pool", wkpad[:, :, 0:64], ukv[:, :, 0, :], reads=[], writes=["ev_wk"])
            k.dma("pool", wv[:], ukv[:, :, 1, :], writes=["ev_wv"])
            selpe = self.sb("ev_selpe", [32, 96], BF16, es=es)
            k.dma("pool", selpe[:], self.selpe[:, :], writes=["ev_selpe"])
            permT = self.sb("ev_permT", [96, 96], es=es)
            k.dma("sp", permT[:], self.permT[:, :], writes=["ev_permT"])
            gq = self.sb("ev_gq", [128, 2], es=es)
            gkv = self.sb("ev_gkv", [128, 1], es=es)
            gqn = self.sb("ev_gqn", [96, 1], es=es)
            gkn = self.sb("ev_gkn", [96, 1], es=es)
            self.col_load(gq, self.qa_g[j, :], 128, ["ev_g"])
            self.col_load(gkv, self.kva_g[j, :], 128, ["ev_g"])
            self.col_load(gqn, self.qn_g[j, :], 96, ["ev_g"])
            self.col_load(gkn, self.kn_g[j, :], 96, ["ev_g"])
            Wt = self.load_mod(es, 1, "ev_W")
            SHt = self.load_mod(es, 0, "ev_SH")

            ht = [self.sb("ev_h%d" % i, [128, D], es=es) for i in range(2)]
            at = [self.sb("ev_a%d" % i, [128, D], es=es) for i in range(2)]
            self.nsc = [(self.sb("ev_ss%d" % i, [128, 4], es=es), self.sb("ev_jk%d" % i, [128, D], BF16, es=es)) for i in range(2)]
            aT = [self.sb("ev_aT%d" % i, [128, KC, 512], BF16, es=es) for i in range(2)]
            poolS = [self.sb("ev_pS%d" % i, [128, 4, 512], es=es) for i in range(2)]
            zq = self.sb("ev_zq", [128, 2, 512], es=es)
            zkv = self.sb("ev_zkv", [128, 512], es=es)
            kpe = self.sb("ev_kpe", [32, 512], BF16, es=es)
            sq = self.sb("ev_sq", [128, 2, 512], es=es)
            rq = self.sb("ev_rq", [128, 512], es=es)
            cqT = self.sb("ev_cqT", [128, 2, 512], BF16, es=es)
            ckvT = self.sb("ev_ckvT", [128, 512], BF16, es=es)
            cosS = self.sb("ev_cos", [96, 512], es=es)
            sinS = self.sb("ev_sin", [96, 512], es=es)
            hq = [self.sb("ev_hq%d" % i, [96, 512], es=es) for i in range(2)]
            hsq = [self.sb("ev_hsq%d" % i, [96, 512], es=es) for i in range(2)]
            hr = [self.sb("ev_hr%d" % i, [96, 512], es=es) for i in range(2)]
            hn = [self.sb("ev_hn%d" % i, [96, 512], es=es) for i in range(2)]
            ho = [self.sb("ev_ho%d" % i, [96, 512], BF16, es=es) for i in range(2)]
            vS = [self.sb("ev_vS%d" % i, [128, 512], BF16, es=es) for i in range(2)]
            hcnt = [0]
            bi = 0
            for sti, (t0, n, s) in enumerate(self.stiles()):
                ai = sti % 2
                for blk in range(n // 128):
                    i = bi % 2
                    bi += 1
                    r0 = t0 + blk * 128
                    k.dma("sp", ht[i][:], self.h_all[r0:r0 + 128, :], reads=self.hreg(r0, 128), writes=[("ev_h", i)])
                    self.tm_norm(ht[i], ("ev_h", i), at[i], ("ev_a", i), s, Wt, SHt, 128, "ev")
                    self.transpose_block(at[i], ("ev_a", i), 128, aT[ai], ("ev_aT", ai), blk * 128)
                for mc in range(8):
                    msz = 128 if mc < 7 else 32
                    b = self.nps()
                    for kc in range(KC):
                        k.op("pe", lambda e: e.matmul(self.ps[b][0:msz, 0:n], lhsT=wIn[:, kc, mc * 128:mc * 128 + msz], rhs=aT[ai][:, kc, 0:n], start=(kc == 0), stop=(kc == KC - 1)),
                             reads=["ev_wIn", ("ev_aT", ai)], writes=[("ps", b)])
                    if mc < 4:
                        k.op("act", lambda e: e.activation(out=poolS[ai][:, mc, 0:n], in_=self.ps[b][:, 0:n], func=AF.Identity), reads=[("ps", b)], writes=[("ev_pS", ai)])
                    elif mc < 6:
                        k.op("act", lambda e: e.activation(out=zq[:, mc - 4, 0:n], in_=self.ps[b][:, 0:n], func=AF.Identity), reads=[("ps", b)], writes=["ev_zq"])
                        k.op("dve", lambda e: e.tensor_tensor(out=sq[:, mc - 4, 0:n], in0=self.ps[b][:, 0:n], in1=zq[:, mc - 4, 0:n], op=ALU.mult), reads=[("ps", b), "ev_zq"], writes=["ev_sq"])
                    elif mc == 6:
                        k.op("act", lambda e: e.activation(out=zkv[:, 0:n], in_=self.ps[b][:, 0:n], func=AF.Identity), reads=[("ps", b)], writes=["ev_zkv"])
                    else:
                        k.op("act", lambda e: e.activation(out=kpe[:, 0:n], in_=self.ps[b][0:32, 0:n], func=AF.Identity), reads=[("ps", b)], writes=["ev_kpe"])
                k.dma("sp", self.poolT[:, t0:t0 + n].rearrange("(g p) t -> p g t", p=128), poolS[ai][:, :, 0:n], reads=[("ev_pS", ai)], writes=[("poolT", t0)])
                k.dma("sp", cosS[:, 0:n], self.cosT[:, t0:t0 + n], reads=self.rope_regs, writes=["ev_cos"])
                k.dma("sp", sinS[:, 0:n], self.sinT[:, t0:t0 + n], reads=self.rope_regs, writes=["ev_sin"])
                b = self.nps()
                for c in range(2):
                    k.op("pe", lambda e: e.matmul(self.ps[b][:, 0:n], lhsT=self.ones[:, :], rhs=sq[:, c, 0:n], start=(c == 0), stop=(c == 1)), reads=["ones", "ev_sq"], writes=[("ps", b)])
                k.op("act", lambda e: e.activation(out=rq[:, 0:n], in_=self.ps[b][:, 0:n], func=AF.Sqrt, scale=1.0 / 256, bias=self.epsT[0:128, 0:1]), reads=[("ps", b), "epsT"], writes=["ev_rq"])
                k.op("dve", lambda e: e.reciprocal(out=rq[:, 0:n], in_=rq[:, 0:n]), reads=["ev_rq"], writes=["ev_rq"])
                for c in range(2):
                    k.op("dve", lambda e: e.scalar_tensor_tensor(out=cqT[:, c, 0:n], in0=zq[:, c, 0:n], scalar=gq[:, c:c + 1], in1=rq[:, 0:n], op0=ALU.mult, op1=ALU.mult),
                         reads=["ev_zq", "ev_g", "ev_rq"], writes=["ev_cqT"])
                k.op("pool", lambda e: e.tensor_tensor(out=sq[:, 0, 0:n], in0=zkv[:, 0:n], in1=zkv[:, 0:n], op=ALU.mult), reads=["ev_zkv", "ev_sq"], writes=["ev_sq"])
                b = self.nps()
                k.op("pe", lambda e: e.matmul(self.ps[b][:, 0:n], lhsT=self.ones[:, :], rhs=sq[:, 0, 0:n], start=True, stop=True), reads=["ones", "ev_sq"], writes=[("ps", b)])
                k.op("act", lambda e: e.activation(out=rq[:, 0:n], in_=self.ps[b][:, 0:n], func=AF.Sqrt, scale=1.0 / 128, bias=self.epsT[0:128, 0:1]), reads=[("ps", b), "ev_rq", "epsT"], writes=["ev_rq"])
                k.op("dve", lambda e: e.reciprocal(out=rq[:, 0:n], in_=rq[:, 0:n]), reads=["ev_rq"], writes=["ev_rq"])
                k.op("dve", lambda e: e.scalar_tensor_tensor(out=ckvT[:, 0:n], in0=zkv[:, 0:n], scalar=gkv[:, 0:1], in1=rq[:, 0:n], op0=ALU.mult, op1=ALU.mult),
                     reads=["ev_zkv", "ev_g", "ev_rq"], writes=["ev_ckvT"])
                for h in range(8):
                    for isk in range(2):
                        b = self.nps()
                        if isk == 0:
                            for c in range(2):
                                k.op("pe", lambda e: e.matmul(self.ps[b][0:96, 0:n], lhsT=wuq[:, c, h * 96:(h + 1) * 96], rhs=cqT[:, c, 0:n], start=(c == 0), stop=(c == 1)),
                                     reads=["ev_wuq", "ev_cqT"], writes=[("ps", b)])
                        else:
                            k.op("pe", lambda e: e.matmul(self.ps[b][0:96, 0:n], lhsT=wkpad[:, h, :], rhs=ckvT[:, 0:n], start=True, stop=False), reads=["ev_wk", "ev_ckvT"], writes=[("ps", b)])
                            k.op("pe", lambda e: e.matmul(self.ps[b][0:96, 0:n], lhsT=selpe[:, :], rhs=kpe[:, 0:n], start=False, stop=True), reads=["ev_selpe", "ev_kpe"], writes=[("ps", b)])
                        self.head_norm_rope(b, n, gkn if isk else gqn, (self.kT if isk else self.qT)[h, :, t0:t0 + n], ("kT" if isk else "qT", h, t0),
                                            hq, hsq, hr, hn, ho, permT, cosS, sinS, hcnt)
                for blk in range(n // 128):
                    b = self.nps()
                    vi = blk % 2
                    k.op("pe", lambda e: e.matmul(self.ps[b][:, 0:512], lhsT=ckvT[:, blk * 128:(blk + 1) * 128], rhs=wv[:].rearrange("p h d -> p (h d)"), start=True, stop=True),
                         reads=["ev_ckvT", "ev_wv"], writes=[("ps", b)])
                    k.op("act", lambda e: e.activation(out=vS[vi][:, :], in_=self.ps[b][:, :], func=AF.Identity), reads=[("ps", b)], writes=[("ev_vS", vi)])
                    r0 = t0 + blk * 128
                    k.dma("sp", self.vR[:, r0:r0 + 128, :].rearrange("h t d -> t h d"), vS[vi][:, :].rearrange("p (h d) -> p h d", h=8), reads=[("ev_vS", vi)], writes=[("vR", r0 // 128)])
            k.barrier()

    def head_norm_rope(self, b, n, gcol, out_ap, oreg, hq, hsq, hr, hn, ho, permT, cosS, sinS, hcnt):
        k = self.k
        i = hcnt[0] % 2
        hcnt[0] += 1
        R = lambda nm: (nm, i)
        k.op("act", lambda e: e.activation(out=hq[i][:, 0:n], in_=self.ps[b][0:96, 0:n], func=AF.Identity), reads=[("ps", b)], writes=[R("hq")])
        k.op("act", lambda e: e.activation(out=hsq[i][:, 0:n], in_=self.ps[b][0:96, 0:n], func=AF.Square), reads=[("ps", b)], writes=[R("hsq")])
        b2 = self.nps()
        k.op("pe", lambda e: e.matmul(self.ps[b2][0:96, 0:n], lhsT=self.ones[0:96, 0:96], rhs=hsq[i][:, 0:n], start=True, stop=True), reads=["ones", R("hsq")], writes=[("ps", b2)])
        k.op("act", lambda e: e.activation(out=hr[i][:, 0:n], in_=self.ps[b2][0:96, 0:n], func=AF.Sqrt, scale=1.0 / 96, bias=self.epsT[0:96, 0:1]), reads=[("ps", b2), "epsT"], writes=[R("hr")])
        k.op("dve", lambda e: e.reciprocal(out=hr[i][:, 0:n], in_=hr[i][:, 0:n]), reads=[R("hr")], writes=[R("hr")])
        k.op("dve", lambda e: e.scalar_tensor_tensor(out=hn[i][:, 0:n], in0=hq[i][:, 0:n], scalar=gcol[:, 0:1], in1=hr[i][:, 0:n], op0=ALU.mult, op1=ALU.mult),
             reads=[R("hq"), "ev_g", R("hr")], writes=[R("hn")])
        b3 = self.nps()
        k.op("pe", lambda e: e.matmul(self.ps[b3][0:96, 0:n], lhsT=permT[:, :], rhs=hn[i][:, 0:n], start=True, stop=True), reads=["ev_permT", R("hn")], writes=[("ps", b3)])
        k.op("dve", lambda e: e.tensor_tensor(out=hsq[i][:, 0:n], in0=self.ps[b3][0:96, 0:n], in1=sinS[:, 0:n], op=ALU.mult), reads=[("ps", b3), "ev_sin", R("hsq")], writes=[R("hsq")])
        k.op("pool", lambda e: e.tensor_tensor(out=hq[i][:, 0:n], in0=hn[i][:, 0:n], in1=cosS[:, 0:n], op=ALU.mult), reads=[R("hn"), "ev_cos", R("hq")], writes=[R("hq")])
        k.op("dve", lambda e: e.tensor_tensor(out=ho[i][:, 0:n], in0=hq[i][:, 0:n], in1=hsq[i][:, 0:n], op=ALU.add), reads=[R("hq"), R("hsq")], writes=[R("ho")])
        k.dma("sp", out_ap, ho[i][:, 0:n], reads=[R("ho")], writes=[oreg])

    def pool_stage(self, j):
        k = self.k
        M, T, TT = self.M, self.T, self.TT
        with ExitStack() as es:
            pw = self.sb("pl_w", [128, 4, 128], BF16, es=es)
            k.dma("pool", pw[:], self.pool_w[j].rearrange("g c d -> c g d"), writes=["pl_w"])
            psc = self.sb("pl_sc", [128, 4], es=es)
            self.col_load(psc, self.pool_scale[j, :], 128, ["pl_sc"])
            xin = [self.sb("pl_x%d" % i, [128, 528], es=es) for i in range(2)]
            s1 = [self.sb("pl_s%d" % i, [128, 528], es=es) for i in range(2)]
            s2 = [self.sb("pl_t%d" % i, [128, 528], es=es) for i in range(2)]
            rc = [self.sb("pl_rc%d" % i, [128, 512], es=es) for i in range(2)]
            po = [self.sb("pl_po%d" % i, [128, 512], BF16, es=es) for i in range(2)]
            co = [self.sb("pl_co%d" % i, [128, 512], BF16, es=es) for i in range(2)]
            cnt = 0
            allpool = [("poolT", t0) for (t0, n, s) in self.stiles()]
            for g, w in enumerate((2, 4, 8, 16)):
                lo = w // 2
                hi = w - lo - 1
                for (q0, qn) in ((0, M), (M, T)):
                    for t0 in range(q0, q0 + qn, 512):
                        n = min(512, q0 + qn - t0)
                        i = cnt % 2
                        cnt += 1
                        X, S1, S2, RC = ("pl_x", i), ("pl_s", i), ("pl_t", i), ("pl_rc", i)
                        a0 = max(q0, t0 - 8)
                        a1 = min(q0 + qn, t0 + n + 8)
                        k.op("pool", lambda e: e.memset(xin[i][:], 0.0), writes=[X])
                        k.dma("sp", xin[i][:, 8 - (t0 - a0):8 + (a1 - t0)], self.poolT[g * 128:(g + 1) * 128, a0:a1], reads=allpool, writes=[X])
                        L = n + 16
                        cur, cr = xin[i], X
                        step = 1
                        tmp = [(S1, s1[i]), (S2, s2[i])]
                        ti = 0
                        while step < w:
                            rgn, dst = tmp[ti % 2]
                            ti += 1
                            L2 = L - step
                            k.op("dve", lambda e: e.tensor_tensor(out=dst[:, 0:L2], in0=cur[:, 0:L2], in1=cur[:, step:step + L2], op=ALU.add), reads=[cr], writes=[rgn])
                            cur, cr, L = dst, rgn, L2
                            step *= 2
                        k.op("pool", lambda e: e.memset(rc[i][:], 1.0 / w), writes=[RC])
                        for tt in range(n):
                            t = t0 + tt - q0
                            c = min(t + hi + 1, qn) - max(t - lo, 0)
                            if c != w:
                                k.op("pool", lambda e: e.memset(rc[i][:, tt:tt + 1], 1.0 / c), writes=[RC])
                            elif tt > 16 and tt < n - 17:
                                pass
                        o = 8 - lo
                        rgn, dst = tmp[ti % 2]
                        k.op("dve", lambda e: e.tensor_tensor(out=dst[:, 0:n], in0=cur[:, o:o + n], in1=rc[i][:, 0:n], op=ALU.mult), reads=[cr, RC], writes=[rgn])
                        k.op("dve", lambda e: e.tensor_tensor(out=po[i][:, 0:n], in0=dst[:, 0:n], in1=xin[i][:, 8:8 + n], op=ALU.subtract), reads=[rgn, X], writes=[("pl_po", i)])
                        b = self.nps()
                        k.op("pe", lambda e: e.matmul(self.ps[b][:, 0:n], lhsT=pw[:, g, :], rhs=po[i][:, 0:n], start=True, stop=True), reads=["pl_w", ("pl_po", i)], writes=[("ps", b)])
                        k.op("act", lambda e: e.activation(out=co[i][:, 0:n], in_=self.ps[b][:, 0:n], func=AF.Identity, scale=psc[:, g:g + 1]), reads=[("ps", b), "pl_sc"], writes=[("pl_co", i)])
                        k.dma("sp", self.catT[g * 128:(g + 1) * 128, t0:t0 + n], co[i][:, 0:n], reads=[("pl_co", i)], writes=[("catT", g, t0)])
            k.barrier()

    def attention(self, j, need_ctx):
        k = self.k
        M, T, TT, NB = self.M, self.T, self.TT, self.NB
        scale = 96.0 ** -0.5
        qregs = lambda nm, h: [(nm, h, t0) for (t0, n, s) in self.stiles()]
        with ExitStack() as es:
            KT = [self.sb("at_K%d" % i, [96, TT], BF16, es=es) for i in range(2)]
            QT = [self.sb("at_Q%d" % i, [96, TT], BF16, es=es) for i in range(2)]
            VA = [self.sb("at_V%d" % i, [128, NB, 65], BF16, es=es) for i in range(2)]
            pT = [self.sb("at_p%d" % i, [128, 512], BF16, es=es) for i in range(3)]
            oS = [self.sb("at_o%d" % i, [65, 512], es=es) for i in range(2)]
            pcnt = 0
            ocnt = 0
            for h in range(8):
                i = h % 2
                k.dma("sp", KT[i][:], self.kT[h, :, :], reads=qregs("kT", h), writes=[("at_K", i)])
                k.dma("sp", QT[i][:], self.qT[h, :, :], reads=qregs("qT", h), writes=[("at_Q", i)])
                k.op("pool", lambda e: e.memset(VA[i][:], 1.0), writes=[("at_V", i)])
                k.dma("sp", VA[i][:, :, 0:64], self.vR[h, :, :].rearrange("(b p) d -> p b d", p=128), reads=[("vR", b_) for b_ in range(NB)], writes=[("at_V", i)])
                qtiles = [(t0, n, s) for (t0, n, s) in self.stiles() if (s == 0 or need_ctx)]
                for (t0, n, s) in qtiles:
                    nkb = NB if s == 0 else M // 128
                    ob = self.nps(0, 2)
                    for kb in range(nkb):
                        sbk = self.nps(2, 8)
                        k.op("pe", lambda e: e.matmul(self.ps[sbk][:, 0:n], lhsT=KT[i][:, kb * 128:(kb + 1) * 128], rhs=QT[i][:, t0:t0 + n], start=True, stop=True),
                             reads=[("at_K", i), ("at_Q", i)], writes=[("ps", sbk)])
                        pi = pcnt % 3
                        pcnt += 1
                        k.op("act", lambda e: e.activation(out=pT[pi][:, 0:n], in_=self.ps[sbk][:, 0:n], func=AF.Exp, scale=scale), reads=[("ps", sbk)], writes=[("at_p", pi)])
                        k.op("pe", lambda e: e.matmul(self.ps[ob][0:65, 0:n], lhsT=VA[i][:, kb, :], rhs=pT[pi][:, 0:n], start=(kb == 0), stop=(kb == nkb - 1)),
                             reads=[("at_V", i), ("at_p", pi)], writes=[("ps", ob)])
                    oi = ocnt % 2
                    ocnt += 1
                    k.op("dve", lambda e: e.tensor_copy(out=oS[oi][:, 0:n], in_=self.ps[ob][0:65, 0:n]), reads=[("ps", ob)], writes=[("at_o", oi)])
                    k.dma("sp", self.attU[h, :, t0:t0 + n], oS[oi][0:64, 0:n], reads=[("at_o", oi)], writes=[("attU", h, t0)])
                    k.dma("sp", self.asum[h:h + 1, t0:t0 + n], oS[oi][64:65, 0:n], reads=[("at_o", oi)], writes=[("asum", h, t0)])
            k.barrier()

    def even_cat_loader(self, es):
        k = self.k
        aU = [self.sb("mo_aU%d" % i, [128, 512], es=es) for i in range(2)]
        sB = [self.sb("mo_sB%d" % i, [128, 512], es=es) for i in range(2)]
        cnt = [0]

        def load(catS, creg, t0, n):
            k.dma("sp", catS[:, 0:4, 0:n], self.catT[0:512, t0:t0 + n].rearrange("(g p) t -> p g t", p=128),
                  reads=[("catT", g, t0) for g in range(4)], writes=[creg])
            for c in range(4):
                i = cnt[0] % 2
                cnt[0] += 1
                A, S = ("mo_aU", i), ("mo_sB", i)
                k.dma("sp", aU[i][:, 0:n], self.attU[2 * c:2 * c + 2, :, t0:t0 + n].rearrange("h d t -> (h d) t"),
                      reads=[("attU", 2 * c, t0), ("attU", 2 * c + 1, t0)], writes=[A])
                for hh in range(2):
                    k.dma("sp", sB[i][hh * 64:(hh + 1) * 64, 0:n], self.asum[2 * c + hh, t0:t0 + n].partition_broadcast(64),
                          reads=[("asum", 2 * c + hh, t0)], writes=[S])
                k.op("dve", lambda e: e.reciprocal(out=sB[i][:, 0:n], in_=sB[i][:, 0:n]), reads=[S], writes=[S])
                k.op("dve", lambda e: e.tensor_tensor(out=catS[:, 4 + c, 0:n], in0=aU[i][:, 0:n], in1=sB[i][:, 0:n], op=ALU.mult), reads=[A, S], writes=[creg])
        return load

    def mix_out(self, layer, w_ap, nkc, need_ctx, loader_factory, glu=False):
        k = self.k
        with ExitStack() as es:
            nout = 2 * D if glu else D
            wo = self.sb("mo_w", [128, nkc, nout], BF16, es=es)
            self.load_w_bf16(wo, w_ap, ["mo_w"])
            load = loader_factory(es)
            G1 = self.load_mod(es, 2, "mo_G1")
            catS = [self.sb("mo_cat%d" % i, [128, nkc, 512], BF16, es=es) for i in range(2)]
            ht = [self.sb("mo_h%d" % i, [128, D], es=es) for i in range(2)]
            tt = [self.sb("mo_t%d" % i, [128, D], es=es) for i in range(2)]
            sg = [self.sb("mo_sg%d" % i, [128, 512], es=es) for i in range(2)] if glu else None
            bi = 0
            for sti, (t0, n, s) in enumerate(self.stiles()):
                if s == 1 and not need_ctx:
                    continue
                ci = sti % 2
                load(catS[ci], ("mo_cat", ci), t0, n)
                for blk in range(n // 128):
                    i = bi % 2
                    bi += 1
                    r0 = t0 + blk * 128
                    k.dma("sp", ht[i][:], self.h_all[r0:r0 + 128, :], reads=self.hreg(r0, 128), writes=[("mo_h", i)])
                    for half in range(2):
                        b = self.nps()
                        for kc in range(nkc):
                            k.op("pe", lambda e: e.matmul(self.ps[b][:, :], lhsT=catS[ci][:, kc, blk * 128:(blk + 1) * 128], rhs=wo[:, kc, half * 512:(half + 1) * 512], start=(kc == 0), stop=(kc == nkc - 1)),
                                 reads=[("mo_cat", ci), "mo_w"], writes=[("ps", b)])
                        if glu:
                            b2 = self.nps()
                            for kc in range(nkc):
                                k.op("pe", lambda e: e.matmul(self.ps[b2][:, :], lhsT=catS[ci][:, kc, blk * 128:(blk + 1) * 128], rhs=wo[:, kc, D + half * 512:D + (half + 1) * 512], start=(kc == 0), stop=(kc == nkc - 1)),
                                     reads=[("mo_cat", ci), "mo_w"], writes=[("ps", b2)])
                            k.op("act", lambda e: e.activation(out=sg[i][:, :], in_=self.ps[b2][:, :], func=AF.Sigmoid), reads=[("ps", b2)], writes=[("mo_sg", i)])
                            k.op("dve", lambda e: e.tensor_tensor(out=sg[i][:, :], in0=self.ps[b][:, :], in1=sg[i][:, :], op=ALU.mult), reads=[("ps", b), ("mo_sg", i)], writes=[("mo_sg", i)])
                            k.op("dve", lambda e: e.tensor_tensor(out=tt[i][:, half * 512:(half + 1) * 512], in0=sg[i][:, :], in1=G1[s][:, half * 512:(half + 1) * 512], op=ALU.mult),
                                 reads=[("mo_sg", i), ("mo_G1", s)], writes=[("mo_t", i)])
                        else:
                            k.op("dve", lambda e: e.tensor_tensor(out=tt[i][:, half * 512:(half + 1) * 512], in0=self.ps[b][:, :], in1=G1[s][:, half * 512:(half + 1) * 512], op=ALU.mult),
                                 reads=[("ps", b), ("mo_G1", s)], writes=[("mo_t", i)])
                    k.op("pool", lambda e: e.tensor_tensor(out=tt[i][:, :], in0=tt[i][:, :], in1=ht[i][:, :], op=ALU.add), reads=[("mo_t", i), ("mo_h", i)], writes=[("mo_t", i)])
                    k.dma("sp", self.h_all[r0:r0 + 128, :], tt[i][:, :], reads=[("mo_t", i)], writes=self.hreg(r0, 128))
            k.barrier()

    def odd_layer(self, layer, need_ctx):
        jo = layer // 2
        if not hasattr(self, "Ugc"):
            NCH = self.TT // 8
            self.NCH = NCH
            self.Ugc = self.scr("Ugc", [128, 64, NCH])
            self.XR = self.scr("XR", [64, 2, 64, NCH])
            self.SF = self.scr("SF", [64, 2, 64, NCH])
            self.WinT = self.scr("WinT", [2, 2, 128, 64, 64])
            self.Wo = self.scr("Wo", [2, 2, 64, 64, 128])
            self.Mm = self.scr("Mm", [128, 64, 128])
            self.D8 = self.scr("D8", [2, 64, 2, 2, 64])
        self.s5_precompute(jo)
        self.s5_pass1(layer, jo)
        self.s5_pass2(layer, jo, need_ctx)

    def sincos(self, a, ai, b, c, P, n, reg):
        k = self.k
        ra, rb, rc = (reg, "a"), (reg, "b"), (reg, "c")
        A, AI, B, C = a[0:P, 0:n], ai[0:P, 0:n], b[0:P, 0:n], c[0:P, 0:n]
        k.op("dve", lambda e: e.tensor_scalar(out=B, in0=A, scalar1=1.0 / TWO_PI, scalar2=None, op0=ALU.mult), reads=[ra], writes=[rb])
        k.op("dve", lambda e: e.tensor_copy(out=AI, in_=B), reads=[rb], writes=[(reg, "ai")])
        k.op("dve", lambda e: e.tensor_copy(out=B, in_=AI), reads=[(reg, "ai")], writes=[rb])
        k.op("dve", lambda e: e.scalar_tensor_tensor(out=A, in0=B, scalar=-TWO_PI, in1=A, op0=ALU.mult, op1=ALU.add), reads=[ra, rb], writes=[ra])
        k.op("dve", lambda e: e.tensor_scalar(out=A, in0=A, scalar1=math.pi, scalar2=-math.pi, op0=ALU.min, op1=ALU.max), reads=[ra], writes=[ra])
        k.op("act", lambda e: e.activation(out=B, in_=A, func=AF.Sin), reads=[ra, rb], writes=[rb])
        k.op("dve", lambda e: e.scalar_tensor_tensor(out=C, in0=A, scalar=-1.0, in1=A, op0=ALU.mult, op1=ALU.max), reads=[ra], writes=[rc])
        k.op("dve", lambda e: e.tensor_scalar(out=C, in0=C, scalar1=-1.0, scalar2=math.pi / 2, op0=ALU.mult, op1=ALU.add), reads=[rc], writes=[rc])
        k.op("act", lambda e: e.activation(out=C, in_=C, func=AF.Sin), reads=[rc], writes=[rc])

    def s5_precompute(self, jo):
        k = self.k
        GB = 16
        with ExitStack() as es:
            sb = lambda n, sh, dt=F32: self.sb("s5p_" + n, sh, dt, es=es)
            maskf = sb("maskf", [128, 128])
            maskr = sb("maskr", [128, 128])
            DT = sb("DT", [128, 64])
            k.op("pool", lambda e: e.memset(maskf[:], 0.0), writes=["s5maskf"])
            k.op("pool", lambda e: e.memset(maskr[:], 0.0), writes=["s5maskr"])
            for jb in range(4):
                j0 = 2 * jb
                k.op("pool", lambda e: e.memset(maskf[jb * 32:(jb + 1) * 32, (j0 + 2) * 16:128], 1.0), writes=["s5maskf"]) if j0 + 2 < 8 else None
                k.op("pool", lambda e: e.memset(maskr[jb * 32:(jb + 1) * 32, 0:j0 * 16], 1.0), writes=["s5maskr"]) if j0 > 0 else None
            bandf = sb("bandf", [128, 32])
            bandr = sb("bandr", [128, 32])
            k.dma("sp", bandf[:], self.band[0], writes=["s5band"])
            k.dma("sp", bandr[:], self.band[1], writes=["s5band"])
            for jb in range(4):
                k.op("pool", lambda e: e.tensor_copy(out=maskf[jb * 32:(jb + 1) * 32, jb * 32:(jb + 1) * 32], in_=bandf[jb * 32:(jb + 1) * 32, :]), reads=["s5band"], writes=["s5maskf"])
                k.op("pool", lambda e: e.tensor_copy(out=maskr[jb * 32:(jb + 1) * 32, jb * 32:(jb + 1) * 32], in_=bandr[jb * 32:(jb + 1) * 32, :]), reads=["s5band"], writes=["s5maskr"])
            dsrc = self.s5_d[jo, :].rearrange("(g q) -> q g", q=16)
            for j in range(8):
                k.dma("sp", DT[j * 16:(j + 1) * 16, :], dsrc, writes=["s5DT"], allow_slow_non_contiguous=True)
            nat = sb("nat", [128, 64])
            lamr = sb("lamr", [64, 64]); lami = sb("lami", [64, 64]); dt = sb("dt", [64, 64])
            lr = sb("lr", [64, 64]); th = sb("th", [64, 64])
            MAG = sb("MAG", [64, 17, 64]); ANG = sb("ANG", [64, 17 * 64]); ANGi = sb("ANGi", [64, 17 * 64], I32)
            SN = sb("SN", [64, 17 * 64]); CS = sb("CS", [64, 17 * 64])
            TAr = sb("TAr", [64, 17, 64]); TAi = sb("TAi", [64, 17, 64])
            t1 = sb("t1", [64, 64]); t2 = sb("t2", [64, 64]); cr = sb("cr", [64, 64]); ci = sb("ci", [64, 64])
            bre = sb("bre", [64, 64, 16]); bim = sb("bim", [64, 64, 16]); Bbr = sb("Bbr", [64, 64, 16]); Bbi = sb("Bbi", [64, 64, 16])
            Ctr = sb("Ctr", [64, 64, 16]); Cti = sb("Cti", [64, 64, 16])
            d8 = sb("d8", [64, 2, 2, 64])
            Ere = sb("Ere", [64, GB, 8, 16]); EimN = sb("EimN", [64, GB, 8, 16]); Eim = sb("Eim", [64, GB, 8, 16])
            Rre = sb("Rre", [64, GB, 8, 16]); Rim = sb("Rim", [64, GB, 8, 16])
            Wre = sb("Wre", [64, GB, 8, 16]); WimN = sb("WimN", [64, GB, 8, 16])
            tmpA = sb("tmpA", [64, GB, 8, 16]); tmpB = sb("tmpB", [64, GB, 8, 16])
            wint = [sb("wint%d" % i, [128, 2, GB, 64]) for i in range(1)]
            Macc = sb("Macc", [128, 64, 128])
            mt = sb("mt", [128, 128])
            for d in range(2):
                R_ = lambda nm: ("s5p", nm)
                for (src, dst, nm) in ((self.s5_lam_re, lamr, "lamr"), (self.s5_lam_im, lami, "lami")):
                    k.dma("sp", nat[0:64, :], src[jo, d], writes=[R_("nat")])
                    b = self.nps()
                    k.op("pe", lambda e: e.transpose(out=self.ps[b][0:64, 0:64], in_=nat[0:64, :], identity=self.idt[0:64, 0:64]), reads=[R_("nat"), "idt"], writes=[("ps", b)])
                    k.op("dve", lambda e: e.tensor_copy(out=dst[:], in_=self.ps[b][0:64, 0:64]), reads=[("ps", b)], writes=[R_(nm)])
                k.dma("sp", dt[:], self.s5_log_dt[jo, d, :].partition_broadcast(64), writes=[R_("dt")])
                k.op("act", lambda e: e.activation(out=dt[:], in_=dt[:], func=AF.Exp), reads=[R_("dt")], writes=[R_("dt")])
                k.op("dve", lambda e: e.tensor_tensor(out=lr[:], in0=lamr[:], in1=dt[:], op=ALU.mult), reads=[R_("lamr"), R_("dt")], writes=[R_("lr")])
                k.op("dve", lambda e: e.tensor_tensor(out=th[:], in0=lami[:], in1=dt[:], op=ALU.mult), reads=[R_("lami"), R_("dt")], writes=[R_("th")])
                if d == 0:
                    kl = [7 - j for j in range(8)] + [t - 7 for t in range(8)] + [8]
                else:
                    kl = [j for j in range(8)] + [-t for t in range(8)] + [8]
                for i, kv in enumerate(kl):
                    k.op("act", lambda e: e.activation(out=MAG[:, i, :], in_=lr[:], func=AF.Exp, scale=float(kv)), reads=[R_("lr")], writes=[R_("MAG")])
                    k.op("dve", lambda e: e.tensor_scalar(out=ANG[:, i * 64:(i + 1) * 64], in0=th[:], scalar1=float(kv), scalar2=None, op0=ALU.mult), reads=[R_("th")], writes=[("s5sc", "a")])
                self.sincos(ANG, ANGi, SN, CS, 64, 17 * 64, "s5sc")
                k.op("dve", lambda e: e.tensor_tensor(out=TAr[:].rearrange("s k g -> s (k g)"), in0=MAG[:].rearrange("s k g -> s (k g)"), in1=CS[:], op=ALU.mult), reads=[R_("MAG"), ("s5sc", "c")], writes=[R_("TAr")])
                k.op("dve", lambda e: e.tensor_tensor(out=TAi[:].rearrange("s k g -> s (k g)"), in0=MAG[:].rearrange("s k g -> s (k g)"), in1=SN[:], op=ALU.mult), reads=[R_("MAG"), ("s5sc", "b")], writes=[R_("TAi")])
                TA = [R_("TAr"), R_("TAi")]
                i1 = kl.index(1)
                k.op("dve", lambda e: e.tensor_scalar(out=t1[:], in0=TAr[:, i1, :], scalar1=-1.0, scalar2=None, op0=ALU.add), reads=TA, writes=[R_("t1")])
                k.op("dve", lambda e: e.tensor_tensor(out=cr[:], in0=t1[:], in1=lamr[:], op=ALU.mult), reads=[R_("t1"), R_("lamr")], writes=[R_("cr")])
                k.op("dve", lambda e: e.tensor_tensor(out=t2[:], in0=TAi[:, i1, :], in1=lami[:], op=ALU.mult), reads=TA + [R_("lami")], writes=[R_("t2")])
                k.op("dve", lambda e: e.tensor_tensor(out=cr[:], in0=cr[:], in1=t2[:], op=ALU.add), reads=[R_("cr"), R_("t2")], writes=[R_("cr")])
                k.op("dve", lambda e: e.tensor_tensor(out=ci[:], in0=TAi[:, i1, :], in1=lamr[:], op=ALU.mult), reads=TA + [R_("lamr")], writes=[R_("ci")])
                k.op("dve", lambda e: e.tensor_tensor(out=t2[:], in0=t1[:], in1=lami[:], op=ALU.mult), reads=[R_("t1"), R_("lami"), R_("t2")], writes=[R_("t2")])
                k.op("dve", lambda e: e.tensor_tensor(out=ci[:], in0=ci[:], in1=t2[:], op=ALU.subtract), reads=[R_("ci"), R_("t2")], writes=[R_("ci")])
                k.op("dve", lambda e: e.tensor_tensor(out=t1[:], in0=lamr[:], in1=lamr[:], op=ALU.mult), reads=[R_("lamr"), R_("t1")], writes=[R_("t1")])
                k.op("dve", lambda e: e.tensor_tensor(out=t2[:], in0=lami[:], in1=lami[:], op=ALU.mult), reads=[R_("lami"), R_("t2")], writes=[R_("t2")])
                k.op("dve", lambda e: e.tensor_tensor(out=t1[:], in0=t1[:], in1=t2[:], op=ALU.add), reads=[R_("t1"), R_("t2")], writes=[R_("t1")])
                k.op("dve", lambda e: e.reciprocal(out=t1[:], in_=t1[:]), reads=[R_("t1")], writes=[R_("t1")])
                k.op("dve", lambda e: e.tensor_tensor(out=cr[:], in0=cr[:], in1=t1[:], op=ALU.mult), reads=[R_("cr"), R_("t1")], writes=[R_("cr")])
                k.op("dve", lambda e: e.tensor_tensor(out=ci[:], in0=ci[:], in1=t1[:], op=ALU.mult), reads=[R_("ci"), R_("t1")], writes=[R_("ci")])
                k.dma("sp", bre[:], self.s5_b_re[jo, d].rearrange("g s q -> s g q"), writes=[R_("bre")])
                k.dma("sp", bim[:], self.s5_b_im[jo, d].rearrange("g s q -> s g q"), writes=[R_("bim")])
                bc = lambda t: t[:].unsqueeze(2).to_broadcast([64, 64, 16])
                k.op("dve", lambda e: e.tensor_tensor(out=Bbr[:], in0=bre[:], in1=bc(cr), op=ALU.mult), reads=[R_("bre"), R_("cr")], writes=[R_("Bbr")])
                k.op("dve", lambda e: e.tensor_tensor(out=Bbi[:], in0=bim[:], in1=bc(cr), op=ALU.mult), reads=[R_("bim"), R_("cr")], writes=[R_("Bbi")])
                k.op("dve", lambda e: e.tensor_tensor(out=bim[:], in0=bim[:], in1=bc(ci), op=ALU.mult), reads=[R_("bim"), R_("ci"), R_("Bbi")], writes=[R_("bim")])
                k.op("dve", lambda e: e.tensor_tensor(out=bre[:], in0=bre[:], in1=bc(ci), op=ALU.mult), reads=[R_("bre"), R_("ci"), R_("Bbr")], writes=[R_("bre")])
                k.op("dve", lambda e: e.tensor_tensor(out=Bbr[:], in0=Bbr[:], in1=bim[:], op=ALU.subtract), reads=[R_("Bbr"), R_("bim")], writes=[R_("Bbr")])
                k.op("dve", lambda e: e.tensor_tensor(out=Bbi[:], in0=Bbi[:], in1=bre[:], op=ALU.add), reads=[R_("Bbi"), R_("bre")], writes=[R_("Bbi")])
                for (src, dst, nm) in ((self.s5_c_re, Ctr, "Ctr"), (self.s5_c_im, Cti, "Cti")):
                    csrc = src[jo, d].rearrange("g p s -> (g p) s")
                    for g8 in range(8):
                        k.dma("sp", nat[:, :], csrc[g8 * 128:(g8 + 1) * 128, :], writes=[R_("nat")])
                        b = self.nps()
                        k.op("pe", lambda e: e.transpose(out=self.ps[b][0:64, 0:128], in_=nat[:, :], identity=self.idt[:, :]), reads=[R_("nat"), "idt"], writes=[("ps", b)])
                        k.op("dve", lambda e: e.tensor_copy(out=dst[:, g8 * 8:(g8 + 1) * 8, :].rearrange("s g p -> s (g p)"), in_=self.ps[b][0:64, 0:128]), reads=[("ps", b)], writes=[R_(nm)])
                k.op("dve", lambda e: e.tensor_copy(out=d8[:, 0, 0, :], in_=TAr[:, 16, :]), reads=TA, writes=[R_("d8")])
                k.op("dve", lambda e: e.tensor_copy(out=d8[:, 0, 1, :], in_=TAr[:, 16, :]), reads=TA, writes=[R_("d8")])
                k.op("dve", lambda e: e.tensor_scalar(out=d8[:, 1, 0, :], in0=TAi[:, 16, :], scalar1=-1.0, scalar2=None, op0=ALU.mult), reads=TA, writes=[R_("d8")])
                k.op("dve", lambda e: e.tensor_copy(out=d8[:, 1, 1, :], in_=TAi[:, 16, :]), reads=TA, writes=[R_("d8")])
                k.dma("sp", self.D8[d], d8[:], reads=[R_("d8")], writes=[("D8", d)])
                for g0 in range(0, 64, GB):
                    gs = slice(g0, g0 + GB)
                    tb = lambda T_, i: T_[:, i, gs].unsqueeze(2).to_broadcast([64, GB, 16])
                    for j in range(8):
                        for (Tidx, Xr, Xi, Ore, Oim, OimN, onm) in ((j, Bbr, Bbi, Ere, Eim, EimN, "E"), (8 + j, Ctr, Cti, Rre, Rim, None, "R")):
                            srcs = TA + [R_("Bbr"), R_("Bbi"), R_("Ctr"), R_("Cti")]
                            k.op("dve", lambda e: e.tensor_tensor(out=tmpA[:, :, j, :], in0=Xr[:, gs, :], in1=tb(TAr, Tidx), op=ALU.mult), reads=srcs, writes=[R_("tmpA")])
                            k.op("pool", lambda e: e.tensor_tensor(out=tmpB[:, :, j, :], in0=Xi[:, gs, :], in1=tb(TAi, Tidx), op=ALU.mult), reads=srcs, writes=[R_("tmpB")])
                            k.op("dve", lambda e: e.tensor_tensor(out=Ore[:, :, j, :], in0=tmpA[:, :, j, :], in1=tmpB[:, :, j, :], op=ALU.subtract), reads=[R_("tmpA"), R_("tmpB")], writes=[R_(onm + "re")])
                            k.op("dve", lambda e: e.tensor_tensor(out=tmpA[:, :, j, :], in0=Xi[:, gs, :], in1=tb(TAr, Tidx), op=ALU.mult), reads=srcs + [R_("tmpA")], writes=[R_("tmpA")])
                            k.op("pool", lambda e: e.tensor_tensor(out=tmpB[:, :, j, :], in0=Xr[:, gs, :], in1=tb(TAi, Tidx), op=ALU.mult), reads=srcs + [R_("tmpB")], writes=[R_("tmpB")])
                            k.op("dve", lambda e: e.tensor_tensor(out=Oim[:, :, j, :], in0=tmpA[:, :, j, :], in1=tmpB[:, :, j, :], op=ALU.add), reads=[R_("tmpA"), R_("tmpB")], writes=[R_(onm + "im")])
                    k.op("dve", lambda e: e.tensor_scalar(out=EimN[:], in0=Eim[:], scalar1=-1.0, scalar2=None, op0=ALU.mult), reads=[R_("Eim")], writes=[R_("EimN")])
                    t8 = lambda T_: T_[:, 16, gs].unsqueeze(2).to_broadcast([64, GB, 128])
                    fl = lambda t: t[:].rearrange("s g j q -> s g (j q)")
                    k.op("dve", lambda e: e.tensor_tensor(out=fl(tmpA), in0=fl(Rre), in1=t8(TAr), op=ALU.mult), reads=TA + [R_("Rre"), R_("tmpA")], writes=[R_("tmpA")])
                    k.op("pool", lambda e: e.tensor_tensor(out=fl(tmpB), in0=fl(Rim), in1=t8(TAi), op=ALU.mult), reads=TA + [R_("Rim"), R_("tmpB")], writes=[R_("tmpB")])
                    k.op("dve", lambda e: e.tensor_tensor(out=fl(Wre), in0=fl(tmpA), in1=fl(tmpB), op=ALU.subtract), reads=[R_("tmpA"), R_("tmpB")], writes=[R_("Wre")])
                    k.op("dve", lambda e: e.tensor_tensor(out=fl(tmpA), in0=fl(Rim), in1=t8(TAr), op=ALU.mult), reads=TA + [R_("Rim"), R_("tmpA")], writes=[R_("tmpA")])
                    k.op("pool", lambda e: e.tensor_tensor(out=fl(tmpB), in0=fl(Rre), in1=t8(TAi), op=ALU.mult), reads=TA + [R_("Rre"), R_("tmpB")], writes=[R_("tmpB")])
                    k.op("dve", lambda e: e.scalar_tensor_tensor(out=fl(WimN), in0=fl(tmpA), scalar=-1.0, in1=fl(tmpB), op0=ALU.mult, op1=ALU.subtract), reads=[R_("tmpA"), R_("tmpB")], writes=[R_("WimN")])
                    k.dma("sp", self.Wo[d, 0, :, gs, :], fl(Wre), reads=[R_("Wre")], writes=[("Wo", d, 0, g0)])
                    k.dma("sp", self.Wo[d, 1, :, gs, :], fl(WimN), reads=[R_("WimN")], writes=[("Wo", d, 1, g0)])
                    for gl in range(GB):
                        g = g0 + gl
                        b = self.nps()
                        k.op("pe", lambda e: e.transpose(out=self.ps[b][:, 0:64], in_=Ere[:, gl, :, :].rearrange("s j q -> s (j q)"), identity=self.idt[0:64, 0:64]), reads=[R_("Ere"), "idt"], writes=[("ps", b)])
                        k.op("pe", lambda e: e.transpose(out=self.ps[b][:, 64:128], in_=Eim[:, gl, :, :].rearrange("s j q -> s (j q)"), identity=self.idt[0:64, 0:64]), reads=[R_("Eim"), "idt"], writes=[("ps", b)])
                        k.op("act", lambda e: e.activation(out=wint[0][:, :, gl, :], in_=self.ps[b][:, 0:128].rearrange("p (r s) -> p r s", r=2), func=AF.Identity), reads=[("ps", b)], writes=[R_("wint")])
                        b2 = self.nps()
                        k.op("pe", lambda e: e.matmul(self.ps[b2][:, 0:128], lhsT=Ere[:, gl, :, :].rearrange("s j q -> s (j q)"), rhs=Rre[:, gl, :, :].rearrange("s j q -> s (j q)"), start=True, stop=False), reads=[R_("Ere"), R_("Rre")], writes=[("ps", b2)])
                        k.op("pe", lambda e: e.matmul(self.ps[b2][:, 0:128], lhsT=EimN[:, gl, :, :].rearrange("s j q -> s (j q)"), rhs=Rim[:, gl, :, :].rearrange("s j q -> s (j q)"), start=False, stop=True), reads=[R_("EimN"), R_("Rim")], writes=[("ps", b2)])
                        if d == 0:
                            k.op("dve", lambda e: e.tensor_tensor(out=mt[:], in0=self.ps[b2][:, 0:128], in1=maskf[:], op=ALU.mult), reads=[("ps", b2), "s5maskf"], writes=[R_("mt")])
                            k.op("dve", lambda e: e.scalar_tensor_tensor(out=Macc[:, g, :], in0=self.idt[:, :], scalar=DT[:, g:g + 1], in1=mt[:], op0=ALU.mult, op1=ALU.add), reads=[R_("mt"), "idt", "s5DT"], writes=[("Macc", g)])
                        else:
                            k.op("dve", lambda e: e.tensor_tensor(out=mt[:], in0=self.ps[b2][:, 0:128], in1=maskr[:], op=ALU.mult), reads=[("ps", b2), "s5maskr"], writes=[R_("mt")])
                            k.op("pool", lambda e: e.tensor_tensor(out=Macc[:, g, :], in0=Macc[:, g, :], in1=mt[:], op=ALU.add), reads=[R_("mt"), ("Macc", g)], writes=[("Macc", g)])
                    for ri in range(2):
                        k.dma("sp", self.WinT[d, ri, :, gs, :], wint[0][:, ri, :, :], reads=[R_("wint")], writes=[("WinT", d, ri, g0)])
            k.dma("sp", self.Mm[:, :, :], Macc[:], reads=[("Macc", g) for g in range(64)], writes=["Mm"])
            k.barrier()

    def s5_regs(self):
        r = ["Mm"]
        for d in range(2):
            r.append(("D8", d))
            for ri in range(2):
                for g0 in range(0, 64, 16):
                    r += [("Wo", d, ri, g0), ("WinT", d, ri, g0)]
        return r

    def s5_scan(self, XS, xreg, D8t, cols, tmp, treg):
        k = self.k
        for (c, cp) in cols:
            prev = XS[:, :, :, cp]
            k.op("pool", lambda e: e.tensor_tensor(out=tmp[:, 0, 0, :], in0=XS[:, 1, :, cp], in1=D8t[:, 1, 0, :], op=ALU.mult), reads=[xreg, "s5D8t"], writes=[(treg, 0)])
            k.op("pool", lambda e: e.tensor_tensor(out=tmp[:, 0, 1, :], in0=XS[:, 0, :, cp], in1=D8t[:, 1, 1, :], op=ALU.mult), reads=[xreg, "s5D8t"], writes=[(treg, 0)])
            k.op("dve", lambda e: e.tensor_tensor(out=tmp[:, 1, :, :], in0=prev, in1=D8t[:, 0, :, :], op=ALU.mult), reads=[xreg, "s5D8t"], writes=[(treg, 1)])
            k.op("dve", lambda e: e.tensor_tensor(out=XS[:, :, :, c], in0=XS[:, :, :, c], in1=tmp[:, 1, :, :], op=ALU.add), reads=[xreg, (treg, 1)], writes=[xreg])
            k.op("dve", lambda e: e.tensor_tensor(out=XS[:, :, :, c], in0=XS[:, :, :, c], in1=tmp[:, 0, :, :], op=ALU.add), reads=[xreg, (treg, 0)], writes=[xreg])

    def s5_pass1(self, layer, jo):
        k = self.k
        with ExitStack() as es:
            sb = lambda n, sh, dt=F32: self.sb("s5a_" + n, sh, dt, es=es)
            wIn = sb("wIn", [128, KC, D], BF16)
            self.load_w_bf16(wIn, self.od_w_in[jo], ["s5wIn"])
            Wt = self.load_mod(es, 1, "s5_W")
            SHt = self.load_mod(es, 0, "s5_SH")
            ht = [sb("h%d" % i, [128, D]) for i in range(2)]
            at = [sb("a%d" % i, [128, D]) for i in range(2)]
            self.nsc = [(sb("ss%d" % i, [128, 4]), sb("jk%d" % i, [128, D], BF16)) for i in range(2)]
            aT = sb("aT", [128, KC, 512], BF16)
            UU = sb("UU", [64, 64, 8, 16])
            Ublk = sb("Ublk", [128, 64, 64])
            XSf = sb("XSf", [64, 2, 64, 65])
            XRb = [sb("XRb%d" % i, [64, 2, 16, 64]) for i in range(2)]
            wt_ = [sb("wt%d" % i, [128, 2, 2, 16, 64]) for i in range(2)]
            D8t = sb("D8t", [64, 2, 2, 64])
            tmp = sb("tmp", [64, 2, 2, 64])
            k.dma("sp", D8t[:], self.D8[0], reads=self.s5_regs(), writes=["s5D8t"])
            k.op("pool", lambda e: e.memset(XSf[:, :, :, 0], 0.0), writes=["XSf"])
            bi = 0
            for sti, (t0, n, s) in enumerate(self.stiles()):
                nch = n // 8
                c0 = t0 // 8
                for blk in range(n // 128):
                    i = bi % 2
                    bi += 1
                    r0 = t0 + blk * 128
                    k.dma("sp", ht[i][:], self.h_all[r0:r0 + 128, :], reads=self.hreg(r0, 128), writes=[("s5h", i)])
                    self.tm_norm(ht[i], ("s5h", i), at[i], ("s5a", i), s, Wt, SHt, 128, "s5")
                    self.transpose_block(at[i], ("s5a", i), 128, aT, "s5aT", blk * 128)
                for j in range(8):
                    for half in range(2):
                        b = self.nps()
                        for kc in range(KC):
                            k.op("pe", lambda e: e.matmul(self.ps[b][0:nch, :], lhsT=aT[:, kc, j:n:8], rhs=wIn[:, kc, half * 512:(half + 1) * 512], start=(kc == 0), stop=(kc == KC - 1)),
                                 reads=["s5aT", "s5wIn"], writes=[("ps", b)])
                        k.op("act" if half else "dve", (lambda e: e.activation(out=UU[0:nch, half * 32:(half + 1) * 32, j, :], in_=self.ps[b][0:nch, :].rearrange("c (g q) -> c g q", q=16), func=AF.Identity)) if half else
                             (lambda e: e.tensor_copy(out=UU[0:nch, half * 32:(half + 1) * 32, j, :], in_=self.ps[b][0:nch, :].rearrange("c (g q) -> c g q", q=16))),
                             reads=[("ps", b)], writes=["s5UU"])
                for g0 in range(0, 64, 4):
                    b = self.nps()
                    for gl in range(4):
                        k.op("pe", lambda e: e.transpose(out=self.ps[b][:, gl * 64:gl * 64 + nch], in_=UU[0:nch, g0 + gl, :, :].rearrange("c j q -> c (j q)"), identity=self.idt[0:nch, 0:nch]),
                             reads=["s5UU", "idt"], writes=[("ps", b)])
                    k.op("act" if (g0 // 4) % 2 else "dve", (lambda e: e.activation(out=Ublk[:, g0:g0 + 4, 0:nch], in_=self.ps[b][:, 0:256].rearrange("p (g c) -> p g c", g=4)[:, :, 0:nch], func=AF.Identity)) if (g0 // 4) % 2 else
                         (lambda e: e.tensor_copy(out=Ublk[:, g0:g0 + 4, 0:nch], in_=self.ps[b][:, 0:256].rearrange("p (g c) -> p g c", g=4)[:, :, 0:nch])),
                         reads=[("ps", b)], writes=["s5Ublk"])
                k.dma("sp", self.Ugc[:, :, c0:c0 + nch], Ublk[:, :, 0:nch], reads=["s5Ublk"], writes=[("Ugc", sti)])
                for g0 in range(0, 64, 16):
                    wi = (g0 // 16) % 2
                    for d in range(2):
                        for ri in range(2):
                            k.dma("sp", wt_[wi][:, d, ri, :, :], self.WinT[d, ri, :, g0:g0 + 16, :], reads=self.s5_regs(), writes=[("s5wt", wi)])
                    for d in range(2):
                        for ri in range(2):
                            for g4 in range(0, 16, 4):
                                b = self.nps()
                                for gl in range(4):
                                    g = g0 + g4 + gl
                                    k.op("pe", lambda e: e.matmul(self.ps[b][0:64, gl * 64:gl * 64 + nch], lhsT=wt_[wi][:, d, ri, g4 + gl, :], rhs=Ublk[:, g, 0:nch], start=True, stop=True),
                                         reads=[("s5wt", wi), "s5Ublk"], writes=[("ps", b)])
                                src = self.ps[b][0:64, 0:256].rearrange("p (g c) -> p g c", g=4)[:, :, 0:nch]
                                if d == 0:
                                    k.op("dve", lambda e: e.tensor_copy(out=XSf[:, ri, g0 + g4:g0 + g4 + 4, 1:1 + nch], in_=src), reads=[("ps", b)], writes=["XSf"])
                                else:
                                    k.op("act", lambda e: e.activation(out=XRb[wi][:, ri, g4:g4 + 4, 0:nch], in_=src, func=AF.Identity), reads=[("ps", b)], writes=[("XRb", wi)])
                    k.dma("sp", self.XR[:, :, g0:g0 + 16, c0:c0 + nch], XRb[wi][:, :, :, 0:nch], reads=[("XRb", wi)], writes=[("XR", sti, g0)])
                self.s5_scan(XSf, "XSf", D8t, [(c + 1, c) for c in range(nch)], tmp, "s5tmp")
                k.dma("sp", self.SF[:, :, :, c0:c0 + nch], XSf[:, :, :, 0:nch], reads=["XSf"], writes=[("SF", sti)])
                k.op("dve", lambda e: e.tensor_copy(out=XSf[:, :, :, 0], in_=XSf[:, :, :, nch]), reads=["XSf"], writes=["XSf"])
            k.barrier()

    def s5_pass2(self, layer, jo, need_ctx):
        k = self.k
        tiles = self.stiles()
        order = [ti for ti, t in enumerate(tiles) if t[2] == 1][::-1] + [ti for ti, t in enumerate(tiles) if t[2] == 0][::-1]
        with ExitStack() as es:
            sb = lambda n, sh, dt=F32: self.sb("s5b_" + n, sh, dt, es=es)
            wo = sb("wo", [128, KC, 2 * D], BF16)
            self.load_w_bf16(wo, self.od_w_out[jo], ["s5wo"])
            G1 = self.load_mod(es, 2, "s5_G1")
            XSr = sb("XSr", [64, 2, 64, 65])
            D8t = sb("D8t", [64, 2, 2, 64])
            tmp = sb("tmp", [64, 2, 2, 64])
            SFb = [sb("SFb%d" % i, [64, 2, 8, 64]) for i in range(2)]
            Ub = [sb("Ub%d" % i, [128, 8, 64]) for i in range(2)]
            Mt = [sb("Mt%d" % i, [128, 8, 128]) for i in range(2)]
            Wt_ = [sb("Wt%d" % i, [64, 2, 2, 8, 128]) for i in range(2)]
            Yblk = sb("Yblk", [128, 64, 64])
            YT = sb("YT", [64, 8, D])
            catS = sb("catS", [128, KC, 512], BF16)
            ht = [sb("h%d" % i, [64, D]) for i in range(2)]
            tt = [sb("t%d" % i, [64, D]) for i in range(2)]
            sg = [sb("sg%d" % i, [64, 512]) for i in range(2)]
            k.dma("sp", D8t[:], self.D8[1], reads=self.s5_regs(), writes=["s5D8t"])
            first = True
            bi = 0
            for oi, sti in enumerate(order):
                t0, n, s = tiles[sti]
                nch = n // 8
                c0 = t0 // 8
                k.dma("sp", XSr[:, :, :, 0:nch], self.XR[:, :, :, c0:c0 + nch], reads=[("XR", sti, g0) for g0 in range(0, 64, 16)], writes=["XSr"])
                if first:
                    k.op("pool", lambda e: e.memset(XSr[:, :, :, nch], 0.0), writes=["XSr"])
                    first = False
                else:
                    k.op("dve", lambda e: e.tensor_copy(out=XSr[:, :, :, nch], in_=tmp2[:, :, :]), reads=["s5carry"], writes=["XSr"])
                self.s5_scan(XSr, "XSr", D8t, [(c, c + 1) for c in range(nch - 1, -1, -1)], tmp, "s5tmp")
                if oi == 0:
                    tmp2 = sb("carry", [64, 2, 64])
                k.op("dve", lambda e: e.tensor_copy(out=tmp2[:, :, :], in_=XSr[:, :, :, 0]), reads=["XSr"], writes=["s5carry"])
                if s == 1 and not need_ctx:
                    continue
                for g0 in range(0, 64, 8):
                    wi = (g0 // 8) % 2
                    k.dma("sp", SFb[wi][:, :, :, 0:nch], self.SF[:, :, g0:g0 + 8, c0:c0 + nch], reads=[("SF", sti)], writes=[("s5SFb", wi)])
                    k.dma("sp", Ub[wi][:, :, 0:nch], self.Ugc[:, g0:g0 + 8, c0:c0 + nch], reads=[("Ugc", sti)], writes=[("s5Ub", wi)])
                    k.dma("sp", Mt[wi][:], self.Mm[:, g0:g0 + 8, :], reads=self.s5_regs(), writes=[("s5Mt", wi)])
                    for d in range(2):
                        for ri in range(2):
                            k.dma("sp", Wt_[wi][:, d, ri, :, :], self.Wo[d, ri, :, g0:g0 + 8, :], reads=self.s5_regs(), writes=[("s5Wt", wi)])
                    for g4 in range(0, 8, 4):
                        b = self.nps()
                        for gl in range(4):
                            g = g0 + g4 + gl
                            o = self.ps[b][:, gl * 64:gl * 64 + nch]
                            k.op("pe", lambda e: e.matmul(o, lhsT=Mt[wi][:, g4 + gl, :], rhs=Ub[wi][:, g4 + gl, 0:nch], start=True, stop=False), reads=[("s5Mt", wi), ("s5Ub", wi)], writes=[("ps", b)])
                            for ri in range(2):
                                k.op("pe", lambda e: e.matmul(o, lhsT=Wt_[wi][:, 0, ri, g4 + gl, :], rhs=SFb[wi][:, ri, g4 + gl, 0:nch], start=False, stop=False), reads=[("s5Wt", wi), ("s5SFb", wi)], writes=[("ps", b)])
                            for ri in range(2):
                                k.op("pe", lambda e: e.matmul(o, lhsT=Wt_[wi][:, 1, ri, g4 + gl, :], rhs=XSr[:, ri, g, 1:1 + nch], start=False, stop=(ri == 1)), reads=[("s5Wt", wi), "XSr"], writes=[("ps", b)])
                        k.op("act", lambda e: e.activation(out=Yblk[:, g0 + g4:g0 + g4 + 4, 0:nch], in_=self.ps[b][:, 0:256].rearrange("p (g c) -> p g c", g=4)[:, :, 0:nch], func=AF.Identity), reads=[("ps", b)], writes=["s5Yblk"])
                for g0 in range(0, 64, 4):
                    b = self.nps()
                    for gl in range(4):
                        k.op("pe", lambda e: e.transpose(out=self.ps[b][0:nch, gl * 128:(gl + 1) * 128], in_=Yblk[:, g0 + gl, 0:nch], identity=self.idt[:, :]), reads=["s5Yblk", "idt"], writes=[("ps", b)])
                    k.op("act", lambda e: e.activation(out=YT[0:nch, :, g0 * 16:(g0 + 4) * 16].rearrange("c t (g p) -> c g t p", g=4), in_=self.ps[b][0:nch, :].rearrange("c (g t p) -> c g t p", g=4, t=8), func=AF.Gelu),
                         reads=[("ps", b)], writes=["s5YT"])
                for tau in range(8):
                    self.transpose_block(YT[:, tau, :], "s5YT", nch, catS, "s5cat", tau * nch)
                    i = bi % 2
                    bi += 1
                    rows = self.h_all[t0 + tau:t0 + n:8, :]
                    k.dma("sp", ht[i][0:nch, :], rows, reads=self.hreg(t0, n), writes=[("s5h2", i)])
                    for half in range(2):
                        b = self.nps()
                        b2 = self.nps()
                        for kc in range(KC):
                            k.op("pe", lambda e: e.matmul(self.ps[b][0:nch, :], lhsT=catS[:, kc, tau * nch:(tau + 1) * nch], rhs=wo[:, kc, half * 512:(half + 1) * 512], start=(kc == 0), stop=(kc == KC - 1)),
                                 reads=["s5cat", "s5wo"], writes=[("ps", b)])
                        for kc in range(KC):
                            k.op("pe", lambda e: e.matmul(self.ps[b2][0:nch, :], lhsT=catS[:, kc, tau * nch:(tau + 1) * nch], rhs=wo[:, kc, D + half * 512:D + (half + 1) * 512], start=(kc == 0), stop=(kc == KC - 1)),
                                 reads=["s5cat", "s5wo"], writes=[("ps", b2)])
                        k.op("act", lambda e: e.activation(out=sg[i][0:nch, :], in_=self.ps[b2][0:nch, :], func=AF.Sigmoid), reads=[("ps", b2)], writes=[("s5sg", i)])
                        k.op("dve", lambda e: e.tensor_tensor(out=sg[i][0:nch, :], in0=self.ps[b][0:nch, :], in1=sg[i][0:nch, :], op=ALU.mult), reads=[("ps", b), ("s5sg", i)], writes=[("s5sg", i)])
                        k.op("dve", lambda e: e.tensor_tensor(out=tt[i][0:nch, half * 512:(half + 1) * 512], in0=sg[i][0:nch, :], in1=G1[s][0:nch, half * 512:(half + 1) * 512], op=ALU.mult),
                             reads=[("s5sg", i), ("s5_G1", s)], writes=[("s5t", i)])
                    k.op("pool", lambda e: e.tensor_tensor(out=tt[i][0:nch, :], in0=tt[i][0:nch, :], in1=ht[i][0:nch, :], op=ALU.add), reads=[("s5t", i), ("s5h2", i)], writes=[("s5t", i)])
                    k.dma("sp", rows, tt[i][0:nch, :], reads=[("s5t", i)], writes=self.hreg(t0, n))
            k.barrier()

    def moe_layer(self, layer, need_ctx):
        k = self.k
        M, T, TT = self.M, self.T, self.TT
        capL, capC = self.capL, self.capC
        nLb = capL // 128
        with ExitStack() as es:
            G2 = self.load_mod(es, 5, "mz_g2")
            nLb_ = capL // 128
            idxT = self.sb("mz_idxT", [128, nLb_ + 1, NE], U32, es=es)
            gateT = self.sb("mz_gateT", [128, nLb_ + 1, NE], es=es)
            tk_es = ExitStack()
            AFF = self.sb("mz_aff", [NE, TT], es=tk_es)
            with ExitStack() as es2:
                Wt = self.load_mod(es2, 4, "mz_W")
                SHt = self.load_mod(es2, 3, "mz_SH")
                rw = self.sb("mz_rw", [128, KC, NE], es=es2)
                k.dma("sp", rw[:], self.router_w[layer].rearrange("(kc p) e -> p kc e", p=128), writes=["mz_rw"])
                ht = [self.sb("mz_h%d" % i, [128, D], es=es2) for i in range(2)]
                at = [self.sb("mz_a%d" % i, [128, D], es=es2) for i in range(2)]
                self.nsc = [(self.sb("mz_ss%d" % i, [128, 4], es=es2), self.sb("mz_jk%d" % i, [128, D], BF16, es=es2)) for i in range(2)]
                h2T = [self.sb("mz_h2T%d" % i, [128, KC, 512], es=es2) for i in range(2)]
                ex = [self.sb("mz_ex%d" % i, [NE, 512], es=es2) for i in range(2)]
                rs = [self.sb("mz_rs%d" % i, [NE, 512], es=es2) for i in range(2)]
                bi = 0
                for sti, (t0, n, s) in enumerate(self.stiles()):
                    if s == 1 and not need_ctx:
                        continue
                    ai = sti % 2
                    for blk in range(n // 128):
                        i = bi % 2
                        bi += 1
                        r0 = t0 + blk * 128
                        k.dma("sp", ht[i][:], self.h_all[r0:r0 + 128, :], reads=self.hreg(r0, 128), writes=[("mz_h", i)])
                        self.tm_norm(ht[i], ("mz_h", i), at[i], ("mz_a", i), s, Wt, SHt, 128, "mz")
                        k.dma("sp", self.h2rows[r0:r0 + 128, :], at[i][:, :], reads=[("mz_a", i)], writes=[("h2rows", r0 // 128)])
                        self.transpose_block(at[i], ("mz_a", i), 128, h2T[ai], ("mz_h2T", ai), blk * 128, evac="dve" if blk % 2 else "act")
                    b = self.nps()
                    for kc in range(KC):
                        k.op("pe", lambda e: e.matmul(self.ps[b][0:NE, 0:n], lhsT=rw[:, kc, :], rhs=h2T[ai][:, kc, 0:n], start=(kc == 0), stop=(kc == KC - 1)),
                             reads=["mz_rw", ("mz_h2T", ai)], writes=[("ps", b)])
                    k.op("act", lambda e: e.activation(out=ex[ai][:, 0:n], in_=self.ps[b][0:NE, 0:n], func=AF.Exp), reads=[("ps", b)], writes=[("mz_ex", ai)])
                    b2 = self.nps()
                    k.op("pe", lambda e: e.matmul(self.ps[b2][0:NE, 0:n], lhsT=self.ones[0:NE, 0:NE], rhs=ex[ai][:, 0:n], start=True, stop=True), reads=["ones", ("mz_ex", ai)], writes=[("ps", b2)])
                    k.op("dve", lambda e: e.reciprocal(out=rs[ai][:, 0:n], in_=self.ps[b2][0:NE, 0:n]), reads=[("ps", b2)], writes=[("mz_rs", ai)])
                    k.op("dve", lambda e: e.tensor_tensor(out=AFF[:, t0:t0 + n], in0=ex[ai][:, 0:n], in1=rs[ai][:, 0:n], op=ALU.mult), reads=[("mz_ex", ai), ("mz_rs", ai)], writes=["mz_aff"])
                k.barrier()
            ncap = capL + (capC if need_ctx else 0)
            nsb = nLb + (1 if need_ctx else 0)
            mx = self.sb("mz_mx", [NE, capL + capC], es=tk_es)
            mi = self.sb("mz_mi", [NE, capL + capC], U32, es=tk_es)
            mf = self.sb("mz_mf", [NE, capL + capC], es=tk_es)
            segs = [(M, T, 0, capL)] + ([(0, M, capL, capC)] if need_ctx else [])
            for (c0, cn, s0, cap) in segs:
                for r in range(cap // 8):
                    sl = slice(s0 + r * 8, s0 + r * 8 + 8)
                    k.op("dve", lambda e: e.max(out=mx[:, sl], in_=AFF[:, c0:c0 + cn]), reads=["mz_aff"], writes=["mz_mx"])
                    k.op("dve", lambda e: e.max_index(out=mi[:, sl], in_max=mx[:, sl], in_values=AFF[:, c0:c0 + cn]), reads=["mz_aff", "mz_mx"], writes=["mz_mi"])
                    k.op("dve", lambda e: e.match_replace(out=AFF[:, c0:c0 + cn], in_to_replace=mx[:, sl], in_values=AFF[:, c0:c0 + cn], imm_value=-1.0), reads=["mz_aff", "mz_mx", "mz_mi"], writes=["mz_aff"])
                k.op("dve", lambda e: e.tensor_copy(out=mf[:, s0:s0 + cap], in_=mi[:, s0:s0 + cap]), reads=["mz_mi"], writes=["mz_mf"])
                k.op("dve", lambda e: e.tensor_scalar(out=mf[:, s0:s0 + cap], in0=mf[:, s0:s0 + cap], scalar1=float(c0), scalar2=None, op0=ALU.add), reads=["mz_mf"], writes=["mz_mf"])
            for sbk in range(nsb):
                s0 = sbk * 128
                nn = 128 if sbk < nLb else capC
                b = self.nps()
                k.op("pe", lambda e: e.transpose(out=self.ps[b][0:nn, 0:NE], in_=mf[:, s0:s0 + nn], identity=self.idt[0:NE, 0:NE]), reads=["mz_mf", "idt"], writes=[("ps", b)])
                k.op("pe", lambda e: e.transpose(out=self.ps[b][0:nn, NE:2 * NE], in_=mx[:, s0:s0 + nn], identity=self.idt[0:NE, 0:NE]), reads=["mz_mx", "idt"], writes=[("ps", b)])
                k.op("dve", lambda e: e.tensor_copy(out=idxT[0:nn, sbk, :], in_=self.ps[b][0:nn, 0:NE]), reads=[("ps", b)], writes=["mz_idxT"])
                k.op("dve", lambda e: e.tensor_copy(out=gateT[0:nn, sbk, :], in_=self.ps[b][0:nn, NE:2 * NE]), reads=[("ps", b)], writes=["mz_gateT"])
            k.barrier()
            tk_es.close()
            wg = self.sb("mz_wg", [128, KC, DFF], BF16, es=es)
            wu = self.sb("mz_wu", [128, KC, DFF], BF16, es=es)
            wd = self.sb("mz_wd", [128, NFC, D], BF16, es=es)
            xg = [self.sb("mz_xg%d" % i, [128, D], es=es) for i in range(2)]
            xeT = [self.sb("mz_xeT%d" % i, [128, KC, 512], BF16, es=es) for i in range(1)]
            actT = self.sb("mz_act", [128, NFC, 512], BF16, es=es)
            sl_ = [self.sb("mz_sl%d" % i, [128, 512], es=es) for i in range(2)]
            ye = [self.sb("mz_ye%d" % i, [128, D], es=es) for i in range(2)]
            allh = [("h", b_) for b_ in range(self.NB)]
            allh2 = [("h2rows", b_) for b_ in range(self.NB)]
            blocks = [(sbk, 128, 0) for sbk in range(nLb)] + ([(nLb, capC, 1)] if need_ctx else [])
            subs = []
            if need_ctx:
                subs.append([blocks[-1]])
            for i0 in range(0, nLb, 4):
                subs.append(blocks[i0:min(i0 + 4, nLb)])
            gcnt = 0
            ycnt = 0
            scnt = 0
            for ex_ in range(NE):
                for kc in range(KC):
                    k.dma("pool", wg[:, kc, :], self.w_gate[layer, ex_, kc * 128:(kc + 1) * 128, :], writes=[("mz_wg", kc)], max_dma_last_dim=4096)
                    k.dma("pool", wu[:, kc, :], self.w_up[layer, ex_, kc * 128:(kc + 1) * 128, :], writes=[("mz_wu", kc)], max_dma_last_dim=4096)
                for fc in range(NFC):
                    fsz = min(128, DFF - fc * 128)
                    k.dma("pool", wd[0:fsz, fc, :], self.w_down[layer, ex_, fc * 128:fc * 128 + fsz, :], writes=[("mz_wd", fc)], max_dma_last_dim=4096)
                for sub in subs:
                    xi = 0
                    scnt += 1
                    ntok = sum(nn for (_, nn, _) in sub)
                    col = 0
                    cols = []
                    for (sbk, nn, s) in sub:
                        gi = gcnt % 2
                        gcnt += 1
                        k.dma("pool", reads=["mz_idxT"] + allh2, writes=[("mz_xg", gi)],
                              fn=lambda e: e.indirect_dma_start(out=xg[gi][0:nn, :], out_offset=None, in_=self.h2rows[:, :],
                                                                in_offset=bass.IndirectOffsetOnAxis(ap=idxT[0:nn, sbk, ex_:ex_ + 1], axis=0)))
                        self.transpose_block(xg[gi], ("mz_xg", gi), nn, xeT[xi], ("mz_xeT", xi), col, evac="act")
                        cols.append(col)
                        col += nn
                    for fc in range(NFC):
                        fsz = min(128, DFF - fc * 128)
                        bg = self.nps()
                        for kc in range(KC):
                            k.op("pe", lambda e: e.matmul(self.ps[bg][0:fsz, 0:ntok], lhsT=wg[:, kc, fc * 128:fc * 128 + fsz], rhs=xeT[xi][:, kc, 0:ntok], start=(kc == 0), stop=(kc == KC - 1)),
                                 reads=[("mz_wg", kc), ("mz_xeT", xi)], writes=[("ps", bg)])
                        bu = self.nps()
                        for kc in range(KC):
                            k.op("pe", lambda e: e.matmul(self.ps[bu][0:fsz, 0:ntok], lhsT=wu[:, kc, fc * 128:fc * 128 + fsz], rhs=xeT[xi][:, kc, 0:ntok], start=(kc == 0), stop=(kc == KC - 1)),
                                 reads=[("mz_wu", kc), ("mz_xeT", xi)], writes=[("ps", bu)])
                        si = fc % 2
                        k.op("act", lambda e: e.activation(out=sl_[si][0:fsz, 0:ntok], in_=self.ps[bg][0:fsz, 0:ntok], func=AF.Silu), reads=[("ps", bg)], writes=[("mz_sl", si)])
                        k.op("dve", lambda e: e.tensor_tensor(out=actT[0:fsz, fc, 0:ntok], in0=self.ps[bu][0:fsz, 0:ntok], in1=sl_[si][0:fsz, 0:ntok], op=ALU.mult),
                             reads=[("ps", bu), ("mz_sl", si)], writes=["mz_act"])
                    for bi_, (sbk, nn, s) in enumerate(sub):
                        yi = ycnt % 2
                        ycnt += 1
                        c0 = cols[bi_]
                        for half in range(2):
                            b = self.nps()
                            for fc in range(NFC):
                                fsz = min(128, DFF - fc * 128)
                                k.op("pe", lambda e: e.matmul(self.ps[b][0:nn, :], lhsT=actT[0:fsz, fc, c0:c0 + nn], rhs=wd[0:fsz, fc, half * 512:(half + 1) * 512], start=(fc == 0), stop=(fc == NFC - 1)),
                                     reads=["mz_act", ("mz_wd", fc)], writes=[("ps", b)])
                            k.op("dve", lambda e: e.scalar_tensor_tensor(out=ye[yi][0:nn, half * 512:(half + 1) * 512], in0=self.ps[b][0:nn, :], scalar=gateT[0:nn, sbk, ex_:ex_ + 1],
                                                                         in1=G2[s][0:nn, half * 512:(half + 1) * 512], op0=ALU.mult, op1=ALU.mult),
                                 reads=[("ps", b), "mz_gateT", ("mz_g2", s)], writes=[("mz_ye", yi)])
                        k.dma("pool", reads=["mz_idxT", ("mz_ye", yi)], writes=allh,
                              fn=lambda e: e.indirect_dma_start(out=self.h_all[:, :], out_offset=bass.IndirectOffsetOnAxis(ap=idxT[0:nn, sbk, ex_:ex_ + 1], axis=0),
                                                                in_=ye[yi][0:nn, :], in_offset=None, compute_op=ALU.add))
            k.barrier()


def host_consts(T, M):
    TT = T + M
    ident = np.eye(128, dtype=np.float32)
    pos96 = np.zeros((96, TT), np.float32)
    t = np.arange(T)
    row = (t // 64).astype(np.float32)
    colp = (t % 64).astype(np.float32)
    inv96 = np.zeros((96, 1), np.float32)
    inv = (10000.0 ** (-np.arange(8, dtype=np.float32) / 8)).astype(np.float32)
    for p in range(64, 96):
        pair = (p - 64) % 16
        pos96[p, M:] = row if pair < 8 else colp
        inv96[p, 0] = inv[pair % 8]
    permT = np.zeros((96, 96), np.float32)
    for m in range(64, 80):
        permT[m + 16, m] = -1.0
    for m in range(80, 96):
        permT[m - 16, m] = 1.0
    selpe = np.zeros((32, 96), np.float32)
    for i in range(32):
        selpe[i, 64 + i] = 1.0
    band = np.zeros((2, 128, 32), np.float32)
    for r in range(128):
        jr = (r % 32) // 16
        for c in range(32):
            jc = c // 16
            band[0, r, c] = 1.0 if jc >= jr else 0.0
            band[1, r, c] = 1.0 if jr >= jc else 0.0
    return dict(ident=ident, pos96=pos96, inv96=inv96, permT=permT, selpe=selpe, band=band)


WEIGHT_KEYS = ["ada_w", "ada_b", "norm1_g", "norm2_g", "router_w", "exp_w_gate", "exp_w_up", "exp_w_down", "ev_w_in", "pool_w",
               "pool_scale", "q_a_norm_g", "w_uq", "kv_a_norm_g", "w_ukv", "q_norm_g", "k_norm_g", "ev_w_out", "od_w_in",
               "s5_lam_re", "s5_lam_im", "s5_log_dt", "s5_b_re", "s5_b_im", "s5_c_re", "s5_c_im", "s5_d", "od_w_out"]


def run(inputs, depth=4, dbg=(), ncores=None):
    x = np.asarray(inputs["x"], np.float32)
    ctx = np.asarray(inputs["ctx"], np.float32)
    B, T, _ = x.shape
    M = ctx.shape[1]
    prog = Prog(T, M, depth, dbg)
    nc = prog.build()
    consts = host_consts(T, M)
    EVK = ["ev_w_in", "pool_w", "pool_scale", "q_a_norm_g", "w_uq", "kv_a_norm_g", "w_ukv", "q_norm_g", "k_norm_g", "ev_w_out"]
    shared = {}
    for kk in WEIGHT_KEYS:
        a = np.asarray(inputs[kk], np.float32)
        nlead = (depth + 1) // 2 if kk in EVK else (max(1, depth // 2) if (kk.startswith("s5_") or kk.startswith("od_")) else depth)
        shared[kk] = np.ascontiguousarray(a[:nlead])
    shared.update(consts)
    c = np.asarray(inputs["c"], np.float32)
    cc = np.asarray(inputs["c_ctx"], np.float32)
    in_maps = []
    for b in range(B):
        m = dict(shared)
        m["x"] = np.ascontiguousarray(x[b])
        m["ctx"] = np.ascontiguousarray(ctx[b])
        m["cfm"] = np.ascontiguousarray(np.concatenate([c[b].reshape(KC, 128).T, cc.reshape(KC, 128).T], axis=1))
        in_maps.append(m)
    ncore = len(in_maps) if ncores is None else ncores
    res = run_bass_kernel_spmd(nc, in_maps[:ncore], core_ids=list(range(ncore)))
    out = np.stack([np.asarray(r["y"]) for r in res.results], axis=0).astype(np.float32)
    return out, res, prog


def kernel(**inputs):
    out, _, _ = run(inputs, depth=4)
    return out
```

```python
import math
import numpy as np
from contextlib import ExitStack
import concourse.bass as bass
import concourse.mybir as mybir
from concourse.bass_utils import run_bass_kernel_spmd

F32 = mybir.dt.float32
BF16 = mybir.dt.bfloat16
I32 = mybir.dt.int32
U32 = mybir.dt.uint32
AF = mybir.ActivationFunctionType
ALU = mybir.AluOpType

D = 1024
KC = 8
NE = 16
DFF = 2752
NFC = 22
EPS = 1e-6
TWO_PI = 2.0 * math.pi


class K:
    def __init__(self, nc, es, n_dma_sems=8):
        self.nc = nc
        self.es = es
        self.eng = {"pe": nc.tensor, "act": nc.scalar, "dve": nc.vector, "pool": nc.gpsimd, "sp": nc.sync}
        self.csem, self.ccnt, self.cgen = {}, {}, {}
        for e in ("pe", "act", "dve", "pool"):
            self.cgen[e] = 0
            self.csem[e] = es.enter_context(nc.semaphore("c_%s_0" % e))
            self.ccnt[e] = 0
        self.dsem, self.dcnt, self.dnext, self.dgen = {}, {}, {}, {}
        for q in ("sp", "act", "pool"):
            self.dsem[q] = [es.enter_context(nc.semaphore("d_%s%d_0" % (q, i))) for i in range(n_dma_sems)]
            self.dcnt[q] = [0] * n_dma_sems
            self.dgen[q] = [0] * n_dma_sems
            self.dnext[q] = 0
        self.waited = {e: {} for e in self.eng}
        self.lastw = {}
        self.readers = {}
        self.ninstr = 0

    def _wait(self, e, tok):
        if tok is None:
            return
        sem, val, key = tok
        if self.waited[e].get(key, 0) >= val:
            return
        self.eng[e].wait_ge(sem, val)
        self.waited[e][key] = val

    def _deps(self, e, reads, writes):
        for r in reads:
            self._wait(e, self.lastw.get(r))
        for w in writes:
            self._wait(e, self.lastw.get(w))
            for t in self.readers.get(w, ()):
                self._wait(e, t)

    def _record(self, tok, reads, writes):
        for r in reads:
            lst = self.readers.setdefault(r, [])
            lst.append(tok)
            if len(lst) > 24:
                d = {}
                for t in lst:
                    if t[2] not in d or d[t[2]][1] < t[1]:
                        d[t[2]] = t
                self.readers[r] = list(d.values())
        for w in writes:
            self.lastw[w] = tok
            self.readers[w] = []

    def op(self, e, fn, reads=(), writes=()):
        self._deps(e, reads, writes)
        if self.ccnt[e] >= 30000:
            self.cgen[e] += 1
            self.csem[e] = self.es.enter_context(self.nc.semaphore("c_%s_%d" % (e, self.cgen[e])))
            self.ccnt[e] = 0
        ins = fn(self.eng[e])
        self.ccnt[e] += 1
        ins.then_inc(self.csem[e], 1)
        tok = (self.csem[e], self.ccnt[e], "c_%s_%d" % (e, self.cgen[e]))
        self._record(tok, reads, writes)
        self.ninstr += 1
        return tok

    def dma(self, q, out=None, in_=None, reads=(), writes=(), fn=None, **kw):
        self._deps(q, reads, writes)
        i = self.dnext[q]
        self.dnext[q] = (i + 1) % len(self.dsem[q])
        if self.dcnt[q][i] >= 30000:
            self._wait(q, (self.dsem[q][i], self.dcnt[q][i], "d_%s%d_%d" % (q, i, self.dgen[q][i])))
            self.dgen[q][i] += 1
            self.dsem[q][i] = self.es.enter_context(self.nc.semaphore("d_%s%d_%d" % (q, i, self.dgen[q][i])))
            self.dcnt[q][i] = 0
        sem = self.dsem[q][i]
        key = "d_%s%d_%d" % (q, i, self.dgen[q][i])
        if self.dcnt[q][i] > 0:
            self._wait(q, (sem, self.dcnt[q][i], key))
        if fn is not None:
            ins = fn(self.eng[q])
        else:
            ins = self.eng[q].dma_start(out=out, in_=in_, **kw)
        self.dcnt[q][i] += 16
        ins.then_inc(sem, 16)
        tok = (sem, self.dcnt[q][i], key)
        self._record(tok, reads, writes)
        self.ninstr += 1
        return tok

    def barrier(self):
        toks = []
        for e in ("pe", "act", "dve", "pool"):
            if self.ccnt[e] > 0:
                toks.append((self.csem[e], self.ccnt[e], "c_%s_%d" % (e, self.cgen[e])))
        for q in ("sp", "act", "pool"):
            for i in range(len(self.dsem[q])):
                if self.dcnt[q][i] > 0:
                    toks.append((self.dsem[q][i], self.dcnt[q][i], "d_%s%d_%d" % (q, i, self.dgen[q][i])))
        for e in self.eng:
            for t in toks:
                if not (t[2].startswith("c_" + e + "_")):
                    self._wait(e, t)

    def wait_all(self, e):
        for r, t in list(self.lastw.items()):
            self._wait(e, t)


class Prog:
    def __init__(self, T, M, depth, dbg=()):
        self.T, self.M, self.depth = T, M, depth
        self.TT = T + M
        self.NB = self.TT // 128
        self.dbg = dbg
        self.capL = 2 * T // NE
        self.capC = 2 * M // NE
        nc = self.nc = bass.Bass("TRN2", target_bir_lowering=False)
        self.es = ExitStack()
        self.k = K(nc, self.es)
        self._uid = 0

        def din(name, shape, dt=F32):
            return nc.dram_tensor(name, list(shape), dt, kind="ExternalInput").ap()

        self.din = din
        TT = self.TT
        self.x = din("x", [T, D])
        self.ctx = din("ctx", [M, D])
        self.cfm = din("cfm", [128, 2 * KC])
        self.ident = din("ident", [128, 128])
        self.pos96 = din("pos96", [96, TT])
        self.inv96 = din("inv96", [96, 1])
        self.permT = din("permT", [96, 96])
        self.selpe = din("selpe", [32, 96])
        self.band = din("band", [2, 128, 32])
        nl = depth
        ne_ = (depth + 1) // 2
        no_ = max(1, depth // 2)
        self.ada_w = din("ada_w", [nl, D, 6 * D])
        self.ada_b = din("ada_b", [nl, 6 * D])
        self.norm1_g = din("norm1_g", [nl, D])
        self.norm2_g = din("norm2_g", [nl, D])
        self.router_w = din("router_w", [nl, D, NE])
        self.w_gate = din("exp_w_gate", [nl, NE, D, DFF])
        self.w_up = din("exp_w_up", [nl, NE, D, DFF])
        self.w_down = din("exp_w_down", [nl, NE, DFF, D])
        self.ev_w_in = din("ev_w_in", [ne_, D, 928])
        self.pool_w = din("pool_w", [ne_, 4, 128, 128])
        self.pool_scale = din("pool_scale", [ne_, 512])
        self.qa_g = din("q_a_norm_g", [ne_, 256])
        self.w_uq = din("w_uq", [ne_, 256, 768])
        self.kva_g = din("kv_a_norm_g", [ne_, 128])
        self.w_ukv = din("w_ukv", [ne_, 128, 1024])
        self.qn_g = din("q_norm_g", [ne_, 96])
        self.kn_g = din("k_norm_g", [ne_, 96])
        self.ev_w_out = din("ev_w_out", [ne_, D, D])
        self.od_w_in = din("od_w_in", [no_, D, D])
        self.s5_lam_re = din("s5_lam_re", [no_, 2, 64, 64])
        self.s5_lam_im = din("s5_lam_im", [no_, 2, 64, 64])
        self.s5_log_dt = din("s5_log_dt", [no_, 2, 64])
        self.s5_b_re = din("s5_b_re", [no_, 2, 64, 64, 16])
        self.s5_b_im = din("s5_b_im", [no_, 2, 64, 64, 16])
        self.s5_c_re = din("s5_c_re", [no_, 2, 64, 16, 64])
        self.s5_c_im = din("s5_c_im", [no_, 2, 64, 16, 64])
        self.s5_d = din("s5_d", [no_, D])
        self.od_w_out = din("od_w_out", [no_, D, 2 * D])
        self.y = nc.dram_tensor("y", [T, D], F32, kind="ExternalOutput").ap()

        def scr(name, shape, dt=F32):
            return nc.dram_tensor(name, list(shape), dt, kind=("ExternalOutput" if name in dbg else "Internal")).ap()

        self.scr = scr
        self.h_all = scr("h_all", [TT, D])
        self.h2rows = scr("h2rows", [TT, D])
        self.poolT = scr("poolT", [512, TT])
        self.catT = scr("catT", [D, TT], BF16)
        self.qT = scr("qT", [8, 96, TT], BF16)
        self.kT = scr("kT", [8, 96, TT], BF16)
        self.vR = scr("vR", [8, TT, 64], BF16)
        self.attU = scr("attU", [8, 64, TT])
        self.asum = scr("asum", [8, TT])
        self.cosT = scr("cosT", [96, TT])
        self.sinT = scr("sinT", [96, TT])
        self.dbg_out = {}

        self.P = self.es
        self.idt = self.sb("idt", [128, 128])
        self.ones = self.sb("ones", [128, 128])
        self.cf = self.sb("cf", [128, 2 * KC])
        self.modrows = scr("modrows", [2, 6 * D])
        self.epsT = self.sb("epsT", [128, 1])
        self.ps = [self.es.enter_context(nc.psum_tensor("ps%d" % i, [128, 512], F32)) for i in range(8)]
        self.psi = 0

    def sb(self, name, shape, dt=F32, es=None):
        self._uid += 1
        return (es or self.es).enter_context(self.nc.sbuf_tensor("%s_u%d" % (name, self._uid), list(shape), dt))

    def dbg_tensor(self, name, shape, dt=F32):
        t = self.nc.dram_tensor("dbg_" + name, list(shape), dt, kind="ExternalOutput").ap()
        self.dbg_out[name] = t
        return t

    def nps(self, lo=0, hi=8):
        i = lo + (self.psi % (hi - lo))
        self.psi += 1
        return i

    def hreg(self, t0, n):
        return [("h", b) for b in range(t0 // 128, (t0 + n + 127) // 128)]

    def col_load(self, dst, src1d, p, writes):
        ncol = src1d.shape[0] // p
        for c in range(ncol):
            self.k.dma("sp", dst[0:p, c:c + 1], src1d[c * p:(c + 1) * p].rearrange("(p o) -> p o", o=1), writes=writes)

    def bcast_row(self, q, dst, row_ap, n, writes):
        self.k.dma(q, dst, row_ap.partition_broadcast(dst.shape[0]), writes=writes)

    def build(self):
        k, nc = self.k, self.nc
        T, M, TT = self.T, self.M, self.TT
        k.dma("sp", self.idt[:], self.ident[:, :], writes=["idt"])
        k.op("pool", lambda e: e.memset(self.ones[:], 1.0), writes=["ones"])
        k.op("pool", lambda e: e.memset(self.epsT[:], EPS), writes=["epsT"])
        k.dma("sp", self.h_all[0:M, :], self.ctx[:, :], writes=self.hreg(0, M))
        nch = max(1, T // 1024)
        for i in range(nch):
            n = T // nch
            k.dma("sp", self.h_all[M + i * n:M + (i + 1) * n, :], self.x[i * n:(i + 1) * n, :], writes=self.hreg(M + i * n, n))
        with ExitStack() as es:
            cf = self.cf
            sg = self.sb("cf_sg", [128, 2 * KC], es=es)
            k.dma("sp", cf[:], self.cfm[:, :], writes=["cf"])
            k.op("act", lambda e: e.activation(out=sg[:], in_=cf[:], func=AF.Sigmoid), reads=["cf"], writes=["cf_sg"])
            k.op("dve", lambda e: e.tensor_tensor(out=cf[:], in0=cf[:], in1=sg[:], op=ALU.mult), reads=["cf", "cf_sg"], writes=["cf"])
            self.rope_tables(es)
            k.barrier()
        for layer in range(self.depth):
            need_ctx = layer < self.depth - 1
            if True:
                self.adaln(layer)
                if layer % 2 == 0:
                    self.even_layer(layer, need_ctx)
                else:
                    self.odd_layer(layer, need_ctx)
                self.moe_layer(layer, need_ctx)
        nch = max(1, T // 1024)
        for i in range(nch):
            n = T // nch
            k.dma("sp", self.y[i * n:(i + 1) * n, :], self.h_all[M + i * n:M + (i + 1) * n, :], reads=self.hreg(M + i * n, n), writes=[("y", i)])
        for name, (src, reg) in getattr(self, "dbg_copies", {}).items():
            pass
        k.wait_all("sp")
        self.es.close()
        return nc

    def rope_tables(self, es0):
        k = self.k
        TT = self.TT
        with ExitStack() as es:
            W = 1024
            inv = self.sb("rp_inv", [96, 1], es=es)
            k.dma("sp", inv[:], self.inv96[:, :], writes=["rp_inv"])
            tl = [(self.sb("rp_a%d" % i, [96, W], es=es), self.sb("rp_ai%d" % i, [96, W], I32, es=es), self.sb("rp_b%d" % i, [96, W], es=es), self.sb("rp_c%d" % i, [96, W], es=es)) for i in range(2)]
            for ti_, t0 in enumerate(range(0, TT, W)):
                n = min(W, TT - t0)
                a, ai, b, c = tl[ti_ % 2]
                ra, rai, rb, rc = ("rpa", ti_ % 2), ("rpai", ti_ % 2), ("rpb", ti_ % 2), ("rpc", ti_ % 2)
                k.dma("sp", a[:, 0:n], self.pos96[:, t0:t0 + n], writes=[ra])
                k.op("dve", lambda e: e.tensor_scalar(out=a[:, 0:n], in0=a[:, 0:n], scalar1=inv[:, 0:1], scalar2=None, op0=ALU.mult), reads=[ra, "rp_inv"], writes=[ra])
                k.op("dve", lambda e: e.tensor_scalar(out=b[:, 0:n], in0=a[:, 0:n], scalar1=1.0 / TWO_PI, scalar2=None, op0=ALU.mult), reads=[ra], writes=[rb])
                k.op("dve", lambda e: e.tensor_copy(out=ai[:, 0:n], in_=b[:, 0:n]), reads=[rb], writes=[rai])
                k.op("dve", lambda e: e.tensor_copy(out=b[:, 0:n], in_=ai[:, 0:n]), reads=[rai], writes=[rb])
                k.op("dve", lambda e: e.scalar_tensor_tensor(out=a[:, 0:n], in0=b[:, 0:n], scalar=-TWO_PI, in1=a[:, 0:n], op0=ALU.mult, op1=ALU.add), reads=[ra, rb], writes=[ra])
                k.op("dve", lambda e: e.tensor_scalar(out=a[:, 0:n], in0=a[:, 0:n], scalar1=math.pi, scalar2=-math.pi, op0=ALU.min, op1=ALU.max), reads=[ra], writes=[ra])
                k.op("act", lambda e: e.activation(out=b[:, 0:n], in_=a[:, 0:n], func=AF.Sin), reads=[ra, rb], writes=[rb])
                k.dma("sp", self.sinT[:, t0:t0 + n], b[:, 0:n], reads=[rb], writes=[("sinT", t0)])
                k.op("dve", lambda e: e.scalar_tensor_tensor(out=c[:, 0:n], in0=a[:, 0:n], scalar=-1.0, in1=a[:, 0:n], op0=ALU.mult, op1=ALU.max), reads=[ra], writes=[rc])
                k.op("dve", lambda e: e.tensor_scalar(out=c[:, 0:n], in0=c[:, 0:n], scalar1=-1.0, scalar2=math.pi / 2, op0=ALU.mult, op1=ALU.add), reads=[rc], writes=[rc])
                k.op("act", lambda e: e.activation(out=c[:, 0:n], in_=c[:, 0:n], func=AF.Sin), reads=[rc], writes=[rc])
                k.dma("sp", self.cosT[:, t0:t0 + n], c[:, 0:n], reads=[rc], writes=[("cosT", t0)])
            k.barrier()
            self.rope_regs = [("sinT", t0) for t0 in range(0, TT, W)] + [("cosT", t0) for t0 in range(0, TT, W)]

    def adaln(self, layer):
        k = self.k
        with ExitStack() as es:
            wt = [self.sb("adaw_%d" % i, [128, KC, 512], es=es) for i in range(2)]
            bt = self.sb("adab", [2, 6 * D], es=es)
            mod = self.sb("adamod", [2, 6 * D], es=es)
            g1 = self.sb("ada_g1", [2, D], es=es)
            g2 = self.sb("ada_g2", [2, D], es=es)
            k.dma("sp", bt[:], self.ada_b[layer, :].partition_broadcast(2), writes=["adab"])
            k.dma("sp", g1[:], self.norm1_g[layer, :].partition_broadcast(2), writes=["ada_g"])
            k.dma("sp", g2[:], self.norm2_g[layer, :].partition_broadcast(2), writes=["ada_g"])
            for nb in range(12):
                i = nb % 2
                c0 = nb * 512
                k.dma("pool" if nb % 2 else "sp", wt[i][:], self.ada_w[layer, :, c0:c0 + 512].rearrange("(kc p) n -> p kc n", p=128), writes=[("adaw", i)])
                b = self.nps()
                for kc in range(KC):
                    k.op("pe", lambda e: e.matmul(self.ps[b][0:2, :], lhsT=self.cf[:, kc:2 * KC:KC], rhs=wt[i][:, kc, :], start=(kc == 0), stop=(kc == KC - 1)),
                         reads=["cf", ("adaw", i)], writes=[("ps", b)])
                k.op("dve", lambda e: e.tensor_tensor(out=mod[:, c0:c0 + 512], in0=self.ps[b][0:2, :], in1=bt[:, c0:c0 + 512], op=ALU.add),
                     reads=[("ps", b), "adab"], writes=["adamod"])
            for (gt, c0) in ((g1, D), (g2, 4 * D)):
                k.op("dve", lambda e: e.scalar_tensor_tensor(out=mod[:, c0:c0 + D], in0=mod[:, c0:c0 + D], scalar=1.0, in1=gt[:], op0=ALU.add, op1=ALU.mult),
                     reads=["adamod", "ada_g"], writes=["adamod"])
            k.dma("sp", self.modrows[:, :], mod[:], reads=["adamod"], writes=["modrows"])
            k.barrier()

    def load_mod(self, es, chunk, name):
        out = []
        for s in range(2):
            t = self.sb("%s_%d" % (name, s), [128, D], es=es)
            self.k.dma("sp", t[:], self.modrows[s, chunk * D:(chunk + 1) * D].partition_broadcast(128), reads=["modrows"], writes=[(name, s)])
            out.append(t)
        return out

    def tm_norm(self, ht, hreg, at, areg, s, Wt, SHt, n, tag):
        k = self.k
        i = self.nsc_i = (getattr(self, "nsc_i", 0) + 1) % len(self.nsc)
        ss, junk = self.nsc[i]
        rs, rj = ("nss", i), ("nj", i)
        k.op("act", lambda e: e.activation(out=junk[0:n, :], in_=ht[0:n, :], func=AF.Square, accum_out=ss[0:n, 0:1]), reads=[hreg], writes=[rs, rj])
        k.op("dve", lambda e: e.tensor_scalar(out=ss[0:n, 1:2], in0=ss[0:n, 0:1], scalar1=1.0 / D, scalar2=EPS, op0=ALU.mult, op1=ALU.add), reads=[rs], writes=[rs])
        k.op("act", lambda e: e.activation(out=ss[0:n, 2:3], in_=ss[0:n, 1:2], func=AF.Sqrt), reads=[rs], writes=[rs])
        k.op("dve", lambda e: e.reciprocal(out=ss[0:n, 3:4], in_=ss[0:n, 2:3]), reads=[rs], writes=[rs])
        k.op("dve", lambda e: e.scalar_tensor_tensor(out=at[0:n, :], in0=ht[0:n, :], scalar=ss[0:n, 3:4], in1=Wt[s][0:n, :], op0=ALU.mult, op1=ALU.mult),
             reads=[hreg, rs, (tag + "_W", s)], writes=[areg])
        k.op("pool", lambda e: e.tensor_tensor(out=at[0:n, :], in0=at[0:n, :], in1=SHt[s][0:n, :], op=ALU.add), reads=[areg, (tag + "_SH", s)], writes=[areg])

    def transpose_block(self, at, areg, n, dst, dreg, col0, evac="act"):
        k = self.k
        for half in range(2):
            b = self.nps()
            for j in range(4):
                kc = half * 4 + j
                k.op("pe", lambda e: e.transpose(out=self.ps[b][:, j * 128:j * 128 + n], in_=at[0:n, kc * 128:(kc + 1) * 128], identity=self.idt[0:n, 0:n]),
                     reads=[areg, "idt"], writes=[("ps", b)])
            src = self.ps[b][:, :].rearrange("p (j c) -> p j c", j=4)[:, :, 0:n]
            dsta = dst[:, half * 4:half * 4 + 4, col0:col0 + n]
            if evac == "act":
                k.op("act", lambda e: e.activation(out=dsta, in_=src, func=AF.Identity), reads=[("ps", b)], writes=[dreg])
            else:
                k.op("dve", lambda e: e.tensor_copy(out=dsta, in_=src), reads=[("ps", b)], writes=[dreg])

    def load_w_bf16(self, dst, src_rows_ap, writes, q="pool"):
        nk = dst.shape[1]
        for kc in range(nk):
            self.k.dma(q, dst[:, kc, :], src_rows_ap[kc * 128:(kc + 1) * 128, :], writes=writes, max_dma_last_dim=4096)

    def stiles(self):
        out = []
        for t0 in range(0, self.M, 512):
            out.append((t0, min(512, self.M - t0), 1))
        for t0 in range(self.M, self.TT, 512):
            out.append((t0, min(512, self.TT - t0), 0))
        return out

    def even_layer(self, layer, need_ctx):
        j = layer // 2
        self.even_proj(layer, j)
        self.pool_stage(j)
        self.attention(j, need_ctx)
        self.mix_out(layer, self.ev_w_out[j], 8, need_ctx, self.even_cat_loader)

    def even_proj(self, layer, j):
        k = self.k
        M, TT = self.M, self.TT
        with ExitStack() as es:
            wIn = self.sb("ev_wIn", [128, KC, 928], BF16, es=es)
            self.load_w_bf16(wIn, self.ev_w_in[j], ["ev_wIn"])
            wuq = self.sb("ev_wuq", [128, 2, 768], BF16, es=es)
            self.load_w_bf16(wuq, self.w_uq[j], ["ev_wuq"])
            wkpad = self.sb("ev_wk", [128, 8, 96], BF16, es=es)
            wv = self.sb("ev_wv", [128, 8, 64], BF16, es=es)
            k.op("pool", lambda e: e.memset(wkpad[:], 0.0), writes=["ev_wk"])
            ukv = self.w_ukv[j].rearrange("k (h two d) -> k h two d", h=8, two=2)
            k.dma("pool", wkpad[:, :, 0:64], ukv[:, :, 0, :], reads=[], writes=["ev_wk"])
            k.dma("pool", wv[:], ukv[:, :, 1, :], writes=["ev_wv"])
            selpe = self.sb("ev_selpe", [32, 96], BF16, es=es)
            k.dma("pool", selpe[:], self.selpe[:, :], writes=["ev_selpe"])
            permT = self.sb("ev_permT", [96, 96], es=es)
            k.dma("sp", permT[:], self.permT[:, :], writes=["ev_permT"])
            gq = self.sb("ev_gq", [128, 2], es=es)
            gkv = self.sb("ev_gkv", [128, 1], es=es)
            gqn = self.sb("ev_gqn", [96, 1], es=es)
            gkn = self.sb("ev_gkn", [96, 1], es=es)
            self.col_load(gq, self.qa_g[j, :], 128, ["ev_g"])
            self.col_load(gkv, self.kva_g[j, :], 128, ["ev_g"])
            self.col_load(gqn, self.qn_g[j, :], 96, ["ev_g"])
            self.col_load(gkn, self.kn_g[j, :], 96, ["ev_g"])
            Wt = self.load_mod(es, 1, "ev_W")
            SHt = self.load_mod(es, 0, "ev_SH")

            ht = [self.sb("ev_h%d" % i, [128, D], es=es) for i in range(2)]
            at = [self.sb("ev_a%d" % i, [128, D], es=es) for i in range(2)]
            self.nsc = [(self.sb("ev_ss%d" % i, [128, 4], es=es), self.sb("ev_jk%d" % i, [128, D], BF16, es=es)) for i in range(2)]
            aT = [self.sb("ev_aT%d" % i, [128, KC, 512], BF16, es=es) for i in range(2)]
            poolS = [self.sb("ev_pS%d" % i, [128, 4, 512], es=es) for i in range(2)]
            zq = self.sb("ev_zq", [128, 2, 512], es=es)
            zkv = self.sb("ev_zkv", [128, 512], es=es)
            kpe = self.sb("ev_kpe", [32, 512], BF16, es=es)
            sq = self.sb("ev_sq", [128, 2, 512], es=es)
            rq = self.sb("ev_rq", [128, 512], es=es)
            cqT = self.sb("ev_cqT", [128, 2, 512], BF16, es=es)
            ckvT = self.sb("ev_ckvT", [128, 512], BF16, es=es)
            cosS = self.sb("ev_cos", [96, 512], es=es)
            sinS = self.sb("ev_sin", [96, 512], es=es)
            hq = [self.sb("ev_hq%d" % i, [96, 512], es=es) for i in range(2)]
            hsq = [self.sb("ev_hsq%d" % i, [96, 512], es=es) for i in range(2)]
            hr = [self.sb("ev_hr%d" % i, [96, 512], es=es) for i in range(2)]
            hn = [self.sb("ev_hn%d" % i, [96, 512], es=es) for i in range(2)]
            ho = [self.sb("ev_ho%d" % i, [96, 512], BF16, es=es) for i in range(2)]
            vS = [self.sb("ev_vS%d" % i, [128, 512], BF16, es=es) for i in range(2)]
            hcnt = [0]
            bi = 0
            for sti, (t0, n, s) in enumerate(self.stiles()):
                ai = sti % 2
                for blk in range(n // 128):
                    i = bi % 2
                    bi += 1
                    r0 = t0 + blk * 128
                    k.dma("sp", ht[i][:], self.h_all[r0:r0 + 128, :], reads=self.hreg(r0, 128), writes=[("ev_h", i)])
                    self.tm_norm(ht[i], ("ev_h", i), at[i], ("ev_a", i), s, Wt, SHt, 128, "ev")
                    self.transpose_block(at[i], ("ev_a", i), 128, aT[ai], ("ev_aT", ai), blk * 128)
                for mc in range(8):
                    msz = 128 if mc < 7 else 32
                    b = self.nps()
                    for kc in range(KC):
                        k.op("pe", lambda e: e.matmul(self.ps[b][0:msz, 0:n], lhsT=wIn[:, kc, mc * 128:mc * 128 + msz], rhs=aT[ai][:, kc, 0:n], start=(kc == 0), stop=(kc == KC - 1)),
                             reads=["ev_wIn", ("ev_aT", ai)], writes=[("ps", b)])
                    if mc < 4:
                        k.op("act", lambda e: e.activation(out=poolS[ai][:, mc, 0:n], in_=self.ps[b][:, 0:n], func=AF.Identity), reads=[("ps", b)], writes=[("ev_pS", ai)])
                    elif mc < 6:
                        k.op("act", lambda e: e.activation(out=zq[:, mc - 4, 0:n], in_=self.ps[b][:, 0:n], func=AF.Identity), reads=[("ps", b)], writes=["ev_zq"])
                        k.op("dve", lambda e: e.tensor_tensor(out=sq[:, mc - 4, 0:n], in0=self.ps[b][:, 0:n], in1=zq[:, mc - 4, 0:n], op=ALU.mult), reads=[("ps", b), "ev_zq"], writes=["ev_sq"])
                    elif mc == 6:
                        k.op("act", lambda e: e.activation(out=zkv[:, 0:n], in_=self.ps[b][:, 0:n], func=AF.Identity), reads=[("ps", b)], writes=["ev_zkv"])
                    else:
                        k.op("act", lambda e: e.activation(out=kpe[:, 0:n], in_=self.ps[b][0:32, 0:n], func=AF.Identity), reads=[("ps", b)], writes=["ev_kpe"])
                k.dma("sp", self.poolT[:, t0:t0 + n].rearrange("(g p) t -> p g t", p=128), poolS[ai][:, :, 0:n], reads=[("ev_pS", ai)], writes=[("poolT", t0)])
                k.dma("sp", cosS[:, 0:n], self.cosT[:, t0:t0 + n], reads=self.rope_regs, writes=["ev_cos"])
                k.dma("sp", sinS[:, 0:n], self.sinT[:, t0:t0 + n], reads=self.rope_regs, writes=["ev_sin"])
                b = self.nps()
                for c in range(2):
                    k.op("pe", lambda e: e.matmul(self.ps[b][:, 0:n], lhsT=self.ones[:, :], rhs=sq[:, c, 0:n], start=(c == 0), stop=(c == 1)), reads=["ones", "ev_sq"], writes=[("ps", b)])
                k.op("act", lambda e: e.activation(out=rq[:, 0:n], in_=self.ps[b][:, 0:n], func=AF.Sqrt, scale=1.0 / 256, bias=self.epsT[0:128, 0:1]), reads=[("ps", b), "epsT"], writes=["ev_rq"])
                k.op("dve", lambda e: e.reciprocal(out=rq[:, 0:n], in_=rq[:, 0:n]), reads=["ev_rq"], writes=["ev_rq"])
                for c in range(2):
                    k.op("dve", lambda e: e.scalar_tensor_tensor(out=cqT[:, c, 0:n], in0=zq[:, c, 0:n], scalar=gq[:, c:c + 1], in1=rq[:, 0:n], op0=ALU.mult, op1=ALU.mult),
                         reads=["ev_zq", "ev_g", "ev_rq"], writes=["ev_cqT"])
                k.op("pool", lambda e: e.tensor_tensor(out=sq[:, 0, 0:n], in0=zkv[:, 0:n], in1=zkv[:, 0:n], op=ALU.mult), reads=["ev_zkv", "ev_sq"], writes=["ev_sq"])
                b = self.nps()
                k.op("pe", lambda e: e.matmul(self.ps[b][:, 0:n], lhsT=self.ones[:, :], rhs=sq[:, 0, 0:n], start=True, stop=True), reads=["ones", "ev_sq"], writes=[("ps", b)])
                k.op("act", lambda e: e.activation(out=rq[:, 0:n], in_=self.ps[b][:, 0:n], func=AF.Sqrt, scale=1.0 / 128, bias=self.epsT[0:128, 0:1]), reads=[("ps", b), "ev_rq", "epsT"], writes=["ev_rq"])
                k.op("dve", lambda e: e.reciprocal(out=rq[:, 0:n], in_=rq[:, 0:n]), reads=["ev_rq"], writes=["ev_rq"])
                k.op("dve", lambda e: e.scalar_tensor_tensor(out=ckvT[:, 0:n], in0=zkv[:, 0:n], scalar=gkv[:, 0:1], in1=rq[:, 0:n], op0=ALU.mult, op1=ALU.mult),
                     reads=["ev_zkv", "ev_g", "ev_rq"], writes=["ev_ckvT"])
                for h in range(8):
                    for isk in range(2):
                        b = self.nps()
                        if isk == 0:
                            for c in range(2):
                                k.op("pe", lambda e: e.matmul(self.ps[b][0:96, 0:n], lhsT=wuq[:, c, h * 96:(h + 1) * 96], rhs=cqT[:, c, 0:n], start=(c == 0), stop=(c == 1)),
                                     reads=["ev_wuq", "ev_cqT"], writes=[("ps", b)])
                        else:
                            k.op("pe", lambda e: e.matmul(self.ps[b][0:96, 0:n], lhsT=wkpad[:, h, :], rhs=ckvT[:, 0:n], start=True, stop=False), reads=["ev_wk", "ev_ckvT"], writes=[("ps", b)])
                            k.op("pe", lambda e: e.matmul(self.ps[b][0:96, 0:n], lhsT=selpe[:, :], rhs=kpe[:, 0:n], start=False, stop=True), reads=["ev_selpe", "ev_kpe"], writes=[("ps", b)])
                        self.head_norm_rope(b, n, gkn if isk else gqn, (self.kT if isk else self.qT)[h, :, t0:t0 + n], ("kT" if isk else "qT", h, t0),
                                            hq, hsq, hr, hn, ho, permT, cosS, sinS, hcnt)
                for blk in range(n // 128):
                    b = self.nps()
                    vi = blk % 2
                    k.op("pe", lambda e: e.matmul(self.ps[b][:, 0:512], lhsT=ckvT[:, blk * 128:(blk + 1) * 128], rhs=wv[:].rearrange("p h d -> p (h d)"), start=True, stop=True),
                         reads=["ev_ckvT", "ev_wv"], writes=[("ps", b)])
                    k.op("act", lambda e: e.activation(out=vS[vi][:, :], in_=self.ps[b][:, :], func=AF.Identity), reads=[("ps", b)], writes=[("ev_vS", vi)])
                    r0 = t0 + blk * 128
                    k.dma("sp", self.vR[:, r0:r0 + 128, :].rearrange("h t d -> t h d"), vS[vi][:, :].rearrange("p (h d) -> p h d", h=8), reads=[("ev_vS", vi)], writes=[("vR", r0 // 128)])
            k.barrier()

    def head_norm_rope(self, b, n, gcol, out_ap, oreg, hq, hsq, hr, hn, ho, permT, cosS, sinS, hcnt):
        k = self.k
        i = hcnt[0] % 2
        hcnt[0] += 1
        R = lambda nm: (nm, i)
        k.op("act", lambda e: e.activation(out=hq[i][:, 0:n], in_=self.ps[b][0:96, 0:n], func=AF.Identity), reads=[("ps", b)], writes=[R("hq")])
        k.op("act", lambda e: e.activation(out=hsq[i][:, 0:n], in_=self.ps[b][0:96, 0:n], func=AF.Square), reads=[("ps", b)], writes=[R("hsq")])
        b2 = self.nps()
        k.op("pe", lambda e: e.matmul(self.ps[b2][0:96, 0:n], lhsT=self.ones[0:96, 0:96], rhs=hsq[i][:, 0:n], start=True, stop=True), reads=["ones", R("hsq")], writes=[("ps", b2)])
        k.op("act", lambda e: e.activation(out=hr[i][:, 0:n], in_=self.ps[b2][0:96, 0:n], func=AF.Sqrt, scale=1.0 / 96, bias=self.epsT[0:96, 0:1]), reads=[("ps", b2), "epsT"], writes=[R("hr")])
        k.op("dve", lambda e: e.reciprocal(out=hr[i][:, 0:n], in_=hr[i][:, 0:n]), reads=[R("hr")], writes=[R("hr")])
        k.op("dve", lambda e: e.scalar_tensor_tensor(out=hn[i][:, 0:n], in0=hq[i][:, 0:n], scalar=gcol[:, 0:1], in1=hr[i][:, 0:n], op0=ALU.mult, op1=ALU.mult),
             reads=[R("hq"), "ev_g", R("hr")], writes=[R("hn")])
        b3 = self.nps()
        k.op("pe", lambda e: e.matmul(self.ps[b3][0:96, 0:n], lhsT=permT[:, :], rhs=hn[i][:, 0:n], start=True, stop=True), reads=["ev_permT", R("hn")], writes=[("ps", b3)])
        k.op("dve", lambda e: e.tensor_tensor(out=hsq[i][:, 0:n], in0=self.ps[b3][0:96, 0:n], in1=sinS[:, 0:n], op=ALU.mult), reads=[("ps", b3), "ev_sin", R("hsq")], writes=[R("hsq")])
        k.op("pool", lambda e: e.tensor_tensor(out=hq[i][:, 0:n], in0=hn[i][:, 0:n], in1=cosS[:, 0:n], op=ALU.mult), reads=[R("hn"), "ev_cos", R("hq")], writes=[R("hq")])
        k.op("dve", lambda e: e.tensor_tensor(out=ho[i][:, 0:n], in0=hq[i][:, 0:n], in1=hsq[i][:, 0:n], op=ALU.add), reads=[R("hq"), R("hsq")], writes=[R("ho")])
        k.dma("sp", out_ap, ho[i][:, 0:n], reads=[R("ho")], writes=[oreg])

    def pool_stage(self, j):
        k = self.k
        M, T, TT = self.M, self.T, self.TT
        with ExitStack() as es:
            pw = self.sb("pl_w", [128, 4, 128], BF16, es=es)
            k.dma("pool", pw[:], self.pool_w[j].rearrange("g c d -> c g d"), writes=["pl_w"])
            psc = self.sb("pl_sc", [128, 4], es=es)
            self.col_load(psc, self.pool_scale[j, :], 128, ["pl_sc"])
            xin = [self.sb("pl_x%d" % i, [128, 528], es=es) for i in range(2)]
            s1 = [self.sb("pl_s%d" % i, [128, 528], es=es) for i in range(2)]
            s2 = [self.sb("pl_t%d" % i, [128, 528], es=es) for i in range(2)]
            rc = [self.sb("pl_rc%d" % i, [128, 512], es=es) for i in range(2)]
            po = [self.sb("pl_po%d" % i, [128, 512], BF16, es=es) for i in range(2)]
            co = [self.sb("pl_co%d" % i, [128, 512], BF16, es=es) for i in range(2)]
            cnt = 0
            allpool = [("poolT", t0) for (t0, n, s) in self.stiles()]
            for g, w in enumerate((2, 4, 8, 16)):
                lo = w // 2
                hi = w - lo - 1
                for (q0, qn) in ((0, M), (M, T)):
                    for t0 in range(q0, q0 + qn, 512):
                        n = min(512, q0 + qn - t0)
                        i = cnt % 2
                        cnt += 1
                        X, S1, S2, RC = ("pl_x", i), ("pl_s", i), ("pl_t", i), ("pl_rc", i)
                        a0 = max(q0, t0 - 8)
                        a1 = min(q0 + qn, t0 + n + 8)
                        k.op("pool", lambda e: e.memset(xin[i][:], 0.0), writes=[X])
                        k.dma("sp", xin[i][:, 8 - (t0 - a0):8 + (a1 - t0)], self.poolT[g * 128:(g + 1) * 128, a0:a1], reads=allpool, writes=[X])
                        L = n + 16
                        cur, cr = xin[i], X
                        step = 1
                        tmp = [(S1, s1[i]), (S2, s2[i])]
                        ti = 0
                        while step < w:
                            rgn, dst = tmp[ti % 2]
                            ti += 1
                            L2 = L - step
                            k.op("dve", lambda e: e.tensor_tensor(out=dst[:, 0:L2], in0=cur[:, 0:L2], in1=cur[:, step:step + L2], op=ALU.add), reads=[cr], writes=[rgn])
                            cur, cr, L = dst, rgn, L2
                            step *= 2
                        k.op("pool", lambda e: e.memset(rc[i][:], 1.0 / w), writes=[RC])
                        for tt in range(n):
                            t = t0 + tt - q0
                            c = min(t + hi + 1, qn) - max(t - lo, 0)
                            if c != w:
                                k.op("pool", lambda e: e.memset(rc[i][:, tt:tt + 1], 1.0 / c), writes=[RC])
                            elif tt > 16 and tt < n - 17:
                                pass
                        o = 8 - lo
                        rgn, dst = tmp[ti % 2]
                        k.op("dve", lambda e: e.tensor_tensor(out=dst[:, 0:n], in0=cur[:, o:o + n], in1=rc[i][:, 0:n], op=ALU.mult), reads=[cr, RC], writes=[rgn])
                        k.op("dve", lambda e: e.tensor_tensor(out=po[i][:, 0:n], in0=dst[:, 0:n], in1=xin[i][:, 8:8 + n], op=ALU.subtract), reads=[rgn, X], writes=[("pl_po", i)])
                        b = self.nps()
                        k.op("pe", lambda e: e.matmul(self.ps[b][:, 0:n], lhsT=pw[:, g, :], rhs=po[i][:, 0:n], start=True, stop=True), reads=["pl_w", ("pl_po", i)], writes=[("ps", b)])
                        k.op("act", lambda e: e.activation(out=co[i][:, 0:n], in_=self.ps[b][:, 0:n], func=AF.Identity, scale=psc[:, g:g + 1]), reads=[("ps", b), "pl_sc"], writes=[("pl_co", i)])
                        k.dma("sp", self.catT[g * 128:(g + 1) * 128, t0:t0 + n], co[i][:, 0:n], reads=[("pl_co", i)], writes=[("catT", g, t0)])
            k.barrier()

    def attention(self, j, need_ctx):
        k = self.k
        M, T, TT, NB = self.M, self.T, self.TT, self.NB
        scale = 96.0 ** -0.5
        qregs = lambda nm, h: [(nm, h, t0) for (t0, n, s) in self.stiles()]
        with ExitStack() as es:
            KT = [self.sb("at_K%d" % i, [96, TT], BF16, es=es) for i in range(2)]
            QT = [self.sb("at_Q%d" % i, [96, TT], BF16, es=es) for i in range(2)]
            VA = [self.sb("at_V%d" % i, [128, NB, 65], BF16, es=es) for i in range(2)]
            pT = [self.sb("at_p%d" % i, [128, 512], BF16, es=es) for i in range(4)]
            oS = [self.sb("at_o%d" % i, [65, 512], es=es) for i in range(2)]
            pcnt = 0
            ocnt = 0
            for h in range(8):
                i = h % 2
                k.dma("sp", KT[i][:], self.kT[h, :, :], reads=qregs("kT", h), writes=[("at_K", i)])
                k.dma("sp", QT[i][:], self.qT[h, :, :], reads=qregs("qT", h), writes=[("at_Q", i)])
                k.op("pool", lambda e: e.memset(VA[i][:], 1.0), writes=[("at_V", i)])
                k.dma("sp", VA[i][:, :, 0:64], self.vR[h, :, :].rearrange("(b p) d -> p b d", p=128), reads=[("vR", b_) for b_ in range(NB)], writes=[("at_V", i)])
                qtiles = [(t0, n, s) for (t0, n, s) in self.stiles() if (s == 0 or need_ctx)]
                for (t0, n, s) in qtiles:
                    nkb = NB if s == 0 else M // 128
                    ob = self.nps(0, 2)
                    LA = 3
                    slots = {}
                    for it in range(nkb + LA):
                        if it < nkb:
                            kb = it
                            sbk = self.nps(2, 8)
                            k.op("pe", lambda e: e.matmul(self.ps[sbk][:, 0:n], lhsT=KT[i][:, kb * 128:(kb + 1) * 128], rhs=QT[i][:, t0:t0 + n], start=True, stop=True),
                                 reads=[("at_K", i), ("at_Q", i)], writes=[("ps", sbk)])
                            pi = pcnt % 4
                            pcnt += 1
                            slots[kb] = pi
                            k.op("act", lambda e: e.activation(out=pT[pi][:, 0:n], in_=self.ps[sbk][:, 0:n], func=AF.Exp, scale=scale), reads=[("ps", sbk)], writes=[("at_p", pi)])
                        if it - LA >= 0:
                            kb = it - LA
                            pi = slots.pop(kb)
                            k.op("pe", lambda e: e.matmul(self.ps[ob][0:65, 0:n], lhsT=VA[i][:, kb, :], rhs=pT[pi][:, 0:n], start=(kb == 0), stop=(kb == nkb - 1)),
                                 reads=[("at_V", i), ("at_p", pi)], writes=[("ps", ob)])
                    oi = ocnt % 2
                    ocnt += 1
                    k.op("dve", lambda e: e.tensor_copy(out=oS[oi][:, 0:n], in_=self.ps[ob][0:65, 0:n]), reads=[("ps", ob)], writes=[("at_o", oi)])
                    k.dma("sp", self.attU[h, :, t0:t0 + n], oS[oi][0:64, 0:n], reads=[("at_o", oi)], writes=[("attU", h, t0)])
                    k.dma("sp", self.asum[h:h + 1, t0:t0 + n], oS[oi][64:65, 0:n], reads=[("at_o", oi)], writes=[("asum", h, t0)])
            k.barrier()

    def even_cat_loader(self, es):
        k = self.k
        aU = [self.sb("mo_aU%d" % i, [128, 512], es=es) for i in range(2)]
        sB = [self.sb("mo_sB%d" % i, [128, 512], es=es) for i in range(2)]
        cnt = [0]

        def load(catS, creg, t0, n):
            k.dma("sp", catS[:, 0:4, 0:n], self.catT[0:512, t0:t0 + n].rearrange("(g p) t -> p g t", p=128),
                  reads=[("catT", g, t0) for g in range(4)], writes=[creg])
            for c in range(4):
                i = cnt[0] % 2
                cnt[0] += 1
                A, S = ("mo_aU", i), ("mo_sB", i)
                k.dma("sp", aU[i][:, 0:n], self.attU[2 * c:2 * c + 2, :, t0:t0 + n].rearrange("h d t -> (h d) t"),
                      reads=[("attU", 2 * c, t0), ("attU", 2 * c + 1, t0)], writes=[A])
                for hh in range(2):
                    k.dma("sp", sB[i][hh * 64:(hh + 1) * 64, 0:n], self.asum[2 * c + hh, t0:t0 + n].partition_broadcast(64),
                          reads=[("asum", 2 * c + hh, t0)], writes=[S])
                k.op("dve", lambda e: e.reciprocal(out=sB[i][:, 0:n], in_=sB[i][:, 0:n]), reads=[S], writes=[S])
                k.op("dve", lambda e: e.tensor_tensor(out=catS[:, 4 + c, 0:n], in0=aU[i][:, 0:n], in1=sB[i][:, 0:n], op=ALU.mult), reads=[A, S], writes=[creg])
        return load

    def mix_out(self, layer, w_ap, nkc, need_ctx, loader_factory, glu=False):
        k = self.k
        with ExitStack() as es:
            nout = 2 * D if glu else D
            wo = self.sb("mo_w", [128, nkc, nout], BF16, es=es)
            self.load_w_bf16(wo, w_ap, ["mo_w"])
            load = loader_factory(es)
            G1 = self.load_mod(es, 2, "mo_G1")
            catS = [self.sb("mo_cat%d" % i, [128, nkc, 512], BF16, es=es) for i in range(2)]
            ht = [self.sb("mo_h%d" % i, [128, D], es=es) for i in range(2)]
            tt = [self.sb("mo_t%d" % i, [128, D], es=es) for i in range(2)]
            sg = [self.sb("mo_sg%d" % i, [128, 512], es=es) for i in range(2)] if glu else None
            bi = 0
            for sti, (t0, n, s) in enumerate(self.stiles()):
                if s == 1 and not need_ctx:
                    continue
                ci = sti % 2
                load(catS[ci], ("mo_cat", ci), t0, n)
                for blk in range(n // 128):
                    i = bi % 2
                    bi += 1
                    r0 = t0 + blk * 128
                    k.dma("sp", ht[i][:], self.h_all[r0:r0 + 128, :], reads=self.hreg(r0, 128), writes=[("mo_h", i)])
                    for half in range(2):
                        b = self.nps()
                        for kc in range(nkc):
                            k.op("pe", lambda e: e.matmul(self.ps[b][:, :], lhsT=catS[ci][:, kc, blk * 128:(blk + 1) * 128], rhs=wo[:, kc, half * 512:(half + 1) * 512], start=(kc == 0), stop=(kc == nkc - 1)),
                                 reads=[("mo_cat", ci), "mo_w"], writes=[("ps", b)])
                        if glu:
                            b2 = self.nps()
                            for kc in range(nkc):
                                k.op("pe", lambda e: e.matmul(self.ps[b2][:, :], lhsT=catS[ci][:, kc, blk * 128:(blk + 1) * 128], rhs=wo[:, kc, D + half * 512:D + (half + 1) * 512], start=(kc == 0), stop=(kc == nkc - 1)),
                                     reads=[("mo_cat", ci), "mo_w"], writes=[("ps", b2)])
                            k.op("act", lambda e: e.activation(out=sg[i][:, :], in_=self.ps[b2][:, :], func=AF.Sigmoid), reads=[("ps", b2)], writes=[("mo_sg", i)])
                            k.op("dve", lambda e: e.tensor_tensor(out=sg[i][:, :], in0=self.ps[b][:, :], in1=sg[i][:, :], op=ALU.mult), reads=[("ps", b), ("mo_sg", i)], writes=[("mo_sg", i)])
                            k.op("dve", lambda e: e.tensor_tensor(out=tt[i][:, half * 512:(half + 1) * 512], in0=sg[i][:, :], in1=G1[s][:, half * 512:(half + 1) * 512], op=ALU.mult),
                                 reads=[("mo_sg", i), ("mo_G1", s)], writes=[("mo_t", i)])
                        else:
                            k.op("dve", lambda e: e.tensor_tensor(out=tt[i][:, half * 512:(half + 1) * 512], in0=self.ps[b][:, :], in1=G1[s][:, half * 512:(half + 1) * 512], op=ALU.mult),
                                 reads=[("ps", b), ("mo_G1", s)], writes=[("mo_t", i)])
                    k.op("pool", lambda e: e.tensor_tensor(out=tt[i][:, :], in0=tt[i][:, :], in1=ht[i][:, :], op=ALU.add), reads=[("mo_t", i), ("mo_h", i)], writes=[("mo_t", i)])
                    k.dma("sp", self.h_all[r0:r0 + 128, :], tt[i][:, :], reads=[("mo_t", i)], writes=self.hreg(r0, 128))
            k.barrier()

    def odd_layer(self, layer, need_ctx):
        jo = layer // 2
        if not hasattr(self, "Ugc"):
            NCH = self.TT // 8
            self.NCH = NCH
            self.Ugc = self.scr("Ugc", [128, 64, NCH])
            self.XR = self.scr("XR", [64, 2, 64, NCH])
            self.XF = self.scr("XF", [64, 2, 64, NCH])
            self.SR = self.scr("SR", [64, 2, 64, NCH])
            self.SF = self.scr("SF", [64, 2, 64, NCH])
            self.WinT = self.scr("WinT", [2, 2, 128, 64, 64])
            self.Wo = self.scr("Wo", [2, 2, 64, 64, 128])
            self.Mm = self.scr("Mm", [128, 64, 128])
            self.D8 = self.scr("D8", [2, 64, 2, 2, 64])
        self.s5_precompute(jo)
        self.s5_pass1(layer, jo)
        self.s5_scans()
        self.s5_pass2(layer, jo, need_ctx)

    def sincos(self, a, ai, b, c, P, n, reg):
        k = self.k
        ra, rb, rc = (reg, "a"), (reg, "b"), (reg, "c")
        A, AI, B, C = a[0:P, 0:n], ai[0:P, 0:n], b[0:P, 0:n], c[0:P, 0:n]
        k.op("dve", lambda e: e.tensor_scalar(out=B, in0=A, scalar1=1.0 / TWO_PI, scalar2=None, op0=ALU.mult), reads=[ra], writes=[rb])
        k.op("dve", lambda e: e.tensor_copy(out=AI, in_=B), reads=[rb], writes=[(reg, "ai")])
        k.op("dve", lambda e: e.tensor_copy(out=B, in_=AI), reads=[(reg, "ai")], writes=[rb])
        k.op("dve", lambda e: e.scalar_tensor_tensor(out=A, in0=B, scalar=-TWO_PI, in1=A, op0=ALU.mult, op1=ALU.add), reads=[ra, rb], writes=[ra])
        k.op("dve", lambda e: e.tensor_scalar(out=A, in0=A, scalar1=math.pi, scalar2=-math.pi, op0=ALU.min, op1=ALU.max), reads=[ra], writes=[ra])
        k.op("act", lambda e: e.activation(out=B, in_=A, func=AF.Sin), reads=[ra, rb], writes=[rb])
        k.op("dve", lambda e: e.scalar_tensor_tensor(out=C, in0=A, scalar=-1.0, in1=A, op0=ALU.mult, op1=ALU.max), reads=[ra], writes=[rc])
        k.op("dve", lambda e: e.tensor_scalar(out=C, in0=C, scalar1=-1.0, scalar2=math.pi / 2, op0=ALU.mult, op1=ALU.add), reads=[rc], writes=[rc])
        k.op("act", lambda e: e.activation(out=C, in_=C, func=AF.Sin), reads=[rc], writes=[rc])

    def s5_precompute(self, jo):
        k = self.k
        GB = 16
        with ExitStack() as es:
            sb = lambda n, sh, dt=F32: self.sb("s5p_" + n, sh, dt, es=es)
            maskf = sb("maskf", [128, 128])
            maskr = sb("maskr", [128, 128])
            DT = sb("DT", [128, 64])
            k.op("pool", lambda e: e.memset(maskf[:], 0.0), writes=["s5maskf"])
            k.op("pool", lambda e: e.memset(maskr[:], 0.0), writes=["s5maskr"])
            for jb in range(4):
                j0 = 2 * jb
                k.op("pool", lambda e: e.memset(maskf[jb * 32:(jb + 1) * 32, (j0 + 2) * 16:128], 1.0), writes=["s5maskf"]) if j0 + 2 < 8 else None
                k.op("pool", lambda e: e.memset(maskr[jb * 32:(jb + 1) * 32, 0:j0 * 16], 1.0), writes=["s5maskr"]) if j0 > 0 else None
            bandf = sb("bandf", [128, 32])
            bandr = sb("bandr", [128, 32])
            k.dma("sp", bandf[:], self.band[0], writes=["s5band"])
            k.dma("sp", bandr[:], self.band[1], writes=["s5band"])
            for jb in range(4):
                k.op("pool", lambda e: e.tensor_copy(out=maskf[jb * 32:(jb + 1) * 32, jb * 32:(jb + 1) * 32], in_=bandf[jb * 32:(jb + 1) * 32, :]), reads=["s5band"], writes=["s5maskf"])
                k.op("pool", lambda e: e.tensor_copy(out=maskr[jb * 32:(jb + 1) * 32, jb * 32:(jb + 1) * 32], in_=bandr[jb * 32:(jb + 1) * 32, :]), reads=["s5band"], writes=["s5maskr"])
            dsrc = self.s5_d[jo, :].rearrange("(g q) -> q g", q=16)
            for j in range(8):
                k.dma("sp", DT[j * 16:(j + 1) * 16, :], dsrc, writes=["s5DT"], allow_slow_non_contiguous=True)
            nat = sb("nat", [128, 64])
            lamr = sb("lamr", [64, 64]); lami = sb("lami", [64, 64]); dt = sb("dt", [64, 64])
            lr = sb("lr", [64, 64]); th = sb("th", [64, 64])
            MAG = sb("MAG", [64, 17, 64]); ANG = sb("ANG", [64, 17 * 64]); ANGi = sb("ANGi", [64, 17 * 64], I32)
            SN = sb("SN", [64, 17 * 64]); CS = sb("CS", [64, 17 * 64])
            TAr = sb("TAr", [64, 17, 64]); TAi = sb("TAi", [64, 17, 64])
            t1 = sb("t1", [64, 64]); t2 = sb("t2", [64, 64]); cr = sb("cr", [64, 64]); ci = sb("ci", [64, 64])
            bre = sb("bre", [64, 64, 16]); bim = sb("bim", [64, 64, 16]); Bbr = sb("Bbr", [64, 64, 16]); Bbi = sb("Bbi", [64, 64, 16])
            Ctr = sb("Ctr", [64, 64, 16]); Cti = sb("Cti", [64, 64, 16])
            d8 = sb("d8", [64, 2, 2, 64])
            Ere = sb("Ere", [64, GB, 8, 16]); EimN = sb("EimN", [64, GB, 8, 16]); Eim = sb("Eim", [64, GB, 8, 16])
            Rre = sb("Rre", [64, GB, 8, 16]); Rim = sb("Rim", [64, GB, 8, 16])
            Wre = sb("Wre", [64, GB, 8, 16]); WimN = sb("WimN", [64, GB, 8, 16])
            tmpA = sb("tmpA", [64, GB, 8, 16]); tmpB = sb("tmpB", [64, GB, 8, 16])
            wint = [sb("wint%d" % i, [128, 2, GB, 64]) for i in range(1)]
            Macc = sb("Macc", [128, 64, 128])
            mt = sb("mt", [128, 128])
            for d in range(2):
                R_ = lambda nm: ("s5p", nm)
                for (src, dst, nm) in ((self.s5_lam_re, lamr, "lamr"), (self.s5_lam_im, lami, "lami")):
                    k.dma("sp", nat[0:64, :], src[jo, d], writes=[R_("nat")])
                    b = self.nps()
                    k.op("pe", lambda e: e.transpose(out=self.ps[b][0:64, 0:64], in_=nat[0:64, :], identity=self.idt[0:64, 0:64]), reads=[R_("nat"), "idt"], writes=[("ps", b)])
                    k.op("dve", lambda e: e.tensor_copy(out=dst[:], in_=self.ps[b][0:64, 0:64]), reads=[("ps", b)], writes=[R_(nm)])
                k.dma("sp", dt[:], self.s5_log_dt[jo, d, :].partition_broadcast(64), writes=[R_("dt")])
                k.op("act", lambda e: e.activation(out=dt[:], in_=dt[:], func=AF.Exp), reads=[R_("dt")], writes=[R_("dt")])
                k.op("dve", lambda e: e.tensor_tensor(out=lr[:], in0=lamr[:], in1=dt[:], op=ALU.mult), reads=[R_("lamr"), R_("dt")], writes=[R_("lr")])
                k.op("dve", lambda e: e.tensor_tensor(out=th[:], in0=lami[:], in1=dt[:], op=ALU.mult), reads=[R_("lami"), R_("dt")], writes=[R_("th")])
                if d == 0:
                    kl = [7 - j for j in range(8)] + [t - 7 for t in range(8)] + [8]
                else:
                    kl = [j for j in range(8)] + [-t for t in range(8)] + [8]
                for i, kv in enumerate(kl):
                    k.op("act", lambda e: e.activation(out=MAG[:, i, :], in_=lr[:], func=AF.Exp, scale=float(kv)), reads=[R_("lr")], writes=[R_("MAG")])
                    k.op("dve", lambda e: e.tensor_scalar(out=ANG[:, i * 64:(i + 1) * 64], in0=th[:], scalar1=float(kv), scalar2=None, op0=ALU.mult), reads=[R_("th")], writes=[("s5sc", "a")])
                self.sincos(ANG, ANGi, SN, CS, 64, 17 * 64, "s5sc")
                k.op("dve", lambda e: e.tensor_tensor(out=TAr[:].rearrange("s k g -> s (k g)"), in0=MAG[:].rearrange("s k g -> s (k g)"), in1=CS[:], op=ALU.mult), reads=[R_("MAG"), ("s5sc", "c")], writes=[R_("TAr")])
                k.op("dve", lambda e: e.tensor_tensor(out=TAi[:].rearrange("s k g -> s (k g)"), in0=MAG[:].rearrange("s k g -> s (k g)"), in1=SN[:], op=ALU.mult), reads=[R_("MAG"), ("s5sc", "b")], writes=[R_("TAi")])
                TA = [R_("TAr"), R_("TAi")]
                i1 = kl.index(1)
                k.op("dve", lambda e: e.tensor_scalar(out=t1[:], in0=TAr[:, i1, :], scalar1=-1.0, scalar2=None, op0=ALU.add), reads=TA, writes=[R_("t1")])
                k.op("dve", lambda e: e.tensor_tensor(out=cr[:], in0=t1[:], in1=lamr[:], op=ALU.mult), reads=[R_("t1"), R_("lamr")], writes=[R_("cr")])
                k.op("dve", lambda e: e.tensor_tensor(out=t2[:], in0=TAi[:, i1, :], in1=lami[:], op=ALU.mult), reads=TA + [R_("lami")], writes=[R_("t2")])
                k.op("dve", lambda e: e.tensor_tensor(out=cr[:], in0=cr[:], in1=t2[:], op=ALU.add), reads=[R_("cr"), R_("t2")], writes=[R_("cr")])
                k.op("dve", lambda e: e.tensor_tensor(out=ci[:], in0=TAi[:, i1, :], in1=lamr[:], op=ALU.mult), reads=TA + [R_("lamr")], writes=[R_("ci")])
                k.op("dve", lambda e: e.tensor_tensor(out=t2[:], in0=t1[:], in1=lami[:], op=ALU.mult), reads=[R_("t1"), R_("lami"), R_("t2")], writes=[R_("t2")])
                k.op("dve", lambda e: e.tensor_tensor(out=ci[:], in0=ci[:], in1=t2[:], op=ALU.subtract), reads=[R_("ci"), R_("t2")], writes=[R_("ci")])
                k.op("dve", lambda e: e.tensor_tensor(out=t1[:], in0=lamr[:], in1=lamr[:], op=ALU.mult), reads=[R_("lamr"), R_("t1")], writes=[R_("t1")])
                k.op("dve", lambda e: e.tensor_tensor(out=t2[:], in0=lami[:], in1=lami[:], op=ALU.mult), reads=[R_("lami"), R_("t2")], writes=[R_("t2")])
                k.op("dve", lambda e: e.tensor_tensor(out=t1[:], in0=t1[:], in1=t2[:], op=ALU.add), reads=[R_("t1"), R_("t2")], writes=[R_("t1")])
                k.op("dve", lambda e: e.reciprocal(out=t1[:], in_=t1[:]), reads=[R_("t1")], writes=[R_("t1")])
                k.op("dve", lambda e: e.tensor_tensor(out=cr[:], in0=cr[:], in1=t1[:], op=ALU.mult), reads=[R_("cr"), R_("t1")], writes=[R_("cr")])
                k.op("dve", lambda e: e.tensor_tensor(out=ci[:], in0=ci[:], in1=t1[:], op=ALU.mult), reads=[R_("ci"), R_("t1")], writes=[R_("ci")])
                k.dma("sp", bre[:], self.s5_b_re[jo, d].rearrange("g s q -> s g q"), writes=[R_("bre")])
                k.dma("sp", bim[:], self.s5_b_im[jo, d].rearrange("g s q -> s g q"), writes=[R_("bim")])
                bc = lambda t: t[:].unsqueeze(2).to_broadcast([64, 64, 16])
                k.op("dve", lambda e: e.tensor_tensor(out=Bbr[:], in0=bre[:], in1=bc(cr), op=ALU.mult), reads=[R_("bre"), R_("cr")], writes=[R_("Bbr")])
                k.op("dve", lambda e: e.tensor_tensor(out=Bbi[:], in0=bim[:], in1=bc(cr), op=ALU.mult), reads=[R_("bim"), R_("cr")], writes=[R_("Bbi")])
                k.op("dve", lambda e: e.tensor_tensor(out=bim[:], in0=bim[:], in1=bc(ci), op=ALU.mult), reads=[R_("bim"), R_("ci"), R_("Bbi")], writes=[R_("bim")])
                k.op("dve", lambda e: e.tensor_tensor(out=bre[:], in0=bre[:], in1=bc(ci), op=ALU.mult), reads=[R_("bre"), R_("ci"), R_("Bbr")], writes=[R_("bre")])
                k.op("dve", lambda e: e.tensor_tensor(out=Bbr[:], in0=Bbr[:], in1=bim[:], op=ALU.subtract), reads=[R_("Bbr"), R_("bim")], writes=[R_("Bbr")])
                k.op("dve", lambda e: e.tensor_tensor(out=Bbi[:], in0=Bbi[:], in1=bre[:], op=ALU.add), reads=[R_("Bbi"), R_("bre")], writes=[R_("Bbi")])
                for (src, dst, nm) in ((self.s5_c_re, Ctr, "Ctr"), (self.s5_c_im, Cti, "Cti")):
                    csrc = src[jo, d].rearrange("g p s -> (g p) s")
                    for g8 in range(8):
                        k.dma("sp", nat[:, :], csrc[g8 * 128:(g8 + 1) * 128, :], writes=[R_("nat")])
                        b = self.nps()
                        k.op("pe", lambda e: e.transpose(out=self.ps[b][0:64, 0:128], in_=nat[:, :], identity=self.idt[:, :]), reads=[R_("nat"), "idt"], writes=[("ps", b)])
                        k.op("dve", lambda e: e.tensor_copy(out=dst[:, g8 * 8:(g8 + 1) * 8, :].rearrange("s g p -> s (g p)"), in_=self.ps[b][0:64, 0:128]), reads=[("ps", b)], writes=[R_(nm)])
                k.op("dve", lambda e: e.tensor_copy(out=d8[:, 0, 0, :], in_=TAr[:, 16, :]), reads=TA, writes=[R_("d8")])
                k.op("dve", lambda e: e.tensor_copy(out=d8[:, 0, 1, :], in_=TAr[:, 16, :]), reads=TA, writes=[R_("d8")])
                k.op("dve", lambda e: e.tensor_scalar(out=d8[:, 1, 0, :], in0=TAi[:, 16, :], scalar1=-1.0, scalar2=None, op0=ALU.mult), reads=TA, writes=[R_("d8")])
                k.op("dve", lambda e: e.tensor_copy(out=d8[:, 1, 1, :], in_=TAi[:, 16, :]), reads=TA, writes=[R_("d8")])
                k.dma("sp", self.D8[d], d8[:], reads=[R_("d8")], writes=[("D8", d)])
                for g0 in range(0, 64, GB):
                    gs = slice(g0, g0 + GB)
                    tb = lambda T_, i: T_[:, i, gs].unsqueeze(2).to_broadcast([64, GB, 16])
                    for j in range(8):
                        for (Tidx, Xr, Xi, Ore, Oim, OimN, onm) in ((j, Bbr, Bbi, Ere, Eim, EimN, "E"), (8 + j, Ctr, Cti, Rre, Rim, None, "R")):
                            srcs = TA + [R_("Bbr"), R_("Bbi"), R_("Ctr"), R_("Cti")]
                            k.op("dve", lambda e: e.tensor_tensor(out=tmpA[:, :, j, :], in0=Xr[:, gs, :], in1=tb(TAr, Tidx), op=ALU.mult), reads=srcs, writes=[R_("tmpA")])
                            k.op("pool", lambda e: e.tensor_tensor(out=tmpB[:, :, j, :], in0=Xi[:, gs, :], in1=tb(TAi, Tidx), op=ALU.mult), reads=srcs, writes=[R_("tmpB")])
                            k.op("dve", lambda e: e.tensor_tensor(out=Ore[:, :, j, :], in0=tmpA[:, :, j, :], in1=tmpB[:, :, j, :], op=ALU.subtract), reads=[R_("tmpA"), R_("tmpB")], writes=[R_(onm + "re")])
                            k.op("dve", lambda e: e.tensor_tensor(out=tmpA[:, :, j, :], in0=Xi[:, gs, :], in1=tb(TAr, Tidx), op=ALU.mult), reads=srcs + [R_("tmpA")], writes=[R_("tmpA")])
                            k.op("pool", lambda e: e.tensor_tensor(out=tmpB[:, :, j, :], in0=Xr[:, gs, :], in1=tb(TAi, Tidx), op=ALU.mult), reads=srcs + [R_("tmpB")], writes=[R_("tmpB")])
                            k.op("dve", lambda e: e.tensor_tensor(out=Oim[:, :, j, :], in0=tmpA[:, :, j, :], in1=tmpB[:, :, j, :], op=ALU.add), reads=[R_("tmpA"), R_("tmpB")], writes=[R_(onm + "im")])
                    k.op("dve", lambda e: e.tensor_scalar(out=EimN[:], in0=Eim[:], scalar1=-1.0, scalar2=None, op0=ALU.mult), reads=[R_("Eim")], writes=[R_("EimN")])
                    t8 = lambda T_: T_[:, 16, gs].unsqueeze(2).to_broadcast([64, GB, 128])
                    fl = lambda t: t[:].rearrange("s g j q -> s g (j q)")
                    k.op("dve", lambda e: e.tensor_tensor(out=fl(tmpA), in0=fl(Rre), in1=t8(TAr), op=ALU.mult), reads=TA + [R_("Rre"), R_("tmpA")], writes=[R_("tmpA")])
                    k.op("pool", lambda e: e.tensor_tensor(out=fl(tmpB), in0=fl(Rim), in1=t8(TAi), op=ALU.mult), reads=TA + [R_("Rim"), R_("tmpB")], writes=[R_("tmpB")])
                    k.op("dve", lambda e: e.tensor_tensor(out=fl(Wre), in0=fl(tmpA), in1=fl(tmpB), op=ALU.subtract), reads=[R_("tmpA"), R_("tmpB")], writes=[R_("Wre")])
                    k.op("dve", lambda e: e.tensor_tensor(out=fl(tmpA), in0=fl(Rim), in1=t8(TAr), op=ALU.mult), reads=TA + [R_("Rim"), R_("tmpA")], writes=[R_("tmpA")])
                    k.op("pool", lambda e: e.tensor_tensor(out=fl(tmpB), in0=fl(Rre), in1=t8(TAi), op=ALU.mult), reads=TA + [R_("Rre"), R_("tmpB")], writes=[R_("tmpB")])
                    k.op("dve", lambda e: e.scalar_tensor_tensor(out=fl(WimN), in0=fl(tmpA), scalar=-1.0, in1=fl(tmpB), op0=ALU.mult, op1=ALU.subtract), reads=[R_("tmpA"), R_("tmpB")], writes=[R_("WimN")])
                    k.dma("sp", self.Wo[d, 0, :, gs, :], fl(Wre), reads=[R_("Wre")], writes=[("Wo", d, 0, g0)])
                    k.dma("sp", self.Wo[d, 1, :, gs, :], fl(WimN), reads=[R_("WimN")], writes=[("Wo", d, 1, g0)])
                    for gl in range(GB):
                        g = g0 + gl
                        b = self.nps()
                        k.op("pe", lambda e: e.transpose(out=self.ps[b][:, 0:64], in_=Ere[:, gl, :, :].rearrange("s j q -> s (j q)"), identity=self.idt[0:64, 0:64]), reads=[R_("Ere"), "idt"], writes=[("ps", b)])
                        k.op("pe", lambda e: e.transpose(out=self.ps[b][:, 64:128], in_=Eim[:, gl, :, :].rearrange("s j q -> s (j q)"), identity=self.idt[0:64, 0:64]), reads=[R_("Eim"), "idt"], writes=[("ps", b)])
                        k.op("act", lambda e: e.activation(out=wint[0][:, :, gl, :], in_=self.ps[b][:, 0:128].rearrange("p (r s) -> p r s", r=2), func=AF.Identity), reads=[("ps", b)], writes=[R_("wint")])
                        b2 = self.nps()
                        k.op("pe", lambda e: e.matmul(self.ps[b2][:, 0:128], lhsT=Ere[:, gl, :, :].rearrange("s j q -> s (j q)"), rhs=Rre[:, gl, :, :].rearrange("s j q -> s (j q)"), start=True, stop=False), reads=[R_("Ere"), R_("Rre")], writes=[("ps", b2)])
                        k.op("pe", lambda e: e.matmul(self.ps[b2][:, 0:128], lhsT=EimN[:, gl, :, :].rearrange("s j q -> s (j q)"), rhs=Rim[:, gl, :, :].rearrange("s j q -> s (j q)"), start=False, stop=True), reads=[R_("EimN"), R_("Rim")], writes=[("ps", b2)])
                        if d == 0:
                            k.op("dve", lambda e: e.tensor_tensor(out=mt[:], in0=self.ps[b2][:, 0:128], in1=maskf[:], op=ALU.mult), reads=[("ps", b2), "s5maskf"], writes=[R_("mt")])
                            k.op("dve", lambda e: e.scalar_tensor_tensor(out=Macc[:, g, :], in0=self.idt[:, :], scalar=DT[:, g:g + 1], in1=mt[:], op0=ALU.mult, op1=ALU.add), reads=[R_("mt"), "idt", "s5DT"], writes=[("Macc", g)])
                        else:
                            k.op("dve", lambda e: e.tensor_tensor(out=mt[:], in0=self.ps[b2][:, 0:128], in1=maskr[:], op=ALU.mult), reads=[("ps", b2), "s5maskr"], writes=[R_("mt")])
                            k.op("pool", lambda e: e.tensor_tensor(out=Macc[:, g, :], in0=Macc[:, g, :], in1=mt[:], op=ALU.add), reads=[R_("mt"), ("Macc", g)], writes=[("Macc", g)])
                    for ri in range(2):
                        k.dma("sp", self.WinT[d, ri, :, gs, :], wint[0][:, ri, :, :], reads=[R_("wint")], writes=[("WinT", d, ri, g0)])
            k.dma("sp", self.Mm[:, :, :], Macc[:], reads=[("Macc", g) for g in range(64)], writes=["Mm"])
            k.barrier()

    def s5_regs(self):
        r = ["Mm"]
        for d in range(2):
            r.append(("D8", d))
            for ri in range(2):
                for g0 in range(0, 64, 16):
                    r += [("Wo", d, ri, g0), ("WinT", d, ri, g0)]
        return r

    def s5_scan(self, XS, xreg, D8t, cols, tmp, treg, dreg="s5D8t"):
        k = self.k
        for (c, cp) in cols:
            prev = XS[:, :, :, cp]
            k.op("dve", lambda e: e.tensor_tensor(out=tmp[:, 0, 0, :], in0=XS[:, 1, :, cp], in1=D8t[:, 1, 0, :], op=ALU.mult), reads=[xreg, dreg], writes=[(treg, 0)])
            k.op("dve", lambda e: e.tensor_tensor(out=tmp[:, 0, 1, :], in0=XS[:, 0, :, cp], in1=D8t[:, 1, 1, :], op=ALU.mult), reads=[xreg, dreg], writes=[(treg, 0)])
            k.op("dve", lambda e: e.tensor_tensor(out=tmp[:, 1, :, :], in0=prev, in1=D8t[:, 0, :, :], op=ALU.mult), reads=[xreg, dreg], writes=[(treg, 1)])
            k.op("dve", lambda e: e.tensor_tensor(out=XS[:, :, :, c], in0=XS[:, :, :, c], in1=tmp[:, 1, :, :], op=ALU.add), reads=[xreg, (treg, 1)], writes=[xreg])
            k.op("dve", lambda e: e.tensor_tensor(out=XS[:, :, :, c], in0=XS[:, :, :, c], in1=tmp[:, 0, :, :], op=ALU.add), reads=[xreg, (treg, 0)], writes=[xreg])

    def s5_pass1(self, layer, jo):
        k = self.k
        with ExitStack() as es:
            sb = lambda n, sh, dt=F32: self.sb("s5a_" + n, sh, dt, es=es)
            wIn = sb("wIn", [128, KC, D], BF16)
            self.load_w_bf16(wIn, self.od_w_in[jo], ["s5wIn"])
            Wt = self.load_mod(es, 1, "s5_W")
            SHt = self.load_mod(es, 0, "s5_SH")
            ht = [sb("h%d" % i, [128, D]) for i in range(2)]
            at = [sb("a%d" % i, [128, D]) for i in range(2)]
            self.nsc = [(sb("ss%d" % i, [128, 4]), sb("jk%d" % i, [128, D], BF16)) for i in range(2)]
            aT = sb("aT", [128, KC, 512], BF16)
            UU = sb("UU", [64, 64, 8, 16])
            Ublk = sb("Ublk", [128, 64, 64])
            XRb = [sb("XRb%d" % i, [64, 2, 2, 16, 64]) for i in range(2)]
            wt_ = [sb("wt%d" % i, [128, 2, 2, 16, 64]) for i in range(2)]
            bi = 0
            for sti, (t0, n, s) in enumerate(self.stiles()):
                nch = n // 8
                c0 = t0 // 8
                for blk in range(n // 128):
                    i = bi % 2
                    bi += 1
                    r0 = t0 + blk * 128
                    k.dma("sp", ht[i][:], self.h_all[r0:r0 + 128, :], reads=self.hreg(r0, 128), writes=[("s5h", i)])
                    self.tm_norm(ht[i], ("s5h", i), at[i], ("s5a", i), s, Wt, SHt, 128, "s5")
                    self.transpose_block(at[i], ("s5a", i), 128, aT, "s5aT", blk * 128)
                for j in range(8):
                    for half in range(2):
                        b = self.nps()
                        for kc in range(KC):
                            k.op("pe", lambda e: e.matmul(self.ps[b][0:nch, :], lhsT=aT[:, kc, j:n:8], rhs=wIn[:, kc, half * 512:(half + 1) * 512], start=(kc == 0), stop=(kc == KC - 1)),
                                 reads=["s5aT", "s5wIn"], writes=[("ps", b)])
                        k.op("act" if half else "dve", (lambda e: e.activation(out=UU[0:nch, half * 32:(half + 1) * 32, j, :], in_=self.ps[b][0:nch, :].rearrange("c (g q) -> c g q", q=16), func=AF.Identity)) if half else
                             (lambda e: e.tensor_copy(out=UU[0:nch, half * 32:(half + 1) * 32, j, :], in_=self.ps[b][0:nch, :].rearrange("c (g q) -> c g q", q=16))),
                             reads=[("ps", b)], writes=["s5UU"])
                for g0 in range(0, 64, 4):
                    b = self.nps()
                    for gl in range(4):
                        k.op("pe", lambda e: e.transpose(out=self.ps[b][:, gl * 64:gl * 64 + nch], in_=UU[0:nch, g0 + gl, :, :].rearrange("c j q -> c (j q)"), identity=self.idt[0:nch, 0:nch]),
                             reads=["s5UU", "idt"], writes=[("ps", b)])
                    k.op("act" if (g0 // 4) % 2 else "dve", (lambda e: e.activation(out=Ublk[:, g0:g0 + 4, 0:nch], in_=self.ps[b][:, 0:256].rearrange("p (g c) -> p g c", g=4)[:, :, 0:nch], func=AF.Identity)) if (g0 // 4) % 2 else
                         (lambda e: e.tensor_copy(out=Ublk[:, g0:g0 + 4, 0:nch], in_=self.ps[b][:, 0:256].rearrange("p (g c) -> p g c", g=4)[:, :, 0:nch])),
                         reads=[("ps", b)], writes=["s5Ublk"])
                k.dma("sp", self.Ugc[:, :, c0:c0 + nch], Ublk[:, :, 0:nch], reads=["s5Ublk"], writes=[("Ugc", sti)])
                for g0 in range(0, 64, 16):
                    wi = (g0 // 16) % 2
                    for d in range(2):
                        for ri in range(2):
                            k.dma("sp", wt_[wi][:, d, ri, :, :], self.WinT[d, ri, :, g0:g0 + 16, :], reads=self.s5_regs(), writes=[("s5wt", wi)])
                    for d in range(2):
                        for ri in range(2):
                            for g4 in range(0, 16, 4):
                                b = self.nps()
                                for gl in range(4):
                                    g = g0 + g4 + gl
                                    k.op("pe", lambda e: e.matmul(self.ps[b][0:64, gl * 64:gl * 64 + nch], lhsT=wt_[wi][:, d, ri, g4 + gl, :], rhs=Ublk[:, g, 0:nch], start=True, stop=True),
                                         reads=[("s5wt", wi), "s5Ublk"], writes=[("ps", b)])
                                src = self.ps[b][0:64, 0:256].rearrange("p (g c) -> p g c", g=4)[:, :, 0:nch]
                                if (ri + d) % 2 == 0:
                                    k.op("dve", lambda e: e.tensor_copy(out=XRb[wi][:, d, ri, g4:g4 + 4, 0:nch], in_=src), reads=[("ps", b)], writes=[("XRb", wi)])
                                else:
                                    k.op("act", lambda e: e.activation(out=XRb[wi][:, d, ri, g4:g4 + 4, 0:nch], in_=src, func=AF.Identity), reads=[("ps", b)], writes=[("XRb", wi)])
                    k.dma("sp", self.XF[:, :, g0:g0 + 16, c0:c0 + nch], XRb[wi][:, 0, :, :, 0:nch], reads=[("XRb", wi)], writes=[("XF", sti, g0)])
                    k.dma("sp", self.XR[:, :, g0:g0 + 16, c0:c0 + nch], XRb[wi][:, 1, :, :, 0:nch], reads=[("XRb", wi)], writes=[("XR", sti, g0)])
            k.barrier()

    def s5_scans(self):
        k = self.k
        tiles = self.stiles()
        with ExitStack() as es:
            sb = lambda n, sh, dt=F32: self.sb("s5s_" + n, sh, dt, es=es)
            XS = [sb("XS%d" % i, [64, 2, 64, 65]) for i in range(2)]
            D8t = [sb("D8t%d" % d, [64, 2, 2, 64]) for d in range(2)]
            tmp = sb("tmp", [64, 2, 2, 64])
            for d in range(2):
                k.dma("sp", D8t[d][:], self.D8[d], reads=self.s5_regs(), writes=[("s5D8t", d)])
            cnt = 0
            for d in range(2):
                if d == 0:
                    order = list(range(len(tiles)))
                else:
                    order = [ti for ti, t in enumerate(tiles) if t[2] == 1][::-1] + [ti for ti, t in enumerate(tiles) if t[2] == 0][::-1]
                src = self.XF if d == 0 else self.XR
                dst = self.SF if d == 0 else self.SR
                prev = None
                for sti in order:
                    t0, n, s_ = tiles[sti]
                    nch = n // 8
                    c0 = t0 // 8
                    i = cnt % 2
                    cnt += 1
                    X, xr = XS[i], ("s5XS", i)
                    xo = 1 if d == 0 else 0
                    cin = 0 if d == 0 else nch
                    k.dma("sp", X[:, :, :, xo:xo + nch], src[:, :, :, c0:c0 + nch], reads=[("XF" if d == 0 else "XR", sti, g0) for g0 in range(0, 64, 16)], writes=[xr])
                    if prev is None:
                        k.op("pool", lambda e: e.memset(X[:, :, :, cin], 0.0), writes=[xr])
                    else:
                        pX, pxr, pcol = prev
                        k.op("dve", lambda e: e.tensor_copy(out=X[:, :, :, cin], in_=pX[:, :, :, pcol]), reads=[pxr], writes=[xr])
                    if d == 0:
                        cols = [(c + 1, c) for c in range(nch)]
                        prev = (X, xr, nch)
                    else:
                        cols = [(c, c + 1) for c in range(nch - 1, -1, -1)]
                        prev = (X, xr, 0)
                    self.s5_scan(X, xr, D8t[d], cols, tmp, "s5tmp", dreg=("s5D8t", d))
                    io = 0 if d == 0 else 1
                    k.dma("sp", dst[:, :, :, c0:c0 + nch], X[:, :, :, io:io + nch], reads=[xr], writes=[("SF" if d == 0 else "SR", sti)])
            k.barrier()

    def s5_pass2(self, layer, jo, need_ctx):
        k = self.k
        tiles = self.stiles()
        order = [ti for ti, t in enumerate(tiles) if t[2] == 1][::-1] + [ti for ti, t in enumerate(tiles) if t[2] == 0][::-1]
        with ExitStack() as es:
            sb = lambda n, sh, dt=F32: self.sb("s5b_" + n, sh, dt, es=es)
            wo = sb("wo", [128, KC, 2 * D], BF16)
            self.load_w_bf16(wo, self.od_w_out[jo], ["s5wo"])
            G1 = self.load_mod(es, 2, "s5_G1")
            SRb = [sb("SRb%d" % i, [64, 2, 8, 64]) for i in range(2)]
            SFb = [sb("SFb%d" % i, [64, 2, 8, 64]) for i in range(2)]
            Ub = [sb("Ub%d" % i, [128, 8, 64]) for i in range(2)]
            Mt = [sb("Mt%d" % i, [128, 8, 128]) for i in range(2)]
            Wt_ = [sb("Wt%d" % i, [64, 2, 2, 8, 128]) for i in range(2)]
            Yblk = sb("Yblk", [128, 64, 64])
            YT = sb("YT", [64, 8, D])
            catS = sb("catS", [128, KC, 512], BF16)
            ht = [sb("h%d" % i, [64, D]) for i in range(2)]
            tt = [sb("t%d" % i, [64, D]) for i in range(2)]
            sg = [sb("sg%d" % i, [64, 512]) for i in range(2)]
            bi = 0
            for oi, sti in enumerate(order):
                t0, n, s = tiles[sti]
                nch = n // 8
                c0 = t0 // 8
                if s == 1 and not need_ctx:
                    continue
                for g0 in range(0, 64, 8):
                    wi = (g0 // 8) % 2
                    k.dma("sp", SFb[wi][:, :, :, 0:nch], self.SF[:, :, g0:g0 + 8, c0:c0 + nch], reads=[("SF", sti)], writes=[("s5SFb", wi)])
                    k.dma("sp", SRb[wi][:, :, :, 0:nch], self.SR[:, :, g0:g0 + 8, c0:c0 + nch], reads=[("SR", sti)], writes=[("s5SRb", wi)])
                    k.dma("sp", Ub[wi][:, :, 0:nch], self.Ugc[:, g0:g0 + 8, c0:c0 + nch], reads=[("Ugc", sti)], writes=[("s5Ub", wi)])
                    k.dma("sp", Mt[wi][:], self.Mm[:, g0:g0 + 8, :], reads=self.s5_regs(), writes=[("s5Mt", wi)])
                    for d in range(2):
                        for ri in range(2):
                            k.dma("sp", Wt_[wi][:, d, ri, :, :], self.Wo[d, ri, :, g0:g0 + 8, :], reads=self.s5_regs(), writes=[("s5Wt", wi)])
                    for g4 in range(0, 8, 4):
                        b = self.nps()
                        for gl in range(4):
                            g = g0 + g4 + gl
                            o = self.ps[b][:, gl * 64:gl * 64 + nch]
                            k.op("pe", lambda e: e.matmul(o, lhsT=Mt[wi][:, g4 + gl, :], rhs=Ub[wi][:, g4 + gl, 0:nch], start=True, stop=False), reads=[("s5Mt", wi), ("s5Ub", wi)], writes=[("ps", b)])
                            for ri in range(2):
                                k.op("pe", lambda e: e.matmul(o, lhsT=Wt_[wi][:, 0, ri, g4 + gl, :], rhs=SFb[wi][:, ri, g4 + gl, 0:nch], start=False, stop=False), reads=[("s5Wt", wi), ("s5SFb", wi)], writes=[("ps", b)])
                            for ri in range(2):
                                k.op("pe", lambda e: e.matmul(o, lhsT=Wt_[wi][:, 1, ri, g4 + gl, :], rhs=SRb[wi][:, ri, g4 + gl, 0:nch], start=False, stop=(ri == 1)), reads=[("s5Wt", wi), ("s5SRb", wi)], writes=[("ps", b)])
                        k.op("act", lambda e: e.activation(out=Yblk[:, g0 + g4:g0 + g4 + 4, 0:nch], in_=self.ps[b][:, 0:256].rearrange("p (g c) -> p g c", g=4)[:, :, 0:nch], func=AF.Identity), reads=[("ps", b)], writes=["s5Yblk"])
                for g0 in range(0, 64, 4):
                    b = self.nps()
                    for gl in range(4):
                        k.op("pe", lambda e: e.transpose(out=self.ps[b][0:nch, gl * 128:(gl + 1) * 128], in_=Yblk[:, g0 + gl, 0:nch], identity=self.idt[:, :]), reads=["s5Yblk", "idt"], writes=[("ps", b)])
                    k.op("act", lambda e: e.activation(out=YT[0:nch, :, g0 * 16:(g0 + 4) * 16].rearrange("c t (g p) -> c g t p", g=4), in_=self.ps[b][0:nch, :].rearrange("c (g t p) -> c g t p", g=4, t=8), func=AF.Gelu),
                         reads=[("ps", b)], writes=["s5YT"])
                for tau in range(8):
                    self.transpose_block(YT[:, tau, :], "s5YT", nch, catS, "s5cat", tau * nch)
                    i = bi % 2
                    bi += 1
                    rows = self.h_all[t0 + tau:t0 + n:8, :]
                    k.dma("sp", ht[i][0:nch, :], rows, reads=self.hreg(t0, n), writes=[("s5h2", i)])
                    for half in range(2):
                        b = self.nps()
                        b2 = self.nps()
                        for kc in range(KC):
                            k.op("pe", lambda e: e.matmul(self.ps[b][0:nch, :], lhsT=catS[:, kc, tau * nch:(tau + 1) * nch], rhs=wo[:, kc, half * 512:(half + 1) * 512], start=(kc == 0), stop=(kc == KC - 1)),
                                 reads=["s5cat", "s5wo"], writes=[("ps", b)])
                        for kc in range(KC):
                            k.op("pe", lambda e: e.matmul(self.ps[b2][0:nch, :], lhsT=catS[:, kc, tau * nch:(tau + 1) * nch], rhs=wo[:, kc, D + half * 512:D + (half + 1) * 512], start=(kc == 0), stop=(kc == KC - 1)),
                                 reads=["s5cat", "s5wo"], writes=[("ps", b2)])
                        k.op("act", lambda e: e.activation(out=sg[i][0:nch, :], in_=self.ps[b2][0:nch, :], func=AF.Sigmoid), reads=[("ps", b2)], writes=[("s5sg", i)])
                        k.op("dve", lambda e: e.tensor_tensor(out=sg[i][0:nch, :], in0=self.ps[b][0:nch, :], in1=sg[i][0:nch, :], op=ALU.mult), reads=[("ps", b), ("s5sg", i)], writes=[("s5sg", i)])
                        k.op("dve", lambda e: e.tensor_tensor(out=tt[i][0:nch, half * 512:(half + 1) * 512], in0=sg[i][0:nch, :], in1=G1[s][0:nch, half * 512:(half + 1) * 512], op=ALU.mult),
                             reads=[("s5sg", i), ("s5_G1", s)], writes=[("s5t", i)])
                    k.op("pool", lambda e: e.tensor_tensor(out=tt[i][0:nch, :], in0=tt[i][0:nch, :], in1=ht[i][0:nch, :], op=ALU.add), reads=[("s5t", i), ("s5h2", i)], writes=[("s5t", i)])
                    k.dma("sp", rows, tt[i][0:nch, :], reads=[("s5t", i)], writes=self.hreg(t0, n))
            k.barrier()

    def moe_layer(self, layer, need_ctx):
        k = self.k
        M, T, TT = self.M, self.T, self.TT
        capL, capC = self.capL, self.capC
        nLb = capL // 128
        with ExitStack() as es:
            G2 = self.load_mod(es, 5, "mz_g2")
            nLb_ = capL // 128
            idxT = self.sb("mz_idxT", [128, nLb_ + 1, NE], U32, es=es)
            gateT = self.sb("mz_gateT", [128, nLb_ + 1, NE], es=es)
            tk_es = ExitStack()
            AFF = self.sb("mz_aff", [NE, TT], es=tk_es)
            with ExitStack() as es2:
                Wt = self.load_mod(es2, 4, "mz_W")
                SHt = self.load_mod(es2, 3, "mz_SH")
                rw = self.sb("mz_rw", [128, KC, NE], es=es2)
                k.dma("sp", rw[:], self.router_w[layer].rearrange("(kc p) e -> p kc e", p=128), writes=["mz_rw"])
                ht = [self.sb("mz_h%d" % i, [128, D], es=es2) for i in range(2)]
                at = [self.sb("mz_a%d" % i, [128, D], es=es2) for i in range(2)]
                self.nsc = [(self.sb("mz_ss%d" % i, [128, 4], es=es2), self.sb("mz_jk%d" % i, [128, D], BF16, es=es2)) for i in range(2)]
                h2T = [self.sb("mz_h2T%d" % i, [128, KC, 512], es=es2) for i in range(2)]
                ex = [self.sb("mz_ex%d" % i, [NE, 512], es=es2) for i in range(2)]
                rs = [self.sb("mz_rs%d" % i, [NE, 512], es=es2) for i in range(2)]
                bi = 0
                for sti, (t0, n, s) in enumerate(self.stiles()):
                    if s == 1 and not need_ctx:
                        continue
                    ai = sti % 2
                    for blk in range(n // 128):
                        i = bi % 2
                        bi += 1
                        r0 = t0 + blk * 128
                        k.dma("sp", ht[i][:], self.h_all[r0:r0 + 128, :], reads=self.hreg(r0, 128), writes=[("mz_h", i)])
                        self.tm_norm(ht[i], ("mz_h", i), at[i], ("mz_a", i), s, Wt, SHt, 128, "mz")
                        k.dma("sp", self.h2rows[r0:r0 + 128, :], at[i][:, :], reads=[("mz_a", i)], writes=[("h2rows", r0 // 128)])
                        self.transpose_block(at[i], ("mz_a", i), 128, h2T[ai], ("mz_h2T", ai), blk * 128, evac="dve" if blk % 2 else "act")
                    b = self.nps()
                    for kc in range(KC):
                        k.op("pe", lambda e: e.matmul(self.ps[b][0:NE, 0:n], lhsT=rw[:, kc, :], rhs=h2T[ai][:, kc, 0:n], start=(kc == 0), stop=(kc == KC - 1)),
                             reads=["mz_rw", ("mz_h2T", ai)], writes=[("ps", b)])
                    k.op("act", lambda e: e.activation(out=ex[ai][:, 0:n], in_=self.ps[b][0:NE, 0:n], func=AF.Exp), reads=[("ps", b)], writes=[("mz_ex", ai)])
                    b2 = self.nps()
                    k.op("pe", lambda e: e.matmul(self.ps[b2][0:NE, 0:n], lhsT=self.ones[0:NE, 0:NE], rhs=ex[ai][:, 0:n], start=True, stop=True), reads=["ones", ("mz_ex", ai)], writes=[("ps", b2)])
                    k.op("dve", lambda e: e.reciprocal(out=rs[ai][:, 0:n], in_=self.ps[b2][0:NE, 0:n]), reads=[("ps", b2)], writes=[("mz_rs", ai)])
                    k.op("dve", lambda e: e.tensor_tensor(out=AFF[:, t0:t0 + n], in0=ex[ai][:, 0:n], in1=rs[ai][:, 0:n], op=ALU.mult), reads=[("mz_ex", ai), ("mz_rs", ai)], writes=["mz_aff"])
                k.barrier()
            ncap = capL + (capC if need_ctx else 0)
            nsb = nLb + (1 if need_ctx else 0)
            mx = self.sb("mz_mx", [NE, capL + capC], es=tk_es)
            mi = self.sb("mz_mi", [NE, capL + capC], U32, es=tk_es)
            mf = self.sb("mz_mf", [NE, capL + capC], es=tk_es)
            segs = [(M, T, 0, capL)] + ([(0, M, capL, capC)] if need_ctx else [])
            for (c0, cn, s0, cap) in segs:
                for r in range(cap // 8):
                    sl = slice(s0 + r * 8, s0 + r * 8 + 8)
                    k.op("dve", lambda e: e.max(out=mx[:, sl], in_=AFF[:, c0:c0 + cn]), reads=["mz_aff"], writes=["mz_mx"])
                    k.op("dve", lambda e: e.max_index(out=mi[:, sl], in_max=mx[:, sl], in_values=AFF[:, c0:c0 + cn]), reads=["mz_aff", "mz_mx"], writes=["mz_mi"])
                    k.op("dve", lambda e: e.match_replace(out=AFF[:, c0:c0 + cn], in_to_replace=mx[:, sl], in_values=AFF[:, c0:c0 + cn], imm_value=-1.0), reads=["mz_aff", "mz_mx", "mz_mi"], writes=["mz_aff"])
                k.op("dve", lambda e: e.tensor_copy(out=mf[:, s0:s0 + cap], in_=mi[:, s0:s0 + cap]), reads=["mz_mi"], writes=["mz_mf"])
                k.op("dve", lambda e: e.tensor_scalar(out=mf[:, s0:s0 + cap], in0=mf[:, s0:s0 + cap], scalar1=float(c0), scalar2=None, op0=ALU.add), reads=["mz_mf"], writes=["mz_mf"])
            for sbk in range(nsb):
                s0 = sbk * 128
                nn = 128 if sbk < nLb else capC
                b = self.nps()
                k.op("pe", lambda e: e.transpose(out=self.ps[b][0:nn, 0:NE], in_=mf[:, s0:s0 + nn], identity=self.idt[0:NE, 0:NE]), reads=["mz_mf", "idt"], writes=[("ps", b)])
                k.op("pe", lambda e: e.transpose(out=self.ps[b][0:nn, NE:2 * NE], in_=mx[:, s0:s0 + nn], identity=self.idt[0:NE, 0:NE]), reads=["mz_mx", "idt"], writes=[("ps", b)])
                k.op("dve", lambda e: e.tensor_copy(out=idxT[0:nn, sbk, :], in_=self.ps[b][0:nn, 0:NE]), reads=[("ps", b)], writes=["mz_idxT"])
                k.op("dve", lambda e: e.tensor_copy(out=gateT[0:nn, sbk, :], in_=self.ps[b][0:nn, NE:2 * NE]), reads=[("ps", b)], writes=["mz_gateT"])
            k.barrier()
            tk_es.close()
            wg = self.sb("mz_wg", [128, KC, DFF], BF16, es=es)
            wu = self.sb("mz_wu", [128, KC, DFF], BF16, es=es)
            wd = self.sb("mz_wd", [128, NFC, D], BF16, es=es)
            xg = [self.sb("mz_xg%d" % i, [128, D], es=es) for i in range(2)]
            xeT = [self.sb("mz_xeT%d" % i, [128, KC, 512], BF16, es=es) for i in range(1)]
            actT = self.sb("mz_act", [128, NFC, 512], BF16, es=es)
            sl_ = [self.sb("mz_sl%d" % i, [128, 512], es=es) for i in range(2)]
            ye = [self.sb("mz_ye%d" % i, [128, D], es=es) for i in range(2)]
            allh = [("h", b_) for b_ in range(self.NB)]
            allh2 = [("h2rows", b_) for b_ in range(self.NB)]
            blocks = [(sbk, 128, 0) for sbk in range(nLb)] + ([(nLb, capC, 1)] if need_ctx else [])
            subs = []
            if need_ctx:
                subs.append([blocks[-1]])
            for i0 in range(0, nLb, 4):
                subs.append(blocks[i0:min(i0 + 4, nLb)])
            gcnt = 0
            ycnt = 0
            scnt = 0
            for ex_ in range(NE):
                for kc in range(KC):
                    k.dma("pool", wg[:, kc, :], self.w_gate[layer, ex_, kc * 128:(kc + 1) * 128, :], writes=[("mz_wg", kc)], max_dma_last_dim=4096)
                    k.dma("pool", wu[:, kc, :], self.w_up[layer, ex_, kc * 128:(kc + 1) * 128, :], writes=[("mz_wu", kc)], max_dma_last_dim=4096)
                for fc in range(NFC):
                    fsz = min(128, DFF - fc * 128)
                    k.dma("pool", wd[0:fsz, fc, :], self.w_down[layer, ex_, fc * 128:fc * 128 + fsz, :], writes=[("mz_wd", fc)], max_dma_last_dim=4096)
                for sub in subs:
                    xi = 0
                    scnt += 1
                    ntok = sum(nn for (_, nn, _) in sub)
                    col = 0
                    cols = []
                    for (sbk, nn, s) in sub:
                        gi = gcnt % 2
                        gcnt += 1
                        k.dma("pool", reads=["mz_idxT"] + allh2, writes=[("mz_xg", gi)],
                              fn=lambda e: e.indirect_dma_start(out=xg[gi][0:nn, :], out_offset=None, in_=self.h2rows[:, :],
                                                                in_offset=bass.IndirectOffsetOnAxis(ap=idxT[0:nn, sbk, ex_:ex_ + 1], axis=0)))
                        self.transpose_block(xg[gi], ("mz_xg", gi), nn, xeT[xi], ("mz_xeT", xi), col, evac="act")
                        cols.append(col)
                        col += nn
                    for fc in range(NFC):
                        fsz = min(128, DFF - fc * 128)
                        bg = self.nps()
                        for kc in range(KC):
                            k.op("pe", lambda e: e.matmul(self.ps[bg][0:fsz, 0:ntok], lhsT=wg[:, kc, fc * 128:fc * 128 + fsz], rhs=xeT[xi][:, kc, 0:ntok], start=(kc == 0), stop=(kc == KC - 1)),
                                 reads=[("mz_wg", kc), ("mz_xeT", xi)], writes=[("ps", bg)])
                        bu = self.nps()
                        for kc in range(KC):
                            k.op("pe", lambda e: e.matmul(self.ps[bu][0:fsz, 0:ntok], lhsT=wu[:, kc, fc * 128:fc * 128 + fsz], rhs=xeT[xi][:, kc, 0:ntok], start=(kc == 0), stop=(kc == KC - 1)),
                                 reads=[("mz_wu", kc), ("mz_xeT", xi)], writes=[("ps", bu)])
                        si = fc % 2
                        k.op("act", lambda e: e.activation(out=sl_[si][0:fsz, 0:ntok], in_=self.ps[bg][0:fsz, 0:ntok], func=AF.Silu), reads=[("ps", bg)], writes=[("mz_sl", si)])
                        k.op("dve", lambda e: e.tensor_tensor(out=actT[0:fsz, fc, 0:ntok], in0=self.ps[bu][0:fsz, 0:ntok], in1=sl_[si][0:fsz, 0:ntok], op=ALU.mult),
                             reads=[("ps", bu), ("mz_sl", si)], writes=["mz_act"])
                    for bi_, (sbk, nn, s) in enumerate(sub):
                        yi = ycnt % 2
                        ycnt += 1
                        c0 = cols[bi_]
                        for half in range(2):
                            b = self.nps()
                            for fc in range(NFC):
                                fsz = min(128, DFF - fc * 128)
                                k.op("pe", lambda e: e.matmul(self.ps[b][0:nn, :], lhsT=actT[0:fsz, fc, c0:c0 + nn], rhs=wd[0:fsz, fc, half * 512:(half + 1) * 512], start=(fc == 0), stop=(fc == NFC - 1)),
                                     reads=["mz_act", ("mz_wd", fc)], writes=[("ps", b)])
                            k.op("dve", lambda e: e.scalar_tensor_tensor(out=ye[yi][0:nn, half * 512:(half + 1) * 512], in0=self.ps[b][0:nn, :], scalar=gateT[0:nn, sbk, ex_:ex_ + 1],
                                                                         in1=G2[s][0:nn, half * 512:(half + 1) * 512], op0=ALU.mult, op1=ALU.mult),
                                 reads=[("ps", b), "mz_gateT", ("mz_g2", s)], writes=[("mz_ye", yi)])
                        k.dma("pool", reads=["mz_idxT", ("mz_ye", yi)], writes=allh,
                              fn=lambda e: e.indirect_dma_start(out=self.h_all[:, :], out_offset=bass.IndirectOffsetOnAxis(ap=idxT[0:nn, sbk, ex_:ex_ + 1], axis=0),
                                                                in_=ye[yi][0:nn, :], in_offset=None, compute_op=ALU.add))
            k.barrier()


def host_consts(T, M):
    TT = T + M
    ident = np.eye(128, dtype=np.float32)
    pos96 = np.zeros((96, TT), np.float32)
    t = np.arange(T)
    row = (t // 64).astype(np.float32)
    colp = (t % 64).astype(np.float32)
    inv96 = np.zeros((96, 1), np.float32)
    inv = (10000.0 ** (-np.arange(8, dtype=np.float32) / 8)).astype(np.float32)
    for p in range(64, 96):
        pair = (p - 64) % 16
        pos96[p, M:] = row if pair < 8 else colp
        inv96[p, 0] = inv[pair % 8]
    permT = np.zeros((96, 96), np.float32)
    for m in range(64, 80):
        permT[m + 16, m] = -1.0
    for m in range(80, 96):
        permT[m - 16, m] = 1.0
    selpe = np.zeros((32, 96), np.float32)
    for i in range(32):
        selpe[i, 64 + i] = 1.0
    band = np.zeros((2, 128, 32), np.float32)
    for r in range(128):
        jr = (r % 32) // 16
        for c in range(32):
            jc = c // 16
            band[0, r, c] = 1.0 if jc >= jr else 0.0
            band[1, r, c] = 1.0 if jr >= jc else 0.0
    return dict(ident=ident, pos96=pos96, inv96=inv96, permT=permT, selpe=selpe, band=band)


WEIGHT_KEYS = ["ada_w", "ada_b", "norm1_g", "norm2_g", "router_w", "exp_w_gate", "exp_w_up", "exp_w_down", "ev_w_in", "pool_w",
               "pool_scale", "q_a_norm_g", "w_uq", "kv_a_norm_g", "w_ukv", "q_norm_g", "k_norm_g", "ev_w_out", "od_w_in",
               "s5_lam_re", "s5_lam_im", "s5_log_dt", "s5_b_re", "s5_b_im", "s5_c_re", "s5_c_im", "s5_d", "od_w_out"]


def run(inputs, depth=4, dbg=(), ncores=None):
    x = np.asarray(inputs["x"], np.float32)
    ctx = np.asarray(inputs["ctx"], np.float32)
    B, T, _ = x.shape
    M = ctx.shape[1]
    prog = Prog(T, M, depth, dbg)
    nc = prog.build()
    consts = host_consts(T, M)
    EVK = ["ev_w_in", "pool_w", "pool_scale", "q_a_norm_g", "w_uq", "kv_a_norm_g", "w_ukv", "q_norm_g", "k_norm_g", "ev_w_out"]
    shared = {}
    for kk in WEIGHT_KEYS:
        a = np.asarray(inputs[kk], np.float32)
        nlead = (depth + 1) // 2 if kk in EVK else (max(1, depth // 2) if (kk.startswith("s5_") or kk.startswith("od_")) else depth)
        shared[kk] = np.ascontiguousarray(a[:nlead])
    shared.update(consts)
    c = np.asarray(inputs["c"], np.float32)
    cc = np.asarray(inputs["c_ctx"], np.float32)
    in_maps = []
    for b in range(B):
        m = dict(shared)
        m["x"] = np.ascontiguousarray(x[b])
        m["ctx"] = np.ascontiguousarray(ctx[b])
        m["cfm"] = np.ascontiguousarray(np.concatenate([c[b].reshape(KC, 128).T, cc.reshape(KC, 128).T], axis=1))
        in_maps.append(m)
    ncore = len(in_maps) if ncores is None else ncores
    res = run_bass_kernel_spmd(nc, in_maps[:ncore], core_ids=list(range(ncore)))
    out = np.stack([np.asarray(r["y"]) for r in res.results], axis=0).astype(np.float32)
    return out, res, prog


def kernel(**inputs):
    out, _, _ = run(inputs, depth=4)
    return out
```

```python
import math
import numpy as np
from contextlib import ExitStack
import concourse.bass as bass
import concourse.mybir as mybir
from concourse.bass_utils import run_bass_kernel_spmd

F32 = mybir.dt.float32
BF16 = mybir.dt.bfloat16
I32 = mybir.dt.int32
U32 = mybir.dt.uint32
AF = mybir.ActivationFunctionType
ALU = mybir.AluOpType

D = 1024
KC = 8
NE = 16
DFF = 2752
NFC = 22
EPS = 1e-6
TWO_PI = 2.0 * math.pi


class K:
    def __init__(self, nc, es, n_dma_sems=8):
        self.nc = nc
        self.es = es
        self.eng = {"pe": nc.tensor, "act": nc.scalar, "dve": nc.vector, "pool": nc.gpsimd, "sp": nc.sync}
        self.csem, self.ccnt, self.cgen = {}, {}, {}
        for e in ("pe", "act", "dve", "pool"):
            self.cgen[e] = 0
            self.csem[e] = es.enter_context(nc.semaphore("c_%s_0" % e))
            self.ccnt[e] = 0
        self.dsem, self.dcnt, self.dnext, self.dgen = {}, {}, {}, {}
        for q in ("sp", "act", "pool"):
            self.dsem[q] = [es.enter_context(nc.semaphore("d_%s%d_0" % (q, i))) for i in range(n_dma_sems)]
            self.dcnt[q] = [0] * n_dma_sems
            self.dgen[q] = [0] * n_dma_sems
            self.dnext[q] = 0
        self.waited = {e: {} for e in self.eng}
        self.lastw = {}
        self.readers = {}
        self.ninstr = 0

    def _wait(self, e, tok):
        if tok is None:
            return
        sem, val, key = tok
        if self.waited[e].get(key, 0) >= val:
            return
        self.eng[e].wait_ge(sem, val)
        self.waited[e][key] = val

    def _deps(self, e, reads, writes):
        for r in reads:
            self._wait(e, self.lastw.get(r))
        for w in writes:
            self._wait(e, self.lastw.get(w))
            for t in self.readers.get(w, ()):
                self._wait(e, t)

    def _record(self, tok, reads, writes):
        for r in reads:
            lst = self.readers.setdefault(r, [])
            lst.append(tok)
            if len(lst) > 24:
                d = {}
                for t in lst:
                    if t[2] not in d or d[t[2]][1] < t[1]:
                        d[t[2]] = t
                self.readers[r] = list(d.values())
        for w in writes:
            self.lastw[w] = tok
            self.readers[w] = []

    def op(self, e, fn, reads=(), writes=()):
        self._deps(e, reads, writes)
        if self.ccnt[e] >= 30000:
            self.cgen[e] += 1
            self.csem[e] = self.es.enter_context(self.nc.semaphore("c_%s_%d" % (e, self.cgen[e])))
            self.ccnt[e] = 0
        ins = fn(self.eng[e])
        self.ccnt[e] += 1
        ins.then_inc(self.csem[e], 1)
        tok = (self.csem[e], self.ccnt[e], "c_%s_%d" % (e, self.cgen[e]))
        self._record(tok, reads, writes)
        self.ninstr += 1
        return tok

    def dma(self, q, out=None, in_=None, reads=(), writes=(), fn=None, **kw):
        self._deps(q, reads, writes)
        i = self.dnext[q]
        self.dnext[q] = (i + 1) % len(self.dsem[q])
        if self.dcnt[q][i] >= 30000:
            self._wait(q, (self.dsem[q][i], self.dcnt[q][i], "d_%s%d_%d" % (q, i, self.dgen[q][i])))
            self.dgen[q][i] += 1
            self.dsem[q][i] = self.es.enter_context(self.nc.semaphore("d_%s%d_%d" % (q, i, self.dgen[q][i])))
            self.dcnt[q][i] = 0
        sem = self.dsem[q][i]
        key = "d_%s%d_%d" % (q, i, self.dgen[q][i])
        if self.dcnt[q][i] > 0:
            self._wait(q, (sem, self.dcnt[q][i], key))
        if fn is not None:
            ins = fn(self.eng[q])
        else:
            ins = self.eng[q].dma_start(out=out, in_=in_, **kw)
        self.dcnt[q][i] += 16
        ins.then_inc(sem, 16)
        tok = (sem, self.dcnt[q][i], key)
        self._record(tok, reads, writes)
        self.ninstr += 1
        return tok

    def barrier(self):
        toks = []
        for e in ("pe", "act", "dve", "pool"):
            if self.ccnt[e] > 0:
                toks.append((self.csem[e], self.ccnt[e], "c_%s_%d" % (e, self.cgen[e])))
        for q in ("sp", "act", "pool"):
            for i in range(len(self.dsem[q])):
                if self.dcnt[q][i] > 0:
                    toks.append((self.dsem[q][i], self.dcnt[q][i], "d_%s%d_%d" % (q, i, self.dgen[q][i])))
        for e in self.eng:
            for t in toks:
                if not (t[2].startswith("c_" + e + "_")):
                    self._wait(e, t)

    def wait_all(self, e):
        for r, t in list(self.lastw.items()):
            self._wait(e, t)


class Prog:
    def __init__(self, T, M, depth, dbg=()):
        self.T, self.M, self.depth = T, M, depth
        self.TT = T + M
        self.NB = self.TT // 128
        self.dbg = dbg
        self.capL = 2 * T // NE
        self.capC = 2 * M // NE
        nc = self.nc = bass.Bass("TRN2", target_bir_lowering=False)
        self.es = ExitStack()
        self.k = K(nc, self.es)
        self._uid = 0

        def din(name, shape, dt=F32):
            return nc.dram_tensor(name, list(shape), dt, kind="ExternalInput").ap()

        self.din = din
        TT = self.TT
        self.x = din("x", [T, D])
        self.ctx = din("ctx", [M, D])
        self.cfm = din("cfm", [128, 2 * KC])
        self.ident = din("ident", [128, 128])
        self.pos96 = din("pos96", [96, TT])
        self.inv96 = din("inv96", [96, 1])
        self.permT = din("permT", [96, 96])
        self.selpe = din("selpe", [32, 96])
        self.band = din("band", [2, 128, 32])
        nl = depth
        ne_ = (depth + 1) // 2
        no_ = max(1, depth // 2)
        self.ada_w = din("ada_w", [nl, D, 6 * D])
        self.ada_b = din("ada_b", [nl, 6 * D])
        self.norm1_g = din("norm1_g", [nl, D])
        self.norm2_g = din("norm2_g", [nl, D])
        self.router_w = din("router_w", [nl, D, NE])
        self.w_gate = din("exp_w_gate", [nl, NE, D, DFF])
        self.w_up = din("exp_w_up", [nl, NE, D, DFF])
        self.w_down = din("exp_w_down", [nl, NE, DFF, D])
        self.ev_w_in = din("ev_w_in", [ne_, D, 928])
        self.pool_w = din("pool_w", [ne_, 4, 128, 128])
        self.pool_scale = din("pool_scale", [ne_, 512])
        self.qa_g = din("q_a_norm_g", [ne_, 256])
        self.w_uq = din("w_uq", [ne_, 256, 768])
        self.kva_g = din("kv_a_norm_g", [ne_, 128])
        self.w_ukv = din("w_ukv", [ne_, 128, 1024])
        self.qn_g = din("q_norm_g", [ne_, 96])
        self.kn_g = din("k_norm_g", [ne_, 96])
        self.ev_w_out = din("ev_w_out", [ne_, D, D])
        self.od_w_in = din("od_w_in", [no_, D, D])
        self.s5_lam_re = din("s5_lam_re", [no_, 2, 64, 64])
        self.s5_lam_im = din("s5_lam_im", [no_, 2, 64, 64])
        self.s5_log_dt = din("s5_log_dt", [no_, 2, 64])
        self.s5_b_re = din("s5_b_re", [no_, 2, 64, 64, 16])
        self.s5_b_im = din("s5_b_im", [no_, 2, 64, 64, 16])
        self.s5_c_re = din("s5_c_re", [no_, 2, 64, 16, 64])
        self.s5_c_im = din("s5_c_im", [no_, 2, 64, 16, 64])
        self.s5_d = din("s5_d", [no_, D])
        self.od_w_out = din("od_w_out", [no_, D, 2 * D])
        self.y = nc.dram_tensor("y", [T, D], F32, kind="ExternalOutput").ap()

        def scr(name, shape, dt=F32):
            return nc.dram_tensor(name, list(shape), dt, kind=("ExternalOutput" if name in dbg else "Internal")).ap()

        self.scr = scr
        self.h_all = scr("h_all", [TT, D])
        self.h2rows = scr("h2rows", [TT, D])
        self.poolT = scr("poolT", [512, TT])
        self.catT = scr("catT", [D, TT], BF16)
        self.qT = scr("qT", [8, 96, TT], BF16)
        self.kT = scr("kT", [8, 96, TT], BF16)
        self.vR = scr("vR", [8, TT, 64], BF16)
        self.attU = scr("attU", [8, 64, TT])
        self.asum = scr("asum", [8, TT])
        self.cosT = scr("cosT", [96, TT])
        self.sinT = scr("sinT", [96, TT])
        self.dbg_out = {}

        self.P = self.es
        self.idt = self.sb("idt", [128, 128])
        self.ones = self.sb("ones", [128, 128])
        self.cf = self.sb("cf", [128, 2 * KC])
        self.modrows = scr("modrows", [2, 6 * D])
        self.epsT = self.sb("epsT", [128, 1])
        self.ps = [self.es.enter_context(nc.psum_tensor("ps%d" % i, [128, 512], F32)) for i in range(8)]
        self.psi = 0

    def sb(self, name, shape, dt=F32, es=None):
        self._uid += 1
        return (es or self.es).enter_context(self.nc.sbuf_tensor("%s_u%d" % (name, self._uid), list(shape), dt))

    def dbg_tensor(self, name, shape, dt=F32):
        t = self.nc.dram_tensor("dbg_" + name, list(shape), dt, kind="ExternalOutput").ap()
        self.dbg_out[name] = t
        return t

    def nps(self, lo=0, hi=8):
        i = lo + (self.psi % (hi - lo))
        self.psi += 1
        return i

    def hreg(self, t0, n):
        return [("h", b) for b in range(t0 // 128, (t0 + n + 127) // 128)]

    def col_load(self, dst, src1d, p, writes):
        ncol = src1d.shape[0] // p
        for c in range(ncol):
            self.k.dma("sp", dst[0:p, c:c + 1], src1d[c * p:(c + 1) * p].rearrange("(p o) -> p o", o=1), writes=writes)

    def bcast_row(self, q, dst, row_ap, n, writes):
        self.k.dma(q, dst, row_ap.partition_broadcast(dst.shape[0]), writes=writes)

    def build(self):
        k, nc = self.k, self.nc
        T, M, TT = self.T, self.M, self.TT
        k.dma("sp", self.idt[:], self.ident[:, :], writes=["idt"])
        k.op("pool", lambda e: e.memset(self.ones[:], 1.0), writes=["ones"])
        k.op("pool", lambda e: e.memset(self.epsT[:], EPS), writes=["epsT"])
        k.dma("sp", self.h_all[0:M, :], self.ctx[:, :], writes=self.hreg(0, M))
        nch = max(1, T // 1024)
        for i in range(nch):
            n = T // nch
            k.dma("sp", self.h_all[M + i * n:M + (i + 1) * n, :], self.x[i * n:(i + 1) * n, :], writes=self.hreg(M + i * n, n))
        with ExitStack() as es:
            cf = self.cf
            sg = self.sb("cf_sg", [128, 2 * KC], es=es)
            k.dma("sp", cf[:], self.cfm[:, :], writes=["cf"])
            k.op("act", lambda e: e.activation(out=sg[:], in_=cf[:], func=AF.Sigmoid), reads=["cf"], writes=["cf_sg"])
            k.op("dve", lambda e: e.tensor_tensor(out=cf[:], in0=cf[:], in1=sg[:], op=ALU.mult), reads=["cf", "cf_sg"], writes=["cf"])
            self.rope_tables(es)
            k.barrier()
        for layer in range(self.depth):
            need_ctx = layer < self.depth - 1
            if True:
                self.adaln(layer)
                if layer % 2 == 0:
                    self.even_layer(layer, need_ctx)
                else:
                    self.odd_layer(layer, need_ctx)
                self.moe_layer(layer, need_ctx)
        nch = max(1, T // 1024)
        for i in range(nch):
            n = T // nch
            k.dma("sp", self.y[i * n:(i + 1) * n, :], self.h_all[M + i * n:M + (i + 1) * n, :], reads=self.hreg(M + i * n, n), writes=[("y", i)])
        for name, (src, reg) in getattr(self, "dbg_copies", {}).items():
            pass
        k.wait_all("sp")
        self.es.close()
        return nc

    def rope_tables(self, es0):
        k = self.k
        TT = self.TT
        with ExitStack() as es:
            W = 1024
            inv = self.sb("rp_inv", [96, 1], es=es)
            k.dma("sp", inv[:], self.inv96[:, :], writes=["rp_inv"])
            tl = [(self.sb("rp_a%d" % i, [96, W], es=es), self.sb("rp_ai%d" % i, [96, W], I32, es=es), self.sb("rp_b%d" % i, [96, W], es=es), self.sb("rp_c%d" % i, [96, W], es=es)) for i in range(2)]
            for ti_, t0 in enumerate(range(0, TT, W)):
                n = min(W, TT - t0)
                a, ai, b, c = tl[ti_ % 2]
                ra, rai, rb, rc = ("rpa", ti_ % 2), ("rpai", ti_ % 2), ("rpb", ti_ % 2), ("rpc", ti_ % 2)
                k.dma("sp", a[:, 0:n], self.pos96[:, t0:t0 + n], writes=[ra])
                k.op("dve", lambda e: e.tensor_scalar(out=a[:, 0:n], in0=a[:, 0:n], scalar1=inv[:, 0:1], scalar2=None, op0=ALU.mult), reads=[ra, "rp_inv"], writes=[ra])
                k.op("dve", lambda e: e.tensor_scalar(out=b[:, 0:n], in0=a[:, 0:n], scalar1=1.0 / TWO_PI, scalar2=None, op0=ALU.mult), reads=[ra], writes=[rb])
                k.op("dve", lambda e: e.tensor_copy(out=ai[:, 0:n], in_=b[:, 0:n]), reads=[rb], writes=[rai])
                k.op("dve", lambda e: e.tensor_copy(out=b[:, 0:n], in_=ai[:, 0:n]), reads=[rai], writes=[rb])
                k.op("dve", lambda e: e.scalar_tensor_tensor(out=a[:, 0:n], in0=b[:, 0:n], scalar=-TWO_PI, in1=a[:, 0:n], op0=ALU.mult, op1=ALU.add), reads=[ra, rb], writes=[ra])
                k.op("dve", lambda e: e.tensor_scalar(out=a[:, 0:n], in0=a[:, 0:n], scalar1=math.pi, scalar2=-math.pi, op0=ALU.min, op1=ALU.max), reads=[ra], writes=[ra])
                k.op("act", lambda e: e.activation(out=b[:, 0:n], in_=a[:, 0:n], func=AF.Sin), reads=[ra, rb], writes=[rb])
                k.dma("sp", self.sinT[:, t0:t0 + n], b[:, 0:n], reads=[rb], writes=[("sinT", t0)])
                k.op("dve", lambda e: e.scalar_tensor_tensor(out=c[:, 0:n], in0=a[:, 0:n], scalar=-1.0, in1=a[:, 0:n], op0=ALU.mult, op1=ALU.max), reads=[ra], writes=[rc])
                k.op("dve", lambda e: e.tensor_scalar(out=c[:, 0:n], in0=c[:, 0:n], scalar1=-1.0, scalar2=math.pi / 2, op0=ALU.mult, op1=ALU.add), reads=[rc], writes=[rc])
                k.op("act", lambda e: e.activation(out=c[:, 0:n], in_=c[:, 0:n], func=AF.Sin), reads=[rc], writes=[rc])
                k.dma("sp", self.cosT[:, t0:t0 + n], c[:, 0:n], reads=[rc], writes=[("cosT", t0)])
            k.barrier()
            self.rope_regs = [("sinT", t0) for t0 in range(0, TT, W)] + [("cosT", t0) for t0 in range(0, TT, W)]

    def adaln(self, layer):
        k = self.k
        with ExitStack() as es:
            wt = [self.sb("adaw_%d" % i, [128, KC, 512], es=es) for i in range(2)]
            bt = self.sb("adab", [2, 6 * D], es=es)
            mod = self.sb("adamod", [2, 6 * D], es=es)
            g1 = self.sb("ada_g1", [2, D], es=es)
            g2 = self.sb("ada_g2", [2, D], es=es)
            k.dma("sp", bt[:], self.ada_b[layer, :].partition_broadcast(2), writes=["adab"])
            k.dma("sp", g1[:], self.norm1_g[layer, :].partition_broadcast(2), writes=["ada_g"])
            k.dma("sp", g2[:], self.norm2_g[layer, :].partition_broadcast(2), writes=["ada_g"])
            for nb in range(12):
                i = nb % 2
                c0 = nb * 512
                k.dma("pool" if nb % 2 else "sp", wt[i][:], self.ada_w[layer, :, c0:c0 + 512].rearrange("(kc p) n -> p kc n", p=128), writes=[("adaw", i)])
                b = self.nps()
                for kc in range(KC):
                    k.op("pe", lambda e: e.matmul(self.ps[b][0:2, :], lhsT=self.cf[:, kc:2 * KC:KC], rhs=wt[i][:, kc, :], start=(kc == 0), stop=(kc == KC - 1)),
                         reads=["cf", ("adaw", i)], writes=[("ps", b)])
                k.op("dve", lambda e: e.tensor_tensor(out=mod[:, c0:c0 + 512], in0=self.ps[b][0:2, :], in1=bt[:, c0:c0 + 512], op=ALU.add),
                     reads=[("ps", b), "adab"], writes=["adamod"])
            for (gt, c0) in ((g1, D), (g2, 4 * D)):
                k.op("dve", lambda e: e.scalar_tensor_tensor(out=mod[:, c0:c0 + D], in0=mod[:, c0:c0 + D], scalar=1.0, in1=gt[:], op0=ALU.add, op1=ALU.mult),
                     reads=["adamod", "ada_g"], writes=["adamod"])
            k.dma("sp", self.modrows[:, :], mod[:], reads=["adamod"], writes=["modrows"])
            k.barrier()

    def load_mod(self, es, chunk, name):
        out = []
        for s in range(2):
            t = self.sb("%s_%d" % (name, s), [128, D], es=es)
            self.k.dma("sp", t[:], self.modrows[s, chunk * D:(chunk + 1) * D].partition_broadcast(128), reads=["modrows"], writes=[(name, s)])
            out.append(t)
        return out

    def tm_norm(self, ht, hreg, at, areg, s, Wt, SHt, n, tag):
        k = self.k
        i = self.nsc_i = (getattr(self, "nsc_i", 0) + 1) % len(self.nsc)
        ss, junk = self.nsc[i]
        rs, rj = ("nss", i), ("nj", i)
        k.op("act", lambda e: e.activation(out=junk[0:n, :], in_=ht[0:n, :], func=AF.Square, accum_out=ss[0:n, 0:1]), reads=[hreg], writes=[rs, rj])
        k.op("dve", lambda e: e.tensor_scalar(out=ss[0:n, 1:2], in0=ss[0:n, 0:1], scalar1=1.0 / D, scalar2=EPS, op0=ALU.mult, op1=ALU.add), reads=[rs], writes=[rs])
        k.op("act", lambda e: e.activation(out=ss[0:n, 2:3], in_=ss[0:n, 1:2], func=AF.Sqrt), reads=[rs], writes=[rs])
        k.op("dve", lambda e: e.reciprocal(out=ss[0:n, 3:4], in_=ss[0:n, 2:3]), reads=[rs], writes=[rs])
        k.op("dve", lambda e: e.scalar_tensor_tensor(out=at[0:n, :], in0=ht[0:n, :], scalar=ss[0:n, 3:4], in1=Wt[s][0:n, :], op0=ALU.mult, op1=ALU.mult),
             reads=[hreg, rs, (tag + "_W", s)], writes=[areg])
        k.op("pool", lambda e: e.tensor_tensor(out=at[0:n, :], in0=at[0:n, :], in1=SHt[s][0:n, :], op=ALU.add), reads=[areg, (tag + "_SH", s)], writes=[areg])

    def transpose_block(self, at, areg, n, dst, dreg, col0, evac="act"):
        k = self.k
        for half in range(2):
            b = self.nps()
            for j in range(4):
                kc = half * 4 + j
                k.op("pe", lambda e: e.transpose(out=self.ps[b][:, j * 128:j * 128 + n], in_=at[0:n, kc * 128:(kc + 1) * 128], identity=self.idt[0:n, 0:n]),
                     reads=[areg, "idt"], writes=[("ps", b)])
            src = self.ps[b][:, :].rearrange("p (j c) -> p j c", j=4)[:, :, 0:n]
            dsta = dst[:, half * 4:half * 4 + 4, col0:col0 + n]
            if evac == "act":
                k.op("act", lambda e: e.activation(out=dsta, in_=src, func=AF.Identity), reads=[("ps", b)], writes=[dreg])
            else:
                k.op("dve", lambda e: e.tensor_copy(out=dsta, in_=src), reads=[("ps", b)], writes=[dreg])

    def load_w_bf16(self, dst, src_rows_ap, writes, q="pool"):
        nk = dst.shape[1]
        for kc in range(nk):
            self.k.dma(q, dst[:, kc, :], src_rows_ap[kc * 128:(kc + 1) * 128, :], writes=writes, max_dma_last_dim=4096)

    def stiles(self):
        out = []
        for t0 in range(0, self.M, 512):
            out.append((t0, min(512, self.M - t0), 1))
        for t0 in range(self.M, self.TT, 512):
            out.append((t0, min(512, self.TT - t0), 0))
        return out

    def even_layer(self, layer, need_ctx):
        j = layer // 2
        self.even_proj(layer, j)
        self.pool_stage(j)
        self.attention(j, need_ctx)
        self.mix_out(layer, self.ev_w_out[j], 8, need_ctx, self.even_cat_loader)

    def even_proj(self, layer, j):
        k = self.k
        M, TT = self.M, self.TT
        with ExitStack() as es:
            wIn = self.sb("ev_wIn", [128, KC, 928], BF16, es=es)
            self.load_w_bf16(wIn, self.ev_w_in[j], ["ev_wIn"])
            wuq = self.sb("ev_wuq", [128, 2, 768], BF16, es=es)
            self.load_w_bf16(wuq, self.w_uq[j], ["ev_wuq"])
            wkpad = self.sb("ev_wk", [128, 8, 96], BF16, es=es)
            wv = self.sb("ev_wv", [128, 8, 64], BF16, es=es)
            k.op("pool", lambda e: e.memset(wkpad[:], 0.0), writes=["ev_wk"])
            ukv = self.w_ukv[j].rearrange("k (h two d) -> k h two d", h=8, two=2)
            k.dma("pool", wkpad[:, :, 0:64], ukv[:, :, 0, :], reads=[], writes=["ev_wk"])
            k.dma("pool", wv[:], ukv[:, :, 1, :], writes=["ev_wv"])
            selpe = self.sb("ev_selpe", [32, 96], BF16, es=es)
            k.dma("pool", selpe[:], self.selpe[:, :], writes=["ev_selpe"])
            permT = self.sb("ev_permT", [96, 96], es=es)
            k.dma("sp", permT[:], self.permT[:, :], writes=["ev_permT"])
            gq = self.sb("ev_gq", [128, 2], es=es)
            gkv = self.sb("ev_gkv", [128, 1], es=es)
            gqn = self.sb("ev_gqn", [96, 1], es=es)
            gkn = self.sb("ev_gkn", [96, 1], es=es)
            self.col_load(gq, self.qa_g[j, :], 128, ["ev_g"])
            self.col_load(gkv, self.kva_g[j, :], 128, ["ev_g"])
            self.col_load(gqn, self.qn_g[j, :], 96, ["ev_g"])
            self.col_load(gkn, self.kn_g[j, :], 96, ["ev_g"])
            Wt = self.load_mod(es, 1, "ev_W")
            SHt = self.load_mod(es, 0, "ev_SH")

            ht = [self.sb("ev_h%d" % i, [128, D], es=es) for i in range(2)]
            at = [self.sb("ev_a%d" % i, [128, D], es=es) for i in range(2)]
            self.nsc = [(self.sb("ev_ss%d" % i, [128, 4], es=es), self.sb("ev_jk%d" % i, [128, D], BF16, es=es)) for i in range(2)]
            aT = [self.sb("ev_aT%d" % i, [128, KC, 512], BF16, es=es) for i in range(2)]
            poolS = [self.sb("ev_pS%d" % i, [128, 4, 512], es=es) for i in range(2)]
            zq = self.sb("ev_zq", [128, 2, 512], es=es)
            zkv = self.sb("ev_zkv", [128, 512], es=es)
            kpe = self.sb("ev_kpe", [32, 512], BF16, es=es)
            sq = self.sb("ev_sq", [128, 2, 512], es=es)
            rq = self.sb("ev_rq", [128, 512], es=es)
            cqT = self.sb("ev_cqT", [128, 2, 512], BF16, es=es)
            ckvT = self.sb("ev_ckvT", [128, 512], BF16, es=es)
            cosS = self.sb("ev_cos", [96, 512], es=es)
            sinS = self.sb("ev_sin", [96, 512], es=es)
            hq = [self.sb("ev_hq%d" % i, [96, 512], es=es) for i in range(2)]
            hsq = [self.sb("ev_hsq%d" % i, [96, 512], es=es) for i in range(2)]
            hr = [self.sb("ev_hr%d" % i, [96, 512], es=es) for i in range(2)]
            hn = [self.sb("ev_hn%d" % i, [96, 512], es=es) for i in range(2)]
            ho = [self.sb("ev_ho%d" % i, [96, 512], BF16, es=es) for i in range(2)]
            vS = [self.sb("ev_vS%d" % i, [128, 512], BF16, es=es) for i in range(2)]
            hcnt = [0]
            bi = 0
            for sti, (t0, n, s) in enumerate(self.stiles()):
                ai = sti % 2
                for blk in range(n // 128):
                    i = bi % 2
                    bi += 1
                    r0 = t0 + blk * 128
                    k.dma("sp", ht[i][:], self.h_all[r0:r0 + 128, :], reads=self.hreg(r0, 128), writes=[("ev_h", i)])
                    self.tm_norm(ht[i], ("ev_h", i), at[i], ("ev_a", i), s, Wt, SHt, 128, "ev")
                    self.transpose_block(at[i], ("ev_a", i), 128, aT[ai], ("ev_aT", ai), blk * 128)
                for mc in range(8):
                    msz = 128 if mc < 7 else 32
                    b = self.nps()
                    for kc in range(KC):
                        k.op("pe", lambda e: e.matmul(self.ps[b][0:msz, 0:n], lhsT=wIn[:, kc, mc * 128:mc * 128 + msz], rhs=aT[ai][:, kc, 0:n], start=(kc == 0), stop=(kc == KC - 1)),
                             reads=["ev_wIn", ("ev_aT", ai)], writes=[("ps", b)])
                    if mc < 4:
                        k.op("act", lambda e: e.activation(out=poolS[ai][:, mc, 0:n], in_=self.ps[b][:, 0:n], func=AF.Identity), reads=[("ps", b)], writes=[("ev_pS", ai)])
                    elif mc < 6:
                        k.op("act", lambda e: e.activation(out=zq[:, mc - 4, 0:n], in_=self.ps[b][:, 0:n], func=AF.Identity), reads=[("ps", b)], writes=["ev_zq"])
                        k.op("dve", lambda e: e.tensor_tensor(out=sq[:, mc - 4, 0:n], in0=self.ps[b][:, 0:n], in1=zq[:, mc - 4, 0:n], op=ALU.mult), reads=[("ps", b), "ev_zq"], writes=["ev_sq"])
                    elif mc == 6:
                        k.op("act", lambda e: e.activation(out=zkv[:, 0:n], in_=self.ps[b][:, 0:n], func=AF.Identity), reads=[("ps", b)], writes=["ev_zkv"])
                    else:
                        k.op("act", lambda e: e.activation(out=kpe[:, 0:n], in_=self.ps[b][0:32, 0:n], func=AF.Identity), reads=[("ps", b)], writes=["ev_kpe"])
                k.dma("sp", self.poolT[:, t0:t0 + n].rearrange("(g p) t -> p g t", p=128), poolS[ai][:, :, 0:n], reads=[("ev_pS", ai)], writes=[("poolT", t0)])
                k.dma("sp", cosS[:, 0:n], self.cosT[:, t0:t0 + n], reads=self.rope_regs, writes=["ev_cos"])
                k.dma("sp", sinS[:, 0:n], self.sinT[:, t0:t0 + n], reads=self.rope_regs, writes=["ev_sin"])
                b = self.nps()
                for c in range(2):
                    k.op("pe", lambda e: e.matmul(self.ps[b][:, 0:n], lhsT=self.ones[:, :], rhs=sq[:, c, 0:n], start=(c == 0), stop=(c == 1)), reads=["ones", "ev_sq"], writes=[("ps", b)])
                k.op("act", lambda e: e.activation(out=rq[:, 0:n], in_=self.ps[b][:, 0:n], func=AF.Sqrt, scale=1.0 / 256, bias=self.epsT[0:128, 0:1]), reads=[("ps", b), "epsT"], writes=["ev_rq"])
                k.op("dve", lambda e: e.reciprocal(out=rq[:, 0:n], in_=rq[:, 0:n]), reads=["ev_rq"], writes=["ev_rq"])
                for c in range(2):
                    k.op("dve", lambda e: e.scalar_tensor_tensor(out=cqT[:, c, 0:n], in0=zq[:, c, 0:n], scalar=gq[:, c:c + 1], in1=rq[:, 0:n], op0=ALU.mult, op1=ALU.mult),
                         reads=["ev_zq", "ev_g", "ev_rq"], writes=["ev_cqT"])
                k.op("pool", lambda e: e.tensor_tensor(out=sq[:, 0, 0:n], in0=zkv[:, 0:n], in1=zkv[:, 0:n], op=ALU.mult), reads=["ev_zkv", "ev_sq"], writes=["ev_sq"])
                b = self.nps()
                k.op("pe", lambda e: e.matmul(self.ps[b][:, 0:n], lhsT=self.ones[:, :], rhs=sq[:, 0, 0:n], start=True, stop=True), reads=["ones", "ev_sq"], writes=[("ps", b)])
                k.op("act", lambda e: e.activation(out=rq[:, 0:n], in_=self.ps[b][:, 0:n], func=AF.Sqrt, scale=1.0 / 128, bias=self.epsT[0:128, 0:1]), reads=[("ps", b), "ev_rq", "epsT"], writes=["ev_rq"])
                k.op("dve", lambda e: e.reciprocal(out=rq[:, 0:n], in_=rq[:, 0:n]), reads=["ev_rq"], writes=["ev_rq"])
                k.op("dve", lambda e: e.scalar_tensor_tensor(out=ckvT[:, 0:n], in0=zkv[:, 0:n], scalar=gkv[:, 0:1], in1=rq[:, 0:n], op0=ALU.mult, op1=ALU.mult),
                     reads=["ev_zkv", "ev_g", "ev_rq"], writes=["ev_ckvT"])
                for h in range(8):
                    for isk in range(2):
                        b = self.nps()
                        if isk == 0:
                            for c in range(2):
                                k.op("pe", lambda e: e.matmul(self.ps[b][0:96, 0:n], lhsT=wuq[:, c, h * 96:(h + 1) * 96], rhs=cqT[:, c, 0:n], start=(c == 0), stop=(c == 1)),
                                     reads=["ev_wuq", "ev_cqT"], writes=[("ps", b)])
                        else:
                            k.op("pe", lambda e: e.matmul(self.ps[b][0:96, 0:n], lhsT=wkpad[:, h, :], rhs=ckvT[:, 0:n], start=True, stop=False), reads=["ev_wk", "ev_ckvT"], writes=[("ps", b)])
                            k.op("pe", lambda e: e.matmul(self.ps[b][0:96, 0:n], lhsT=selpe[:, :], rhs=kpe[:, 0:n], start=False, stop=True), reads=["ev_selpe", "ev_kpe"], writes=[("ps", b)])
                        self.head_norm_rope(b, n, gkn if isk else gqn, (self.kT if isk else self.qT)[h, :, t0:t0 + n], ("kT" if isk else "qT", h, t0),
                                            hq, hsq, hr, hn, ho, permT, cosS, sinS, hcnt)
                for blk in range(n // 128):
                    b = self.nps()
                    vi = blk % 2
                    k.op("pe", lambda e: e.matmul(self.ps[b][:, 0:512], lhsT=ckvT[:, blk * 128:(blk + 1) * 128], rhs=wv[:].rearrange("p h d -> p (h d)"), start=True, stop=True),
                         reads=["ev_ckvT", "ev_wv"], writes=[("ps", b)])
                    k.op("act", lambda e: e.activation(out=vS[vi][:, :], in_=self.ps[b][:, :], func=AF.Identity), reads=[("ps", b)], writes=[("ev_vS", vi)])
                    r0 = t0 + blk * 128
                    k.dma("sp", self.vR[:, r0:r0 + 128, :].rearrange("h t d -> t h d"), vS[vi][:, :].rearrange("p (h d) -> p h d", h=8), reads=[("ev_vS", vi)], writes=[("vR", r0 // 128)])
            k.barrier()

    def head_norm_rope(self, b, n, gcol, out_ap, oreg, hq, hsq, hr, hn, ho, permT, cosS, sinS, hcnt):
        k = self.k
        i = hcnt[0] % 2
        hcnt[0] += 1
        R = lambda nm: (nm, i)
        k.op("act", lambda e: e.activation(out=hq[i][:, 0:n], in_=self.ps[b][0:96, 0:n], func=AF.Identity), reads=[("ps", b)], writes=[R("hq")])
        k.op("act", lambda e: e.activation(out=hsq[i][:, 0:n], in_=self.ps[b][0:96, 0:n], func=AF.Square), reads=[("ps", b)], writes=[R("hsq")])
        b2 = self.nps()
        k.op("pe", lambda e: e.matmul(self.ps[b2][0:96, 0:n], lhsT=self.ones[0:96, 0:96], rhs=hsq[i][:, 0:n], start=True, stop=True), reads=["ones", R("hsq")], writes=[("ps", b2)])
        k.op("act", lambda e: e.activation(out=hr[i][:, 0:n], in_=self.ps[b2][0:96, 0:n], func=AF.Sqrt, scale=1.0 / 96, bias=self.epsT[0:96, 0:1]), reads=[("ps", b2), "epsT"], writes=[R("hr")])
        k.op("dve", lambda e: e.reciprocal(out=hr[i][:, 0:n], in_=hr[i][:, 0:n]), reads=[R("hr")], writes=[R("hr")])
        k.op("dve", lambda e: e.scalar_tensor_tensor(out=hn[i][:, 0:n], in0=hq[i][:, 0:n], scalar=gcol[:, 0:1], in1=hr[i][:, 0:n], op0=ALU.mult, op1=ALU.mult),
             reads=[R("hq"), "ev_g", R("hr")], writes=[R("hn")])
        b3 = self.nps()
        k.op("pe", lambda e: e.matmul(self.ps[b3][0:96, 0:n], lhsT=permT[:, :], rhs=hn[i][:, 0:n], start=True, stop=True), reads=["ev_permT", R("hn")], writes=[("ps", b3)])
        k.op("dve", lambda e: e.tensor_tensor(out=hsq[i][:, 0:n], in0=self.ps[b3][0:96, 0:n], in1=sinS[:, 0:n], op=ALU.mult), reads=[("ps", b3), "ev_sin", R("hsq")], writes=[R("hsq")])
        k.op("pool", lambda e: e.tensor_tensor(out=hq[i][:, 0:n], in0=hn[i][:, 0:n], in1=cosS[:, 0:n], op=ALU.mult), reads=[R("hn"), "ev_cos", R("hq")], writes=[R("hq")])
        k.op("dve", lambda e: e.tensor_tensor(out=ho[i][:, 0:n], in0=hq[i][:, 0:n], in1=hsq[i][:, 0:n], op=ALU.add), reads=[R("hq"), R("hsq")], writes=[R("ho")])
        k.dma("sp", out_ap, ho[i][:, 0:n], reads=[R("ho")], writes=[oreg])

    def pool_stage(self, j):
        k = self.k
        M, T, TT = self.M, self.T, self.TT
        with ExitStack() as es:
            pw = self.sb("pl_w", [128, 4, 128], BF16, es=es)
            k.dma("pool", pw[:], self.pool_w[j].rearrange("g c d -> c g d"), writes=["pl_w"])
            psc = self.sb("pl_sc", [128, 4], es=es)
            self.col_load(psc, self.pool_scale[j, :], 128, ["pl_sc"])
            xin = [self.sb("pl_x%d" % i, [128, 528], es=es) for i in range(2)]
            s1 = [self.sb("pl_s%d" % i, [128, 528], es=es) for i in range(2)]
            s2 = [self.sb("pl_t%d" % i, [128, 528], es=es) for i in range(2)]
            rc = [self.sb("pl_rc%d" % i, [128, 512], es=es) for i in range(2)]
            po = [self.sb("pl_po%d" % i, [128, 512], BF16, es=es) for i in range(2)]
            co = [self.sb("pl_co%d" % i, [128, 512], BF16, es=es) for i in range(2)]
            cnt = 0
            allpool = [("poolT", t0) for (t0, n, s) in self.stiles()]
            for g, w in enumerate((2, 4, 8, 16)):
                lo = w // 2
                hi = w - lo - 1
                for (q0, qn) in ((0, M), (M, T)):
                    for t0 in range(q0, q0 + qn, 512):
                        n = min(512, q0 + qn - t0)
                        i = cnt % 2
                        cnt += 1
                        X, S1, S2, RC = ("pl_x", i), ("pl_s", i), ("pl_t", i), ("pl_rc", i)
                        a0 = max(q0, t0 - 8)
                        a1 = min(q0 + qn, t0 + n + 8)
                        k.op("pool", lambda e: e.memset(xin[i][:], 0.0), writes=[X])
                        k.dma("sp", xin[i][:, 8 - (t0 - a0):8 + (a1 - t0)], self.poolT[g * 128:(g + 1) * 128, a0:a1], reads=allpool, writes=[X])
                        L = n + 16
                        cur, cr = xin[i], X
                        step = 1
                        tmp = [(S1, s1[i]), (S2, s2[i])]
                        ti = 0
                        while step < w:
                            rgn, dst = tmp[ti % 2]
                            ti += 1
                            L2 = L - step
                            k.op("dve", lambda e: e.tensor_tensor(out=dst[:, 0:L2], in0=cur[:, 0:L2], in1=cur[:, step:step + L2], op=ALU.add), reads=[cr], writes=[rgn])
                            cur, cr, L = dst, rgn, L2
                            step *= 2
                        k.op("pool", lambda e: e.memset(rc[i][:], 1.0 / w), writes=[RC])
                        for tt in range(n):
                            t = t0 + tt - q0
                            c = min(t + hi + 1, qn) - max(t - lo, 0)
                            if c != w:
                                k.op("pool", lambda e: e.memset(rc[i][:, tt:tt + 1], 1.0 / c), writes=[RC])
                            elif tt > 16 and tt < n - 17:
                                pass
                        o = 8 - lo
                        rgn, dst = tmp[ti % 2]
                        k.op("dve", lambda e: e.tensor_tensor(out=dst[:, 0:n], in0=cur[:, o:o + n], in1=rc[i][:, 0:n], op=ALU.mult), reads=[cr, RC], writes=[rgn])
                        k.op("dve", lambda e: e.tensor_tensor(out=po[i][:, 0:n], in0=dst[:, 0:n], in1=xin[i][:, 8:8 + n], op=ALU.subtract), reads=[rgn, X], writes=[("pl_po", i)])
                        b = self.nps()
                        k.op("pe", lambda e: e.matmul(self.ps[b][:, 0:n], lhsT=pw[:, g, :], rhs=po[i][:, 0:n], start=True, stop=True), reads=["pl_w", ("pl_po", i)], writes=[("ps", b)])
                        k.op("act", lambda e: e.activation(out=co[i][:, 0:n], in_=self.ps[b][:, 0:n], func=AF.Identity, scale=psc[:, g:g + 1]), reads=[("ps", b), "pl_sc"], writes=[("pl_co", i)])
                        k.dma("sp", self.catT[g * 128:(g + 1) * 128, t0:t0 + n], co[i][:, 0:n], reads=[("pl_co", i)], writes=[("catT", g, t0)])
            k.barrier()

    def attention(self, j, need_ctx):
        k = self.k
        M, T, TT, NB = self.M, self.T, self.TT, self.NB
        scale = 96.0 ** -0.5
        qregs = lambda nm, h: [(nm, h, t0) for (t0, n, s) in self.stiles()]
        with ExitStack() as es:
            KT = [self.sb("at_K%d" % i, [96, TT], BF16, es=es) for i in range(2)]
            QT = [self.sb("at_Q%d" % i, [96, TT], BF16, es=es) for i in range(2)]
            VA = [self.sb("at_V%d" % i, [128, NB, 65], BF16, es=es) for i in range(2)]
            pT = [self.sb("at_p%d" % i, [128, 512], BF16, es=es) for i in range(4)]
            oS = [self.sb("at_o%d" % i, [65, 512], es=es) for i in range(2)]
            pcnt = 0
            ocnt = 0
            for h in range(8):
                i = h % 2
                k.dma("sp", KT[i][:], self.kT[h, :, :], reads=qregs("kT", h), writes=[("at_K", i)])
                k.dma("sp", QT[i][:], self.qT[h, :, :], reads=qregs("qT", h), writes=[("at_Q", i)])
                k.op("pool", lambda e: e.memset(VA[i][:], 1.0), writes=[("at_V", i)])
                k.dma("sp", VA[i][:, :, 0:64], self.vR[h, :, :].rearrange("(b p) d -> p b d", p=128), reads=[("vR", b_) for b_ in range(NB)], writes=[("at_V", i)])
                qtiles = [(t0, n, s) for (t0, n, s) in self.stiles() if (s == 0 or need_ctx)]
                for (t0, n, s) in qtiles:
                    nkb = NB if s == 0 else M // 128
                    ob = self.nps(0, 2)
                    LA = 3
                    slots = {}
                    for it in range(nkb + LA):
                        if it < nkb:
                            kb = it
                            sbk = self.nps(2, 8)
                            k.op("pe", lambda e: e.matmul(self.ps[sbk][:, 0:n], lhsT=KT[i][:, kb * 128:(kb + 1) * 128], rhs=QT[i][:, t0:t0 + n], start=True, stop=True),
                                 reads=[("at_K", i), ("at_Q", i)], writes=[("ps", sbk)])
                            pi = pcnt % 4
                            pcnt += 1
                            slots[kb] = pi
                            k.op("act", lambda e: e.activation(out=pT[pi][:, 0:n], in_=self.ps[sbk][:, 0:n], func=AF.Exp, scale=scale), reads=[("ps", sbk)], writes=[("at_p", pi)])
                        if it - LA >= 0:
                            kb = it - LA
                            pi = slots.pop(kb)
                            k.op("pe", lambda e: e.matmul(self.ps[ob][0:65, 0:n], lhsT=VA[i][:, kb, :], rhs=pT[pi][:, 0:n], start=(kb == 0), stop=(kb == nkb - 1)),
                                 reads=[("at_V", i), ("at_p", pi)], writes=[("ps", ob)])
                    oi = ocnt % 2
                    ocnt += 1
                    k.op("dve", lambda e: e.tensor_copy(out=oS[oi][:, 0:n], in_=self.ps[ob][0:65, 0:n]), reads=[("ps", ob)], writes=[("at_o", oi)])
                    k.dma("sp", self.attU[h, :, t0:t0 + n], oS[oi][0:64, 0:n], reads=[("at_o", oi)], writes=[("attU", h, t0)])
                    k.dma("sp", self.asum[h:h + 1, t0:t0 + n], oS[oi][64:65, 0:n], reads=[("at_o", oi)], writes=[("asum", h, t0)])
            k.barrier()

    def even_cat_loader(self, es):
        k = self.k
        aU = [self.sb("mo_aU%d" % i, [128, 512], es=es) for i in range(2)]
        sB = [self.sb("mo_sB%d" % i, [128, 512], es=es) for i in range(2)]
        cnt = [0]

        def load(catS, creg, t0, n):
            k.dma("sp", catS[:, 0:4, 0:n], self.catT[0:512, t0:t0 + n].rearrange("(g p) t -> p g t", p=128),
                  reads=[("catT", g, t0) for g in range(4)], writes=[creg])
            for c in range(4):
                i = cnt[0] % 2
                cnt[0] += 1
                A, S = ("mo_aU", i), ("mo_sB", i)
                k.dma("sp", aU[i][:, 0:n], self.attU[2 * c:2 * c + 2, :, t0:t0 + n].rearrange("h d t -> (h d) t"),
                      reads=[("attU", 2 * c, t0), ("attU", 2 * c + 1, t0)], writes=[A])
                for hh in range(2):
                    k.dma("sp", sB[i][hh * 64:(hh + 1) * 64, 0:n], self.asum[2 * c + hh, t0:t0 + n].partition_broadcast(64),
                          reads=[("asum", 2 * c + hh, t0)], writes=[S])
                k.op("dve", lambda e: e.reciprocal(out=sB[i][:, 0:n], in_=sB[i][:, 0:n]), reads=[S], writes=[S])
                k.op("dve", lambda e: e.tensor_tensor(out=catS[:, 4 + c, 0:n], in0=aU[i][:, 0:n], in1=sB[i][:, 0:n], op=ALU.mult), reads=[A, S], writes=[creg])
        return load

    def mix_out(self, layer, w_ap, nkc, need_ctx, loader_factory, glu=False):
        k = self.k
        with ExitStack() as es:
            nout = 2 * D if glu else D
            wo = self.sb("mo_w", [128, nkc, nout], BF16, es=es)
            self.load_w_bf16(wo, w_ap, ["mo_w"])
            load = loader_factory(es)
            G1 = self.load_mod(es, 2, "mo_G1")
            catS = [self.sb("mo_cat%d" % i, [128, nkc, 512], BF16, es=es) for i in range(2)]
            ht = [self.sb("mo_h%d" % i, [128, D], es=es) for i in range(2)]
            tt = [self.sb("mo_t%d" % i, [128, D], es=es) for i in range(2)]
            sg = [self.sb("mo_sg%d" % i, [128, 512], es=es) for i in range(2)] if glu else None
            bi = 0
            for sti, (t0, n, s) in enumerate(self.stiles()):
                if s == 1 and not need_ctx:
                    continue
                ci = sti % 2
                load(catS[ci], ("mo_cat", ci), t0, n)
                for blk in range(n // 128):
                    i = bi % 2
                    bi += 1
                    r0 = t0 + blk * 128
                    k.dma("sp", ht[i][:], self.h_all[r0:r0 + 128, :], reads=self.hreg(r0, 128), writes=[("mo_h", i)])
                    for half in range(2):
                        b = self.nps()
                        for kc in range(nkc):
                            k.op("pe", lambda e: e.matmul(self.ps[b][:, :], lhsT=catS[ci][:, kc, blk * 128:(blk + 1) * 128], rhs=wo[:, kc, half * 512:(half + 1) * 512], start=(kc == 0), stop=(kc == nkc - 1)),
                                 reads=[("mo_cat", ci), "mo_w"], writes=[("ps", b)])
                        if glu:
                            b2 = self.nps()
                            for kc in range(nkc):
                                k.op("pe", lambda e: e.matmul(self.ps[b2][:, :], lhsT=catS[ci][:, kc, blk * 128:(blk + 1) * 128], rhs=wo[:, kc, D + half * 512:D + (half + 1) * 512], start=(kc == 0), stop=(kc == nkc - 1)),
                                     reads=[("mo_cat", ci), "mo_w"], writes=[("ps", b2)])
                            k.op("act", lambda e: e.activation(out=sg[i][:, :], in_=self.ps[b2][:, :], func=AF.Sigmoid), reads=[("ps", b2)], writes=[("mo_sg", i)])
                            k.op("dve", lambda e: e.tensor_tensor(out=sg[i][:, :], in0=self.ps[b][:, :], in1=sg[i][:, :], op=ALU.mult), reads=[("ps", b), ("mo_sg", i)], writes=[("mo_sg", i)])
                            k.op("dve", lambda e: e.tensor_tensor(out=tt[i][:, half * 512:(half + 1) * 512], in0=sg[i][:, :], in1=G1[s][:, half * 512:(half + 1) * 512], op=ALU.mult),
                                 reads=[("mo_sg", i), ("mo_G1", s)], writes=[("mo_t", i)])
                        else:
                            k.op("dve", lambda e: e.tensor_tensor(out=tt[i][:, half * 512:(half + 1) * 512], in0=self.ps[b][:, :], in1=G1[s][:, half * 512:(half + 1) * 512], op=ALU.mult),
                                 reads=[("ps", b), ("mo_G1", s)], writes=[("mo_t", i)])
                    k.op("pool", lambda e: e.tensor_tensor(out=tt[i][:, :], in0=tt[i][:, :], in1=ht[i][:, :], op=ALU.add), reads=[("mo_t", i), ("mo_h", i)], writes=[("mo_t", i)])
                    k.dma("sp", self.h_all[r0:r0 + 128, :], tt[i][:, :], reads=[("mo_t", i)], writes=self.hreg(r0, 128))
            k.barrier()

    def odd_layer(self, layer, need_ctx):
        jo = layer // 2
        if not hasattr(self, "Ugc"):
            NCH = self.TT // 8
            self.NCH = NCH
            self.Ugc = self.scr("Ugc", [128, 64, NCH])
            self.XR = self.scr("XR", [64, 2, 64, NCH])
            self.XF = self.scr("XF", [64, 2, 64, NCH])
            self.SR = self.scr("SR", [64, 2, 64, NCH])
            self.SF = self.scr("SF", [64, 2, 64, NCH])
            self.WinT = self.scr("WinT", [2, 2, 128, 64, 64])
            self.Wo = self.scr("Wo", [2, 2, 64, 64, 128])
            self.Mm = self.scr("Mm", [128, 64, 128])
            self.D8 = self.scr("D8", [2, 64, 2, 2, 64])
        self.s5_precompute(jo)
        self.s5_pass1(layer, jo)
        self.s5_scans()
        self.s5_pass2(layer, jo, need_ctx)

    def sincos(self, a, ai, b, c, P, n, reg):
        k = self.k
        ra, rb, rc = (reg, "a"), (reg, "b"), (reg, "c")
        A, AI, B, C = a[0:P, 0:n], ai[0:P, 0:n], b[0:P, 0:n], c[0:P, 0:n]
        k.op("dve", lambda e: e.tensor_scalar(out=B, in0=A, scalar1=1.0 / TWO_PI, scalar2=None, op0=ALU.mult), reads=[ra], writes=[rb])
        k.op("dve", lambda e: e.tensor_copy(out=AI, in_=B), reads=[rb], writes=[(reg, "ai")])
        k.op("dve", lambda e: e.tensor_copy(out=B, in_=AI), reads=[(reg, "ai")], writes=[rb])
        k.op("dve", lambda e: e.scalar_tensor_tensor(out=A, in0=B, scalar=-TWO_PI, in1=A, op0=ALU.mult, op1=ALU.add), reads=[ra, rb], writes=[ra])
        k.op("dve", lambda e: e.tensor_scalar(out=A, in0=A, scalar1=math.pi, scalar2=-math.pi, op0=ALU.min, op1=ALU.max), reads=[ra], writes=[ra])
        k.op("act", lambda e: e.activation(out=B, in_=A, func=AF.Sin), reads=[ra, rb], writes=[rb])
        k.op("dve", lambda e: e.scalar_tensor_tensor(out=C, in0=A, scalar=-1.0, in1=A, op0=ALU.mult, op1=ALU.max), reads=[ra], writes=[rc])
        k.op("dve", lambda e: e.tensor_scalar(out=C, in0=C, scalar1=-1.0, scalar2=math.pi / 2, op0=ALU.mult, op1=ALU.add), reads=[rc], writes=[rc])
        k.op("act", lambda e: e.activation(out=C, in_=C, func=AF.Sin), reads=[rc], writes=[rc])

    def s5_precompute(self, jo):
        k = self.k
        GB = 16
        with ExitStack() as es:
            sb = lambda n, sh, dt=F32: self.sb("s5p_" + n, sh, dt, es=es)
            maskf = sb("maskf", [128, 128])
            maskr = sb("maskr", [128, 128])
            DT = sb("DT", [128, 64])
            k.op("pool", lambda e: e.memset(maskf[:], 0.0), writes=["s5maskf"])
            k.op("pool", lambda e: e.memset(maskr[:], 0.0), writes=["s5maskr"])
            for jb in range(4):
                j0 = 2 * jb
                k.op("pool", lambda e: e.memset(maskf[jb * 32:(jb + 1) * 32, (j0 + 2) * 16:128], 1.0), writes=["s5maskf"]) if j0 + 2 < 8 else None
                k.op("pool", lambda e: e.memset(maskr[jb * 32:(jb + 1) * 32, 0:j0 * 16], 1.0), writes=["s5maskr"]) if j0 > 0 else None
            bandf = sb("bandf", [128, 32])
            bandr = sb("bandr", [128, 32])
            k.dma("sp", bandf[:], self.band[0], writes=["s5band"])
            k.dma("sp", bandr[:], self.band[1], writes=["s5band"])
            for jb in range(4):
                k.op("pool", lambda e: e.tensor_copy(out=maskf[jb * 32:(jb + 1) * 32, jb * 32:(jb + 1) * 32], in_=bandf[jb * 32:(jb + 1) * 32, :]), reads=["s5band"], writes=["s5maskf"])
                k.op("pool", lambda e: e.tensor_copy(out=maskr[jb * 32:(jb + 1) * 32, jb * 32:(jb + 1) * 32], in_=bandr[jb * 32:(jb + 1) * 32, :]), reads=["s5band"], writes=["s5maskr"])
            dsrc = self.s5_d[jo, :].rearrange("(g q) -> q g", q=16)
            for j in range(8):
                k.dma("sp", DT[j * 16:(j + 1) * 16, :], dsrc, writes=["s5DT"], allow_slow_non_contiguous=True)
            nat = sb("nat", [128, 64])
            lamr = sb("lamr", [64, 64]); lami = sb("lami", [64, 64]); dt = sb("dt", [64, 64])
            lr = sb("lr", [64, 64]); th = sb("th", [64, 64])
            MAG = sb("MAG", [64, 17, 64]); ANG = sb("ANG", [64, 17 * 64]); ANGi = sb("ANGi", [64, 17 * 64], I32)
            SN = sb("SN", [64, 17 * 64]); CS = sb("CS", [64, 17 * 64])
            TAr = sb("TAr", [64, 17, 64]); TAi = sb("TAi", [64, 17, 64])
            t1 = sb("t1", [64, 64]); t2 = sb("t2", [64, 64]); cr = sb("cr", [64, 64]); ci = sb("ci", [64, 64])
            bre = sb("bre", [64, 64, 16]); bim = sb("bim", [64, 64, 16]); Bbr = sb("Bbr", [64, 64, 16]); Bbi = sb("Bbi", [64, 64, 16])
            Ctr = sb("Ctr", [64, 64, 16]); Cti = sb("Cti", [64, 64, 16])
            d8 = sb("d8", [64, 2, 2, 64])
            Ere = sb("Ere", [64, GB, 8, 16]); EimN = sb("EimN", [64, GB, 8, 16]); Eim = sb("Eim", [64, GB, 8, 16])
            Rre = sb("Rre", [64, GB, 8, 16]); Rim = sb("Rim", [64, GB, 8, 16])
            Wre = sb("Wre", [64, GB, 8, 16]); WimN = sb("WimN", [64, GB, 8, 16])
            tmpA = sb("tmpA", [64, GB, 8, 16]); tmpB = sb("tmpB", [64, GB, 8, 16])
            wint = [sb("wint%d" % i, [128, 2, GB, 64]) for i in range(1)]
            Macc = sb("Macc", [128, 64, 128])
            mt = sb("mt", [128, 128])
            for d in range(2):
                R_ = lambda nm: ("s5p", nm)
                for (src, dst, nm) in ((self.s5_lam_re, lamr, "lamr"), (self.s5_lam_im, lami, "lami")):
                    k.dma("sp", nat[0:64, :], src[jo, d], writes=[R_("nat")])
                    b = self.nps()
                    k.op("pe", lambda e: e.transpose(out=self.ps[b][0:64, 0:64], in_=nat[0:64, :], identity=self.idt[0:64, 0:64]), reads=[R_("nat"), "idt"], writes=[("ps", b)])
                    k.op("dve", lambda e: e.tensor_copy(out=dst[:], in_=self.ps[b][0:64, 0:64]), reads=[("ps", b)], writes=[R_(nm)])
                k.dma("sp", dt[:], self.s5_log_dt[jo, d, :].partition_broadcast(64), writes=[R_("dt")])
                k.op("act", lambda e: e.activation(out=dt[:], in_=dt[:], func=AF.Exp), reads=[R_("dt")], writes=[R_("dt")])
                k.op("dve", lambda e: e.tensor_tensor(out=lr[:], in0=lamr[:], in1=dt[:], op=ALU.mult), reads=[R_("lamr"), R_("dt")], writes=[R_("lr")])
                k.op("dve", lambda e: e.tensor_tensor(out=th[:], in0=lami[:], in1=dt[:], op=ALU.mult), reads=[R_("lami"), R_("dt")], writes=[R_("th")])
                if d == 0:
                    kl = [7 - j for j in range(8)] + [t - 7 for t in range(8)] + [8]
                else:
                    kl = [j for j in range(8)] + [-t for t in range(8)] + [8]
                for i, kv in enumerate(kl):
                    k.op("act", lambda e: e.activation(out=MAG[:, i, :], in_=lr[:], func=AF.Exp, scale=float(kv)), reads=[R_("lr")], writes=[R_("MAG")])
                    k.op("dve", lambda e: e.tensor_scalar(out=ANG[:, i * 64:(i + 1) * 64], in0=th[:], scalar1=float(kv), scalar2=None, op0=ALU.mult), reads=[R_("th")], writes=[("s5sc", "a")])
                self.sincos(ANG, ANGi, SN, CS, 64, 17 * 64, "s5sc")
                k.op("dve", lambda e: e.tensor_tensor(out=TAr[:].rearrange("s k g -> s (k g)"), in0=MAG[:].rearrange("s k g -> s (k g)"), in1=CS[:], op=ALU.mult), reads=[R_("MAG"), ("s5sc", "c")], writes=[R_("TAr")])
                k.op("dve", lambda e: e.tensor_tensor(out=TAi[:].rearrange("s k g -> s (k g)"), in0=MAG[:].rearrange("s k g -> s (k g)"), in1=SN[:], op=ALU.mult), reads=[R_("MAG"), ("s5sc", "b")], writes=[R_("TAi")])
                TA = [R_("TAr"), R_("TAi")]
                i1 = kl.index(1)
                k.op("dve", lambda e: e.tensor_scalar(out=t1[:], in0=TAr[:, i1, :], scalar1=-1.0, scalar2=None, op0=ALU.add), reads=TA, writes=[R_("t1")])
                k.op("dve", lambda e: e.tensor_tensor(out=cr[:], in0=t1[:], in1=lamr[:], op=ALU.mult), reads=[R_("t1"), R_("lamr")], writes=[R_("cr")])
                k.op("dve", lambda e: e.tensor_tensor(out=t2[:], in0=TAi[:, i1, :], in1=lami[:], op=ALU.mult), reads=TA + [R_("lami")], writes=[R_("t2")])
                k.op("dve", lambda e: e.tensor_tensor(out=cr[:], in0=cr[:], in1=t2[:], op=ALU.add), reads=[R_("cr"), R_("t2")], writes=[R_("cr")])
                k.op("dve", lambda e: e.tensor_tensor(out=ci[:], in0=TAi[:, i1, :], in1=lamr[:], op=ALU.mult), reads=TA + [R_("lamr")], writes=[R_("ci")])
                k.op("dve", lambda e: e.tensor_tensor(out=t2[:], in0=t1[:], in1=lami[:], op=ALU.mult), reads=[R_("t1"), R_("lami"), R_("t2")], writes=[R_("t2")])
                k.op("dve", lambda e: e.tensor_tensor(out=ci[:], in0=ci[:], in1=t2[:], op=ALU.subtract), reads=[R_("ci"), R_("t2")], writes=[R_("ci")])
                k.op("dve", lambda e: e.tensor_tensor(out=t1[:], in0=lamr[:], in1=lamr[:], op=ALU.mult), reads=[R_("lamr"), R_("t1")], writes=[R_("t1")])
                k.op("dve", lambda e: e.tensor_tensor(out=t2[:], in0=lami[:], in1=lami[:], op=ALU.mult), reads=[R_("lami"), R_("t2")], writes=[R_("t2")])
                k.op("dve", lambda e: e.tensor_tensor(out=t1[:], in0=t1[:], in1=t2[:], op=ALU.add), reads=[R_("t1"), R_("t2")], writes=[R_("t1")])
                k.op("dve", lambda e: e.reciprocal(out=t1[:], in_=t1[:]), reads=[R_("t1")], writes=[R_("t1")])
                k.op("dve", lambda e: e.tensor_tensor(out=cr[:], in0=cr[:], in1=t1[:], op=ALU.mult), reads=[R_("cr"), R_("t1")], writes=[R_("cr")])
                k.op("dve", lambda e: e.tensor_tensor(out=ci[:], in0=ci[:], in1=t1[:], op=ALU.mult), reads=[R_("ci"), R_("t1")], writes=[R_("ci")])
                k.dma("sp", bre[:], self.s5_b_re[jo, d].rearrange("g s q -> s g q"), writes=[R_("bre")])
                k.dma("sp", bim[:], self.s5_b_im[jo, d].rearrange("g s q -> s g q"), writes=[R_("bim")])
                bc = lambda t: t[:].unsqueeze(2).to_broadcast([64, 64, 16])
                k.op("dve", lambda e: e.tensor_tensor(out=Bbr[:], in0=bre[:], in1=bc(cr), op=ALU.mult), reads=[R_("bre"), R_("cr")], writes=[R_("Bbr")])
                k.op("dve", lambda e: e.tensor_tensor(out=Bbi[:], in0=bim[:], in1=bc(cr), op=ALU.mult), reads=[R_("bim"), R_("cr")], writes=[R_("Bbi")])
                k.op("dve", lambda e: e.tensor_tensor(out=bim[:], in0=bim[:], in1=bc(ci), op=ALU.mult), reads=[R_("bim"), R_("ci"), R_("Bbi")], writes=[R_("bim")])
                k.op("dve", lambda e: e.tensor_tensor(out=bre[:], in0=bre[:], in1=bc(ci), op=ALU.mult), reads=[R_("bre"), R_("ci"), R_("Bbr")], writes=[R_("bre")])
                k.op("dve", lambda e: e.tensor_tensor(out=Bbr[:], in0=Bbr[:], in1=bim[:], op=ALU.subtract), reads=[R_("Bbr"), R_("bim")], writes=[R_("Bbr")])
                k.op("dve", lambda e: e.tensor_tensor(out=Bbi[:], in0=Bbi[:], in1=bre[:], op=ALU.add), reads=[R_("Bbi"), R_("bre")], writes=[R_("Bbi")])
                for (src, dst, nm) in ((self.s5_c_re, Ctr, "Ctr"), (self.s5_c_im, Cti, "Cti")):
                    csrc = src[jo, d].rearrange("g p s -> (g p) s")
                    for g8 in range(8):
                        k.dma("sp", nat[:, :], csrc[g8 * 128:(g8 + 1) * 128, :], writes=[R_("nat")])
                        b = self.nps()
                        k.op("pe", lambda e: e.transpose(out=self.ps[b][0:64, 0:128], in_=nat[:, :], identity=self.idt[:, :]), reads=[R_("nat"), "idt"], writes=[("ps", b)])
                        k.op("dve", lambda e: e.tensor_copy(out=dst[:, g8 * 8:(g8 + 1) * 8, :].rearrange("s g p -> s (g p)"), in_=self.ps[b][0:64, 0:128]), reads=[("ps", b)], writes=[R_(nm)])
                k.op("dve", lambda e: e.tensor_copy(out=d8[:, 0, 0, :], in_=TAr[:, 16, :]), reads=TA, writes=[R_("d8")])
                k.op("dve", lambda e: e.tensor_copy(out=d8[:, 0, 1, :], in_=TAr[:, 16, :]), reads=TA, writes=[R_("d8")])
                k.op("dve", lambda e: e.tensor_scalar(out=d8[:, 1, 0, :], in0=TAi[:, 16, :], scalar1=-1.0, scalar2=None, op0=ALU.mult), reads=TA, writes=[R_("d8")])
                k.op("dve", lambda e: e.tensor_copy(out=d8[:, 1, 1, :], in_=TAi[:, 16, :]), reads=TA, writes=[R_("d8")])
                k.dma("sp", self.D8[d], d8[:], reads=[R_("d8")], writes=[("D8", d)])
                for g0 in range(0, 64, GB):
                    gs = slice(g0, g0 + GB)
                    tb = lambda T_, i: T_[:, i, gs].unsqueeze(2).to_broadcast([64, GB, 16])
                    for j in range(8):
                        for (Tidx, Xr, Xi, Ore, Oim, OimN, onm) in ((j, Bbr, Bbi, Ere, Eim, EimN, "E"), (8 + j, Ctr, Cti, Rre, Rim, None, "R")):
                            srcs = TA + [R_("Bbr"), R_("Bbi"), R_("Ctr"), R_("Cti")]
                            k.op("dve", lambda e: e.tensor_tensor(out=tmpA[:, :, j, :], in0=Xr[:, gs, :], in1=tb(TAr, Tidx), op=ALU.mult), reads=srcs, writes=[R_("tmpA")])
                            k.op("pool", lambda e: e.tensor_tensor(out=tmpB[:, :, j, :], in0=Xi[:, gs, :], in1=tb(TAi, Tidx), op=ALU.mult), reads=srcs, writes=[R_("tmpB")])
                            k.op("dve", lambda e: e.tensor_tensor(out=Ore[:, :, j, :], in0=tmpA[:, :, j, :], in1=tmpB[:, :, j, :], op=ALU.subtract), reads=[R_("tmpA"), R_("tmpB")], writes=[R_(onm + "re")])
                            k.op("dve", lambda e: e.tensor_tensor(out=tmpA[:, :, j, :], in0=Xi[:, gs, :], in1=tb(TAr, Tidx), op=ALU.mult), reads=srcs + [R_("tmpA")], writes=[R_("tmpA")])
                            k.op("pool", lambda e: e.tensor_tensor(out=tmpB[:, :, j, :], in0=Xr[:, gs, :], in1=tb(TAi, Tidx), op=ALU.mult), reads=srcs + [R_("tmpB")], writes=[R_("tmpB")])
                            k.op("dve", lambda e: e.tensor_tensor(out=Oim[:, :, j, :], in0=tmpA[:, :, j, :], in1=tmpB[:, :, j, :], op=ALU.add), reads=[R_("tmpA"), R_("tmpB")], writes=[R_(onm + "im")])
                    k.op("dve", lambda e: e.tensor_scalar(out=EimN[:], in0=Eim[:], scalar1=-1.0, scalar2=None, op0=ALU.mult), reads=[R_("Eim")], writes=[R_("EimN")])
                    t8 = lambda T_: T_[:, 16, gs].unsqueeze(2).to_broadcast([64, GB, 128])
                    fl = lambda t: t[:].rearrange("s g j q -> s g (j q)")
                    k.op("dve", lambda e: e.tensor_tensor(out=fl(tmpA), in0=fl(Rre), in1=t8(TAr), op=ALU.mult), reads=TA + [R_("Rre"), R_("tmpA")], writes=[R_("tmpA")])
                    k.op("pool", lambda e: e.tensor_tensor(out=fl(tmpB), in0=fl(Rim), in1=t8(TAi), op=ALU.mult), reads=TA + [R_("Rim"), R_("tmpB")], writes=[R_("tmpB")])
                    k.op("dve", lambda e: e.tensor_tensor(out=fl(Wre), in0=fl(tmpA), in1=fl(tmpB), op=ALU.subtract), reads=[R_("tmpA"), R_("tmpB")], writes=[R_("Wre")])
                    k.op("dve", lambda e: e.tensor_tensor(out=fl(tmpA), in0=fl(Rim), in1=t8(TAr), op=ALU.mult), reads=TA + [R_("Rim"), R_("tmpA")], writes=[R_("tmpA")])
                    k.op("pool", lambda e: e.tensor_tensor(out=fl(tmpB), in0=fl(Rre), in1=t8(TAi), op=ALU.mult), reads=TA + [R_("Rre"), R_("tmpB")], writes=[R_("tmpB")])
                    k.op("dve", lambda e: e.scalar_tensor_tensor(out=fl(WimN), in0=fl(tmpA), scalar=-1.0, in1=fl(tmpB), op0=ALU.mult, op1=ALU.subtract), reads=[R_("tmpA"), R_("tmpB")], writes=[R_("WimN")])
                    k.dma("sp", self.Wo[d, 0, :, gs, :], fl(Wre), reads=[R_("Wre")], writes=[("Wo", d, 0, g0)])
                    k.dma("sp", self.Wo[d, 1, :, gs, :], fl(WimN), reads=[R_("WimN")], writes=[("Wo", d, 1, g0)])
                    for gl in range(GB):
                        g = g0 + gl
                        b = self.nps()
                        k.op("pe", lambda e: e.transpose(out=self.ps[b][:, 0:64], in_=Ere[:, gl, :, :].rearrange("s j q -> s (j q)"), identity=self.idt[0:64, 0:64]), reads=[R_("Ere"), "idt"], writes=[("ps", b)])
                        k.op("pe", lambda e: e.transpose(out=self.ps[b][:, 64:128], in_=Eim[:, gl, :, :].rearrange("s j q -> s (j q)"), identity=self.idt[0:64, 0:64]), reads=[R_("Eim"), "idt"], writes=[("ps", b)])
                        k.op("act", lambda e: e.activation(out=wint[0][:, :, gl, :], in_=self.ps[b][:, 0:128].rearrange("p (r s) -> p r s", r=2), func=AF.Identity), reads=[("ps", b)], writes=[R_("wint")])
                        b2 = self.nps()
                        k.op("pe", lambda e: e.matmul(self.ps[b2][:, 0:128], lhsT=Ere[:, gl, :, :].rearrange("s j q -> s (j q)"), rhs=Rre[:, gl, :, :].rearrange("s j q -> s (j q)"), start=True, stop=False), reads=[R_("Ere"), R_("Rre")], writes=[("ps", b2)])
                        k.op("pe", lambda e: e.matmul(self.ps[b2][:, 0:128], lhsT=EimN[:, gl, :, :].rearrange("s j q -> s (j q)"), rhs=Rim[:, gl, :, :].rearrange("s j q -> s (j q)"), start=False, stop=True), reads=[R_("EimN"), R_("Rim")], writes=[("ps", b2)])
                        if d == 0:
                            k.op("dve", lambda e: e.tensor_tensor(out=mt[:], in0=self.ps[b2][:, 0:128], in1=maskf[:], op=ALU.mult), reads=[("ps", b2), "s5maskf"], writes=[R_("mt")])
                            k.op("dve", lambda e: e.scalar_tensor_tensor(out=Macc[:, g, :], in0=self.idt[:, :], scalar=DT[:, g:g + 1], in1=mt[:], op0=ALU.mult, op1=ALU.add), reads=[R_("mt"), "idt", "s5DT"], writes=[("Macc", g)])
                        else:
                            k.op("dve", lambda e: e.tensor_tensor(out=mt[:], in0=self.ps[b2][:, 0:128], in1=maskr[:], op=ALU.mult), reads=[("ps", b2), "s5maskr"], writes=[R_("mt")])
                            k.op("pool", lambda e: e.tensor_tensor(out=Macc[:, g, :], in0=Macc[:, g, :], in1=mt[:], op=ALU.add), reads=[R_("mt"), ("Macc", g)], writes=[("Macc", g)])
                    for ri in range(2):
                        k.dma("sp", self.WinT[d, ri, :, gs, :], wint[0][:, ri, :, :], reads=[R_("wint")], writes=[("WinT", d, ri, g0)])
            k.dma("sp", self.Mm[:, :, :], Macc[:], reads=[("Macc", g) for g in range(64)], writes=["Mm"])
            k.barrier()

    def s5_regs(self):
        r = ["Mm"]
        for d in range(2):
            r.append(("D8", d))
            for ri in range(2):
                for g0 in range(0, 64, 16):
                    r += [("Wo", d, ri, g0), ("WinT", d, ri, g0)]
        return r

    def s5_scan(self, XS, xreg, D8t, cols, tmp, treg, dreg="s5D8t"):
        k = self.k
        for (c, cp) in cols:
            prev = XS[:, :, :, cp]
            k.op("dve", lambda e: e.tensor_tensor(out=tmp[:, 0, 0, :], in0=XS[:, 1, :, cp], in1=D8t[:, 1, 0, :], op=ALU.mult), reads=[xreg, dreg], writes=[(treg, 0)])
            k.op("dve", lambda e: e.tensor_tensor(out=tmp[:, 0, 1, :], in0=XS[:, 0, :, cp], in1=D8t[:, 1, 1, :], op=ALU.mult), reads=[xreg, dreg], writes=[(treg, 0)])
            k.op("dve", lambda e: e.tensor_tensor(out=tmp[:, 1, :, :], in0=prev, in1=D8t[:, 0, :, :], op=ALU.mult), reads=[xreg, dreg], writes=[(treg, 1)])
            k.op("dve", lambda e: e.tensor_tensor(out=XS[:, :, :, c], in0=XS[:, :, :, c], in1=tmp[:, 1, :, :], op=ALU.add), reads=[xreg, (treg, 1)], writes=[xreg])
            k.op("dve", lambda e: e.tensor_tensor(out=XS[:, :, :, c], in0=XS[:, :, :, c], in1=tmp[:, 0, :, :], op=ALU.add), reads=[xreg, (treg, 0)], writes=[xreg])

    def s5_pass1(self, layer, jo):
        k = self.k
        with ExitStack() as es:
            sb = lambda n, sh, dt=F32: self.sb("s5a_" + n, sh, dt, es=es)
            wIn = sb("wIn", [128, KC, D], BF16)
            self.load_w_bf16(wIn, self.od_w_in[jo], ["s5wIn"])
            Wt = self.load_mod(es, 1, "s5_W")
            SHt = self.load_mod(es, 0, "s5_SH")
            ht = [sb("h%d" % i, [128, D]) for i in range(2)]
            at = [sb("a%d" % i, [128, D]) for i in range(2)]
            self.nsc = [(sb("ss%d" % i, [128, 4]), sb("jk%d" % i, [128, D], BF16)) for i in range(2)]
            aT = sb("aT", [128, KC, 512], BF16)
            UU = sb("UU", [64, 64, 8, 16])
            Ublk = sb("Ublk", [128, 64, 64])
            XRb = [sb("XRb%d" % i, [64, 2, 2, 16, 64]) for i in range(2)]
            wt_ = [sb("wt%d" % i, [128, 2, 2, 16, 64]) for i in range(2)]
            bi = 0
            for sti, (t0, n, s) in enumerate(self.stiles()):
                nch = n // 8
                c0 = t0 // 8
                for blk in range(n // 128):
                    i = bi % 2
                    bi += 1
                    r0 = t0 + blk * 128
                    k.dma("sp", ht[i][:], self.h_all[r0:r0 + 128, :], reads=self.hreg(r0, 128), writes=[("s5h", i)])
                    self.tm_norm(ht[i], ("s5h", i), at[i], ("s5a", i), s, Wt, SHt, 128, "s5")
                    self.transpose_block(at[i], ("s5a", i), 128, aT, "s5aT", blk * 128)
                for j in range(8):
                    for half in range(2):
                        b = self.nps()
                        for kc in range(KC):
                            k.op("pe", lambda e: e.matmul(self.ps[b][0:nch, :], lhsT=aT[:, kc, j:n:8], rhs=wIn[:, kc, half * 512:(half + 1) * 512], start=(kc == 0), stop=(kc == KC - 1)),
                                 reads=["s5aT", "s5wIn"], writes=[("ps", b)])
                        k.op("act" if half else "dve", (lambda e: e.activation(out=UU[0:nch, half * 32:(half + 1) * 32, j, :], in_=self.ps[b][0:nch, :].rearrange("c (g q) -> c g q", q=16), func=AF.Identity)) if half else
                             (lambda e: e.tensor_copy(out=UU[0:nch, half * 32:(half + 1) * 32, j, :], in_=self.ps[b][0:nch, :].rearrange("c (g q) -> c g q", q=16))),
                             reads=[("ps", b)], writes=["s5UU"])
                for g0 in range(0, 64, 4):
                    b = self.nps()
                    for gl in range(4):
                        k.op("pe", lambda e: e.transpose(out=self.ps[b][:, gl * 64:gl * 64 + nch], in_=UU[0:nch, g0 + gl, :, :].rearrange("c j q -> c (j q)"), identity=self.idt[0:nch, 0:nch]),
                             reads=["s5UU", "idt"], writes=[("ps", b)])
                    k.op("act" if (g0 // 4) % 2 else "dve", (lambda e: e.activation(out=Ublk[:, g0:g0 + 4, 0:nch], in_=self.ps[b][:, 0:256].rearrange("p (g c) -> p g c", g=4)[:, :, 0:nch], func=AF.Identity)) if (g0 // 4) % 2 else
                         (lambda e: e.tensor_copy(out=Ublk[:, g0:g0 + 4, 0:nch], in_=self.ps[b][:, 0:256].rearrange("p (g c) -> p g c", g=4)[:, :, 0:nch])),
                         reads=[("ps", b)], writes=["s5Ublk"])
                k.dma("sp", self.Ugc[:, :, c0:c0 + nch], Ublk[:, :, 0:nch], reads=["s5Ublk"], writes=[("Ugc", sti)])
                for g0 in range(0, 64, 16):
                    wi = (g0 // 16) % 2
                    for d in range(2):
                        for ri in range(2):
                            k.dma("sp", wt_[wi][:, d, ri, :, :], self.WinT[d, ri, :, g0:g0 + 16, :], reads=self.s5_regs(), writes=[("s5wt", wi)])
                    for d in range(2):
                        for ri in range(2):
                            for g4 in range(0, 16, 4):
                                b = self.nps()
                                for gl in range(4):
                                    g = g0 + g4 + gl
                                    k.op("pe", lambda e: e.matmul(self.ps[b][0:64, gl * 64:gl * 64 + nch], lhsT=wt_[wi][:, d, ri, g4 + gl, :], rhs=Ublk[:, g, 0:nch], start=True, stop=True),
                                         reads=[("s5wt", wi), "s5Ublk"], writes=[("ps", b)])
                                src = self.ps[b][0:64, 0:256].rearrange("p (g c) -> p g c", g=4)[:, :, 0:nch]
                                if (ri + d) % 2 == 0:
                                    k.op("dve", lambda e: e.tensor_copy(out=XRb[wi][:, d, ri, g4:g4 + 4, 0:nch], in_=src), reads=[("ps", b)], writes=[("XRb", wi)])
                                else:
                                    k.op("act", lambda e: e.activation(out=XRb[wi][:, d, ri, g4:g4 + 4, 0:nch], in_=src, func=AF.Identity), reads=[("ps", b)], writes=[("XRb", wi)])
                    k.dma("sp", self.XF[:, :, g0:g0 + 16, c0:c0 + nch], XRb[wi][:, 0, :, :, 0:nch], reads=[("XRb", wi)], writes=[("XF", sti, g0)])
                    k.dma("sp", self.XR[:, :, g0:g0 + 16, c0:c0 + nch], XRb[wi][:, 1, :, :, 0:nch], reads=[("XRb", wi)], writes=[("XR", sti, g0)])
            k.barrier()

    def s5_scans(self):
        k = self.k
        tiles = self.stiles()
        with ExitStack() as es:
            sb = lambda n, sh, dt=F32: self.sb("s5s_" + n, sh, dt, es=es)
            XS = [sb("XS%d" % i, [64, 2, 64, 65]) for i in range(2)]
            D8t = [sb("D8t%d" % d, [64, 2, 2, 64]) for d in range(2)]
            tmp = sb("tmp", [64, 2, 2, 64])
            for d in range(2):
                k.dma("sp", D8t[d][:], self.D8[d], reads=self.s5_regs(), writes=[("s5D8t", d)])
            cnt = 0
            for d in range(2):
                if d == 0:
                    order = list(range(len(tiles)))
                else:
                    order = [ti for ti, t in enumerate(tiles) if t[2] == 1][::-1] + [ti for ti, t in enumerate(tiles) if t[2] == 0][::-1]
                src = self.XF if d == 0 else self.XR
                dst = self.SF if d == 0 else self.SR
                prev = None
                for sti in order:
                    t0, n, s_ = tiles[sti]
                    nch = n // 8
                    c0 = t0 // 8
                    i = cnt % 2
                    cnt += 1
                    X, xr = XS[i], ("s5XS", i)
                    xo = 1 if d == 0 else 0
                    cin = 0 if d == 0 else nch
                    k.dma("sp", X[:, :, :, xo:xo + nch], src[:, :, :, c0:c0 + nch], reads=[("XF" if d == 0 else "XR", sti, g0) for g0 in range(0, 64, 16)], writes=[xr])
                    if prev is None:
                        k.op("pool", lambda e: e.memset(X[:, :, :, cin], 0.0), writes=[xr])
                    else:
                        pX, pxr, pcol = prev
                        k.op("dve", lambda e: e.tensor_copy(out=X[:, :, :, cin], in_=pX[:, :, :, pcol]), reads=[pxr], writes=[xr])
                    if d == 0:
                        cols = [(c + 1, c) for c in range(nch)]
                        prev = (X, xr, nch)
                    else:
                        cols = [(c, c + 1) for c in range(nch - 1, -1, -1)]
                        prev = (X, xr, 0)
                    self.s5_scan(X, xr, D8t[d], cols, tmp, "s5tmp", dreg=("s5D8t", d))
                    io = 0 if d == 0 else 1
                    k.dma("sp", dst[:, :, :, c0:c0 + nch], X[:, :, :, io:io + nch], reads=[xr], writes=[("SF" if d == 0 else "SR", sti)])
            k.barrier()

    def s5_pass2(self, layer, jo, need_ctx):
        k = self.k
        tiles = self.stiles()
        order = [ti for ti, t in enumerate(tiles) if t[2] == 1][::-1] + [ti for ti, t in enumerate(tiles) if t[2] == 0][::-1]
        with ExitStack() as es:
            sb = lambda n, sh, dt=F32: self.sb("s5b_" + n, sh, dt, es=es)
            wo = sb("wo", [128, KC, 2 * D], BF16)
            self.load_w_bf16(wo, self.od_w_out[jo], ["s5wo"])
            G1 = self.load_mod(es, 2, "s5_G1")
            SRb = [sb("SRb%d" % i, [64, 2, 8, 64]) for i in range(2)]
            SFb = [sb("SFb%d" % i, [64, 2, 8, 64]) for i in range(2)]
            Ub = [sb("Ub%d" % i, [128, 8, 64]) for i in range(2)]
            Mt = [sb("Mt%d" % i, [128, 8, 128]) for i in range(2)]
            Wt_ = [sb("Wt%d" % i, [64, 2, 2, 8, 128]) for i in range(2)]
            Yblk = sb("Yblk", [128, 64, 64])
            YT = sb("YT", [64, 8, D])
            catS = sb("catS", [128, KC, 512], BF16)
            ht = [sb("h%d" % i, [64, D]) for i in range(2)]
            tt = [sb("t%d" % i, [64, D]) for i in range(2)]
            sg = [sb("sg%d" % i, [64, 512]) for i in range(2)]
            bi = 0
            for oi, sti in enumerate(order):
                t0, n, s = tiles[sti]
                nch = n // 8
                c0 = t0 // 8
                if s == 1 and not need_ctx:
                    continue
                for g0 in range(0, 64, 8):
                    wi = (g0 // 8) % 2
                    k.dma("sp", SFb[wi][:, :, :, 0:nch], self.SF[:, :, g0:g0 + 8, c0:c0 + nch], reads=[("SF", sti)], writes=[("s5SFb", wi)])
                    k.dma("sp", SRb[wi][:, :, :, 0:nch], self.SR[:, :, g0:g0 + 8, c0:c0 + nch], reads=[("SR", sti)], writes=[("s5SRb", wi)])
                    k.dma("sp", Ub[wi][:, :, 0:nch], self.Ugc[:, g0:g0 + 8, c0:c0 + nch], reads=[("Ugc", sti)], writes=[("s5Ub", wi)])
                    k.dma("sp", Mt[wi][:], self.Mm[:, g0:g0 + 8, :], reads=self.s5_regs(), writes=[("s5Mt", wi)])
                    for d in range(2):
                        for ri in range(2):
                            k.dma("sp", Wt_[wi][:, d, ri, :, :], self.Wo[d, ri, :, g0:g0 + 8, :], reads=self.s5_regs(), writes=[("s5Wt", wi)])
                    for g4 in range(0, 8, 4):
                        b = self.nps()
                        for gl in range(4):
                            g = g0 + g4 + gl
                            o = self.ps[b][:, gl * 64:gl * 64 + nch]
                            k.op("pe", lambda e: e.matmul(o, lhsT=Mt[wi][:, g4 + gl, :], rhs=Ub[wi][:, g4 + gl, 0:nch], start=True, stop=False), reads=[("s5Mt", wi), ("s5Ub", wi)], writes=[("ps", b)])
                            for ri in range(2):
                                k.op("pe", lambda e: e.matmul(o, lhsT=Wt_[wi][:, 0, ri, g4 + gl, :], rhs=SFb[wi][:, ri, g4 + gl, 0:nch], start=False, stop=False), reads=[("s5Wt", wi), ("s5SFb", wi)], writes=[("ps", b)])
                            for ri in range(2):
                                k.op("pe", lambda e: e.matmul(o, lhsT=Wt_[wi][:, 1, ri, g4 + gl, :], rhs=SRb[wi][:, ri, g4 + gl, 0:nch], start=False, stop=(ri == 1)), reads=[("s5Wt", wi), ("s5SRb", wi)], writes=[("ps", b)])
                        k.op("act", lambda e: e.activation(out=Yblk[:, g0 + g4:g0 + g4 + 4, 0:nch], in_=self.ps[b][:, 0:256].rearrange("p (g c) -> p g c", g=4)[:, :, 0:nch], func=AF.Identity), reads=[("ps", b)], writes=["s5Yblk"])
                for g0 in range(0, 64, 4):
                    b = self.nps()
                    for gl in range(4):
                        k.op("pe", lambda e: e.transpose(out=self.ps[b][0:nch, gl * 128:(gl + 1) * 128], in_=Yblk[:, g0 + gl, 0:nch], identity=self.idt[:, :]), reads=["s5Yblk", "idt"], writes=[("ps", b)])
                    k.op("act", lambda e: e.activation(out=YT[0:nch, :, g0 * 16:(g0 + 4) * 16].rearrange("c t (g p) -> c g t p", g=4), in_=self.ps[b][0:nch, :].rearrange("c (g t p) -> c g t p", g=4, t=8), func=AF.Gelu),
                         reads=[("ps", b)], writes=["s5YT"])
                for tau in range(8):
                    self.transpose_block(YT[:, tau, :], "s5YT", nch, catS, "s5cat", tau * nch)
                    i = bi % 2
                    bi += 1
                    rows = self.h_all[t0 + tau:t0 + n:8, :]
                    k.dma("sp", ht[i][0:nch, :], rows, reads=self.hreg(t0, n), writes=[("s5h2", i)])
                    for half in range(2):
                        b = self.nps()
                        b2 = self.nps()
                        for kc in range(KC):
                            k.op("pe", lambda e: e.matmul(self.ps[b][0:nch, :], lhsT=catS[:, kc, tau * nch:(tau + 1) * nch], rhs=wo[:, kc, half * 512:(half + 1) * 512], start=(kc == 0), stop=(kc == KC - 1)),
                                 reads=["s5cat", "s5wo"], writes=[("ps", b)])
                        for kc in range(KC):
                            k.op("pe", lambda e: e.matmul(self.ps[b2][0:nch, :], lhsT=catS[:, kc, tau * nch:(tau + 1) * nch], rhs=wo[:, kc, D + half * 512:D + (half + 1) * 512], start=(kc == 0), stop=(kc == KC - 1)),
                                 reads=["s5cat", "s5wo"], writes=[("ps", b2)])
                        k.op("act", lambda e: e.activation(out=sg[i][0:nch, :], in_=self.ps[b2][0:nch, :], func=AF.Sigmoid), reads=[("ps", b2)], writes=[("s5sg", i)])
                        k.op("dve", lambda e: e.tensor_tensor(out=sg[i][0:nch, :], in0=self.ps[b][0:nch, :], in1=sg[i][0:nch, :], op=ALU.mult), reads=[("ps", b), ("s5sg", i)], writes=[("s5sg", i)])
                        k.op("dve", lambda e: e.tensor_tensor(out=tt[i][0:nch, half * 512:(half + 1) * 512], in0=sg[i][0:nch, :], in1=G1[s][0:nch, half * 512:(half + 1) * 512], op=ALU.mult),
                             reads=[("s5sg", i), ("s5_G1", s)], writes=[("s5t", i)])
                    k.op("pool", lambda e: e.tensor_tensor(out=tt[i][0:nch, :], in0=tt[i][0:nch, :], in1=ht[i][0:nch, :], op=ALU.add), reads=[("s5t", i), ("s5h2", i)], writes=[("s5t", i)])
                    k.dma("sp", rows, tt[i][0:nch, :], reads=[("s5t", i)], writes=self.hreg(t0, n))
            k.barrier()

    def moe_layer(self, layer, need_ctx):
        k = self.k
        M, T, TT = self.M, self.T, self.TT
        capL, capC = self.capL, self.capC
        nLb = capL // 128
        with ExitStack() as es:
            G2 = self.load_mod(es, 5, "mz_g2")
            nLb_ = capL // 128
            idxT = self.sb("mz_idxT", [128, nLb_ + 1, NE], U32, es=es)
            gateT = self.sb("mz_gateT", [128, nLb_ + 1, NE], es=es)
            tk_es = ExitStack()
            AFF = self.sb("mz_aff", [NE, TT], es=tk_es)
            with ExitStack() as es2:
                Wt = self.load_mod(es2, 4, "mz_W")
                SHt = self.load_mod(es2, 3, "mz_SH")
                rw = self.sb("mz_rw", [128, KC, NE], es=es2)
                k.dma("sp", rw[:], self.router_w[layer].rearrange("(kc p) e -> p kc e", p=128), writes=["mz_rw"])
                ht = [self.sb("mz_h%d" % i, [128, D], es=es2) for i in range(2)]
                at = [self.sb("mz_a%d" % i, [128, D], es=es2) for i in range(2)]
                self.nsc = [(self.sb("mz_ss%d" % i, [128, 4], es=es2), self.sb("mz_jk%d" % i, [128, D], BF16, es=es2)) for i in range(2)]
                h2T = [self.sb("mz_h2T%d" % i, [128, KC, 512], es=es2) for i in range(2)]
                ex = [self.sb("mz_ex%d" % i, [NE, 512], es=es2) for i in range(2)]
                rs = [self.sb("mz_rs%d" % i, [NE, 512], es=es2) for i in range(2)]
                bi = 0
                for sti, (t0, n, s) in enumerate(self.stiles()):
                    if s == 1 and not need_ctx:
                        continue
                    ai = sti % 2
                    for blk in range(n // 128):
                        i = bi % 2
                        bi += 1
                        r0 = t0 + blk * 128
                        k.dma("sp", ht[i][:], self.h_all[r0:r0 + 128, :], reads=self.hreg(r0, 128), writes=[("mz_h", i)])
                        self.tm_norm(ht[i], ("mz_h", i), at[i], ("mz_a", i), s, Wt, SHt, 128, "mz")
                        k.dma("sp", self.h2rows[r0:r0 + 128, :], at[i][:, :], reads=[("mz_a", i)], writes=[("h2rows", r0 // 128)])
                        self.transpose_block(at[i], ("mz_a", i), 128, h2T[ai], ("mz_h2T", ai), blk * 128, evac="dve" if blk % 2 else "act")
                    b = self.nps()
                    for kc in range(KC):
                        k.op("pe", lambda e: e.matmul(self.ps[b][0:NE, 0:n], lhsT=rw[:, kc, :], rhs=h2T[ai][:, kc, 0:n], start=(kc == 0), stop=(kc == KC - 1)),
                             reads=["mz_rw", ("mz_h2T", ai)], writes=[("ps", b)])
                    k.op("act", lambda e: e.activation(out=ex[ai][:, 0:n], in_=self.ps[b][0:NE, 0:n], func=AF.Exp), reads=[("ps", b)], writes=[("mz_ex", ai)])
                    b2 = self.nps()
                    k.op("pe", lambda e: e.matmul(self.ps[b2][0:NE, 0:n], lhsT=self.ones[0:NE, 0:NE], rhs=ex[ai][:, 0:n], start=True, stop=True), reads=["ones", ("mz_ex", ai)], writes=[("ps", b2)])
                    k.op("dve", lambda e: e.reciprocal(out=rs[ai][:, 0:n], in_=self.ps[b2][0:NE, 0:n]), reads=[("ps", b2)], writes=[("mz_rs", ai)])
                    k.op("dve", lambda e: e.tensor_tensor(out=AFF[:, t0:t0 + n], in0=ex[ai][:, 0:n], in1=rs[ai][:, 0:n], op=ALU.mult), reads=[("mz_ex", ai), ("mz_rs", ai)], writes=["mz_aff"])
                k.barrier()
            ncap = capL + (capC if need_ctx else 0)
            nsb = nLb + (1 if need_ctx else 0)
            mx = self.sb("mz_mx", [NE, capL + capC], es=tk_es)
            mi = self.sb("mz_mi", [NE, capL + capC], U32, es=tk_es)
            mf = self.sb("mz_mf", [NE, capL + capC], es=tk_es)
            segs = [(M, T, 0, capL)] + ([(0, M, capL, capC)] if need_ctx else [])
            for (c0, cn, s0, cap) in segs:
                for r in range(cap // 8):
                    sl = slice(s0 + r * 8, s0 + r * 8 + 8)
                    k.op("dve", lambda e: e.max(out=mx[:, sl], in_=AFF[:, c0:c0 + cn]), reads=["mz_aff"], writes=["mz_mx"])
                    k.op("dve", lambda e: e.max_index(out=mi[:, sl], in_max=mx[:, sl], in_values=AFF[:, c0:c0 + cn]), reads=["mz_aff", "mz_mx"], writes=["mz_mi"])
                    k.op("dve", lambda e: e.match_replace(out=AFF[:, c0:c0 + cn], in_to_replace=mx[:, sl], in_values=AFF[:, c0:c0 + cn], imm_value=-1.0), reads=["mz_aff", "mz_mx", "mz_mi"], writes=["mz_aff"])
                k.op("dve", lambda e: e.tensor_copy(out=mf[:, s0:s0 + cap], in_=mi[:, s0:s0 + cap]), reads=["mz_mi"], writes=["mz_mf"])
                k.op("dve", lambda e: e.tensor_scalar(out=mf[:, s0:s0 + cap], in0=mf[:, s0:s0 + cap], scalar1=float(c0), scalar2=None, op0=ALU.add), reads=["mz_mf"], writes=["mz_mf"])
            for sbk in range(nsb):
                s0 = sbk * 128
                nn = 128 if sbk < nLb else capC
                b = self.nps()
                k.op("pe", lambda e: e.transpose(out=self.ps[b][0:nn, 0:NE], in_=mf[:, s0:s0 + nn], identity=self.idt[0:NE, 0:NE]), reads=["mz_mf", "idt"], writes=[("ps", b)])
                k.op("pe", lambda e: e.transpose(out=self.ps[b][0:nn, NE:2 * NE], in_=mx[:, s0:s0 + nn], identity=self.idt[0:NE, 0:NE]), reads=["mz_mx", "idt"], writes=[("ps", b)])
                k.op("dve", lambda e: e.tensor_copy(out=idxT[0:nn, sbk, :], in_=self.ps[b][0:nn, 0:NE]), reads=[("ps", b)], writes=["mz_idxT"])
                k.op("dve", lambda e: e.tensor_copy(out=gateT[0:nn, sbk, :], in_=self.ps[b][0:nn, NE:2 * NE]), reads=[("ps", b)], writes=["mz_gateT"])
            k.barrier()
            tk_es.close()
            wg = self.sb("mz_wg", [128, KC, DFF], BF16, es=es)
            wu = self.sb("mz_wu", [128, KC, DFF], BF16, es=es)
            wd = self.sb("mz_wd", [128, NFC, D], BF16, es=es)
            xg = [self.sb("mz_xg%d" % i, [128, D], es=es) for i in range(2)]
            xeT = [self.sb("mz_xeT%d" % i, [128, KC, 512], BF16, es=es) for i in range(1)]
            actT = self.sb("mz_act", [128, NFC, 512], BF16, es=es)
            sl_ = [self.sb("mz_sl%d" % i, [128, 512], es=es) for i in range(2)]
            ye = [self.sb("mz_ye%d" % i, [128, D], es=es) for i in range(2)]
            allh = [("h", b_) for b_ in range(self.NB)]
            allh2 = [("h2rows", b_) for b_ in range(self.NB)]
            blocks = [(sbk, 128, 0) for sbk in range(nLb)] + ([(nLb, capC, 1)] if need_ctx else [])
            subs = []
            if need_ctx:
                subs.append([blocks[-1]])
            for i0 in range(0, nLb, 4):
                subs.append(blocks[i0:min(i0 + 4, nLb)])
            gcnt = 0
            ycnt = 0
            scnt = 0
            def load_gu(e_):
                for kc in range(KC):
                    k.dma("pool", wg[:, kc, :], self.w_gate[layer, e_, kc * 128:(kc + 1) * 128, :], writes=[("mz_wg", kc)], max_dma_last_dim=4096)
                    k.dma("pool", wu[:, kc, :], self.w_up[layer, e_, kc * 128:(kc + 1) * 128, :], writes=[("mz_wu", kc)], max_dma_last_dim=4096)

            def load_d(e_):
                for fc in range(NFC):
                    fsz = min(128, DFF - fc * 128)
                    k.dma("pool", wd[0:fsz, fc, :], self.w_down[layer, e_, fc * 128:fc * 128 + fsz, :], writes=[("mz_wd", fc)], max_dma_last_dim=4096)

            for ex_ in range(NE):
                if ex_ == 0:
                    load_gu(0)
                    load_d(0)
                for si_, sub in enumerate(subs):
                    xi = 0
                    scnt += 1
                    ntok = sum(nn for (_, nn, _) in sub)
                    col = 0
                    cols = []
                    for (sbk, nn, s) in sub:
                        gi = gcnt % 2
                        gcnt += 1
                        k.dma("pool", reads=["mz_idxT"] + allh2, writes=[("mz_xg", gi)],
                              fn=lambda e: e.indirect_dma_start(out=xg[gi][0:nn, :], out_offset=None, in_=self.h2rows[:, :],
                                                                in_offset=bass.IndirectOffsetOnAxis(ap=idxT[0:nn, sbk, ex_:ex_ + 1], axis=0)))
                        self.transpose_block(xg[gi], ("mz_xg", gi), nn, xeT[xi], ("mz_xeT", xi), col, evac="act")
                        cols.append(col)
                        col += nn
                    for fc in range(NFC):
                        fsz = min(128, DFF - fc * 128)
                        bg = self.nps()
                        for kc in range(KC):
                            k.op("pe", lambda e: e.matmul(self.ps[bg][0:fsz, 0:ntok], lhsT=wg[:, kc, fc * 128:fc * 128 + fsz], rhs=xeT[xi][:, kc, 0:ntok], start=(kc == 0), stop=(kc == KC - 1)),
                                 reads=[("mz_wg", kc), ("mz_xeT", xi)], writes=[("ps", bg)])
                        bu = self.nps()
                        for kc in range(KC):
                            k.op("pe", lambda e: e.matmul(self.ps[bu][0:fsz, 0:ntok], lhsT=wu[:, kc, fc * 128:fc * 128 + fsz], rhs=xeT[xi][:, kc, 0:ntok], start=(kc == 0), stop=(kc == KC - 1)),
                                 reads=[("mz_wu", kc), ("mz_xeT", xi)], writes=[("ps", bu)])
                        si = fc % 2
                        k.op("act", lambda e: e.activation(out=sl_[si][0:fsz, 0:ntok], in_=self.ps[bg][0:fsz, 0:ntok], func=AF.Silu), reads=[("ps", bg)], writes=[("mz_sl", si)])
                        k.op("dve", lambda e: e.tensor_tensor(out=actT[0:fsz, fc, 0:ntok], in0=self.ps[bu][0:fsz, 0:ntok], in1=sl_[si][0:fsz, 0:ntok], op=ALU.mult),
                             reads=[("ps", bu), ("mz_sl", si)], writes=["mz_act"])
                    if si_ == len(subs) - 1 and ex_ + 1 < NE:
                        load_gu(ex_ + 1)
                    for bi_, (sbk, nn, s) in enumerate(sub):
                        yi = ycnt % 2
                        ycnt += 1
                        c0 = cols[bi_]
                        for half in range(2):
                            b = self.nps()
                            for fc in range(NFC):
                                fsz = min(128, DFF - fc * 128)
                                k.op("pe", lambda e: e.matmul(self.ps[b][0:nn, :], lhsT=actT[0:fsz, fc, c0:c0 + nn], rhs=wd[0:fsz, fc, half * 512:(half + 1) * 512], start=(fc == 0), stop=(fc == NFC - 1)),
                                     reads=["mz_act", ("mz_wd", fc)], writes=[("ps", b)])
                            k.op("dve", lambda e: e.scalar_tensor_tensor(out=ye[yi][0:nn, half * 512:(half + 1) * 512], in0=self.ps[b][0:nn, :], scalar=gateT[0:nn, sbk, ex_:ex_ + 1],
                                                                         in1=G2[s][0:nn, half * 512:(half + 1) * 512], op0=ALU.mult, op1=ALU.mult),
                                 reads=[("ps", b), "mz_gateT", ("mz_g2", s)], writes=[("mz_ye", yi)])
                        if si_ == len(subs) - 1 and bi_ == len(sub) - 1 and ex_ + 1 < NE:
                            load_d(ex_ + 1)
                        k.dma("pool", reads=["mz_idxT", ("mz_ye", yi)], writes=allh,
                              fn=lambda e: e.indirect_dma_start(out=self.h_all[:, :], out_offset=bass.IndirectOffsetOnAxis(ap=idxT[0:nn, sbk, ex_:ex_ + 1], axis=0),
                                                                in_=ye[yi][0:nn, :], in_offset=None, compute_op=ALU.add))
            k.barrier()


def host_consts(T, M):
    TT = T + M
    ident = np.eye(128, dtype=np.float32)
    pos96 = np.zeros((96, TT), np.float32)
    t = np.arange(T)
    row = (t // 64).astype(np.float32)
    colp = (t % 64).astype(np.float32)
    inv96 = np.zeros((96, 1), np.float32)
    inv = (10000.0 ** (-np.arange(8, dtype=np.float32) / 8)).astype(np.float32)
    for p in range(64, 96):
        pair = (p - 64) % 16
        pos96[p, M:] = row if pair < 8 else colp
        inv96[p, 0] = inv[pair % 8]
    permT = np.zeros((96, 96), np.float32)
    for m in range(64, 80):
        permT[m + 16, m] = -1.0
    for m in range(80, 96):
        permT[m - 16, m] = 1.0
    selpe = np.zeros((32, 96), np.float32)
    for i in range(32):
        selpe[i, 64 + i] = 1.0
    band = np.zeros((2, 128, 32), np.float32)
    for r in range(128):
        jr = (r % 32) // 16
        for c in range(32):
            jc = c // 16
            band[0, r, c] = 1.0 if jc >= jr else 0.0
            band[1, r, c] = 1.0 if jr >= jc else 0.0
    return dict(ident=ident, pos96=pos96, inv96=inv96, permT=permT, selpe=selpe, band=band)


WEIGHT_KEYS = ["ada_w", "ada_b", "norm1_g", "norm2_g", "router_w", "exp_w_gate", "exp_w_up", "exp_w_down", "ev_w_in", "pool_w",
               "pool_scale", "q_a_norm_g", "w_uq", "kv_a_norm_g", "w_ukv", "q_norm_g", "k_norm_g", "ev_w_out", "od_w_in",
               "s5_lam_re", "s5_lam_im", "s5_log_dt", "s5_b_re", "s5_b_im", "s5_c_re", "s5_c_im", "s5_d", "od_w_out"]


def run(inputs, depth=4, dbg=(), ncores=None):
    x = np.asarray(inputs["x"], np.float32)
    ctx = np.asarray(inputs["ctx"], np.float32)
    B, T, _ = x.shape
    M = ctx.shape[1]
    prog = Prog(T, M, depth, dbg)
    nc = prog.build()
    consts = host_consts(T, M)
    EVK = ["ev_w_in", "pool_w", "pool_scale", "q_a_norm_g", "w_uq", "kv_a_norm_g", "w_ukv", "q_norm_g", "k_norm_g", "ev_w_out"]
    shared = {}
    for kk in WEIGHT_KEYS:
        a = np.asarray(inputs[kk], np.float32)
        nlead = (depth + 1) // 2 if kk in EVK else (max(1, depth // 2) if (kk.startswith("s5_") or kk.startswith("od_")) else depth)
        shared[kk] = np.ascontiguousarray(a[:nlead])
    shared.update(consts)
    c = np.asarray(inputs["c"], np.float32)
    cc = np.asarray(inputs["c_ctx"], np.float32)
    in_maps = []
    for b in range(B):
        m = dict(shared)
        m["x"] = np.ascontiguousarray(x[b])
        m["ctx"] = np.ascontiguousarray(ctx[b])
        m["cfm"] = np.ascontiguousarray(np.concatenate([c[b].reshape(KC, 128).T, cc.reshape(KC, 128).T], axis=1))
        in_maps.append(m)
    ncore = len(in_maps) if ncores is None else ncores
    res = run_bass_kernel_spmd(nc, in_maps[:ncore], core_ids=list(range(ncore)))
    out = np.stack([np.asarray(r["y"]) for r in res.results], axis=0).astype(np.float32)
    return out, res, prog


def kernel(**inputs):
    out, _, _ = run(inputs, depth=4)
    return out
```
